# Optimizing a Trainium2 kernel written in Bass

```python
import math
import jax
import jax.numpy as jnp
from jax import lax
import numpy as np

D_MODEL = 1024
BATCH = 16
SEQ = 2048
DEPTH = 2

HEAD_DIM = 64
BLOCK = 128
NSA_HEADS = 4
NSA_KV_HEADS = 1
CMP_LEN = 32
CMP_STRIDE = 16
SEL_LEN = 64
N_SEL = 16
NSA_WINDOW = 512
SEL_Q_BLOCK = 64
SWA_HEADS = 4
SWA_KV_HEADS = 2
SWA_WINDOW = 128
DIL_PATTERNS = ((128, 1), (512, 4), (2048, 16))
DIL_HEADS_PER_GROUP = 4
DIL_Q_HEADS = len(DIL_PATTERNS) * DIL_HEADS_PER_GROUP
DIL_KV_HEADS = 4
SB_HEADS = 4
N_BRANCHES = 4
BRANCH_WIDTH = 4 * HEAD_DIM
NUM_BUCKETS = 32
MAX_DISTANCE = 2048
N_BIAS_HEADS = NSA_HEADS + SWA_HEADS + DIL_Q_HEADS
D_FF_DENSE = 2816
N_EXPERTS = 8
TOP_K = 2
D_FF_EXPERT = 3584
N_DENSE_LAYERS = (DEPTH + 1) // 2
N_MOE_LAYERS = DEPTH // 2
ALPHA = (2 * DEPTH) ** 0.25
BETA = (8 * DEPTH) ** -0.25
LN_EPS = 1e-5
NEG_INF = -1e30
TINY = 1e-30
FORCE_SCORE = 1e4

IN_SPLITS = (
    ('nsa_q', NSA_HEADS * HEAD_DIM), ('nsa_k_cmp', NSA_KV_HEADS * HEAD_DIM), ('nsa_v_cmp', NSA_KV_HEADS * HEAD_DIM),
    ('nsa_k_sel', NSA_KV_HEADS * HEAD_DIM), ('nsa_v_sel', NSA_KV_HEADS * HEAD_DIM),
    ('nsa_k_win', NSA_KV_HEADS * HEAD_DIM), ('nsa_v_win', NSA_KV_HEADS * HEAD_DIM), ('nsa_gate', NSA_HEADS * 3),
    ('swa_q', SWA_HEADS * HEAD_DIM), ('swa_k', SWA_KV_HEADS * HEAD_DIM), ('swa_v', SWA_KV_HEADS * HEAD_DIM),
    ('dil_q', DIL_Q_HEADS * HEAD_DIM), ('dil_k', DIL_KV_HEADS * HEAD_DIM), ('dil_v', DIL_KV_HEADS * HEAD_DIM),
    ('sb_q', SB_HEADS * HEAD_DIM), ('sb_k', SB_HEADS * HEAD_DIM), ('sb_v', SB_HEADS * HEAD_DIM),
    ('merge_gate', N_BRANCHES * D_MODEL),
)
D_IN = sum(sz for _, sz in IN_SPLITS)

kernel_name = 'hybrid_nsa_swa_dilated_stickbreak_moe_block'


def split_cols(z):
    out = {}
    off = 0
    for name, size in IN_SPLITS:
        out[name] = z[..., off:off + size]
        off += size
    return out


def layer_norm(x):
    x32 = x.astype(jnp.float32)
    mu = x32.mean(-1, keepdims=True)
    var = jnp.square(x32 - mu).mean(-1, keepdims=True)
    return ((x32 - mu) * lax.rsqrt(var + LN_EPS)).astype(x.dtype)


def t5_bucket(dist):
    dist = jnp.maximum(dist, 0)
    max_exact = NUM_BUCKETS // 2
    d_f = jnp.maximum(dist, 1).astype(jnp.float32)
    large = max_exact + (jnp.log(d_f / max_exact) / math.log(MAX_DISTANCE / max_exact)
                         * (NUM_BUCKETS - max_exact)).astype(jnp.int32)
    large = jnp.minimum(large, NUM_BUCKETS - 1)
    return jnp.where(dist < max_exact, dist, large)


def banded_attention(q, k, v, n_back, bias_tbl, dist_mult, sink=None):
    n, length, h, dh = q.shape
    g = k.shape[2]
    r = h // g
    blk = min(BLOCK, length)
    nb = -(-length // blk)
    lp = nb * blk
    n_prev = -(-n_back // blk)
    span = (n_prev + 1) * blk
    pad_end = lp - length
    qp = jnp.pad(q, ((0, 0), (0, pad_end), (0, 0), (0, 0))).reshape(n, nb, blk, g, r, dh)
    kv_pad = ((0, 0), (n_prev * blk, pad_end), (0, 0), (0, 0))
    kp = jnp.pad(k, kv_pad)
    vp = jnp.pad(v, kv_pad)

    def band(a):
        return jnp.concatenate(
            [a[:, j * blk:(j + nb) * blk].reshape(n, nb, blk, g, dh) for j in range(n_prev + 1)], axis=2)

    kb, vb = band(kp), band(vp)
    dist = np.arange(blk)[:, None] + n_prev * blk - np.arange(span)[None, :]
    local_ok = (dist >= 0) & (dist <= n_back)
    front_ok = (np.arange(nb)[:, None] - n_prev) * blk + np.arange(span)[None, :] >= 0
    mask = local_ok[None] & front_ok[:, None, :]
    bias = bias_tbl[t5_bucket(jnp.asarray(dist * dist_mult))]
    bias = jnp.transpose(bias, (2, 0, 1)).reshape(g, r, blk, span).astype(jnp.float32)
    s = jnp.einsum('nbqgrd,nbkgd->nbgrqk', qp, kb).astype(jnp.float32) / math.sqrt(dh)
    s = jnp.where(mask[None, :, None, None], s + bias, NEG_INF)
    m = s.max(-1)
    if sink is not None:
        sink_f = sink.astype(jnp.float32).reshape(g, r, 1)
        m = jnp.maximum(m, sink_f)
    p = jnp.exp(s - m[..., None])
    den = p.sum(-1)
    if sink is not None:
        den = den + jnp.exp(sink_f - m)
    o = jnp.einsum('nbgrqk,nbkgd->nbqgrd', p.astype(v.dtype), vb)
    o = o / jnp.transpose(den, (0, 1, 4, 2, 3))[..., None].astype(v.dtype)
    o = o.reshape(n, lp, h, dh)[:, :length]
    lse = jnp.transpose(m + jnp.log(den), (0, 1, 4, 2, 3)).reshape(n, lp, h)[:, :length]
    return o, lse


def compress_blocks(a, pe, w1, w2):
    b, s, g, dh = a.shape
    ratio = CMP_LEN // CMP_STRIDE
    n_chunk = s // CMP_STRIDE
    n_cmp = n_chunk - ratio + 1
    chunks = a.reshape(b, n_chunk, CMP_STRIDE, g, dh)
    blocks = jnp.concatenate([chunks[:, j:j + n_cmp] for j in range(ratio)], axis=2)
    blocks = blocks + pe[None, None, :, None, :]
    flat = jnp.transpose(blocks, (0, 1, 3, 2, 4)).reshape(b, n_cmp, g, CMP_LEN * dh)
    return jax.nn.gelu(flat @ w1) @ w2


def nsa_mixer(q, k_cmp, v_cmp, k_sel, v_sel, k_win, v_win, gate_logits, cmp_pe, cmp_w1, cmp_w2, bias_tbl):
    b, s, h, dh = q.shape
    g = k_cmp.shape[2]
    r = h // g
    qg = q.reshape(b, s, g, r, dh)
    pos = jnp.arange(s)
    kc = compress_blocks(k_cmp, cmp_pe[0], cmp_w1[0], cmp_w2[0])
    vc = compress_blocks(v_cmp, cmp_pe[1], cmp_w1[1], cmp_w2[1])
    n_cmp = kc.shape[1]
    cmp_end = jnp.arange(n_cmp) * CMP_STRIDE + CMP_LEN - 1
    cmp_ok = (cmp_end[None, :] <= pos[:, None])[None, :, None, None, :]
    sc = jnp.einsum('btgrd,bigd->btgri', qg, kc).astype(jnp.float32) / math.sqrt(dh)
    sc = jnp.where(cmp_ok, sc, NEG_INF)
    p = jnp.exp(sc - sc.max(-1, keepdims=True)) * cmp_ok
    p = p / jnp.maximum(p.sum(-1, keepdims=True), TINY)
    o_cmp = jnp.einsum('btgri,bigd->btgrd', p.astype(vc.dtype), vc).reshape(b, s, h, dh)
    n_blk = s // SEL_LEN
    n_blk_pad = max(n_blk, N_SEL)
    ci = np.arange(n_cmp)[:, None]
    sj = np.arange(n_blk_pad)[None, :]
    overlap = ((ci * CMP_STRIDE + CMP_LEN - 1 >= sj * SEL_LEN) & (ci * CMP_STRIDE < (sj + 1) * SEL_LEN)
               & (sj < n_blk)).astype(np.float32)
    blk_score = jnp.einsum('btgi,ij->btgj', p.sum(3), overlap)
    cur = pos // SEL_LEN
    jj = jnp.arange(n_blk_pad)
    forced = (jj[None] == 0) | (jj[None] == cur[:, None]) | (jj[None] == cur[:, None] - 1)
    future = jj[None] > cur[:, None]
    blk_score = jnp.where(forced[None, :, None], FORCE_SCORE,
                          jnp.where(future[None, :, None], NEG_INF, blk_score))
    _, sel_idx = lax.top_k(blk_score, N_SEL)
    pad_len = n_blk_pad * SEL_LEN - s

    def to_blocks(a):
        a = jnp.pad(a, ((0, 0), (0, pad_len), (0, 0), (0, 0)))
        return jnp.transpose(a.reshape(b, n_blk_pad, SEL_LEN, g, dh), (0, 3, 1, 2, 4))

    kb, vb = to_blocks(k_sel), to_blocks(v_sel)
    n_chunks = s // SEL_Q_BLOCK
    q_c = jnp.transpose(qg.reshape(b, n_chunks, SEL_Q_BLOCK, g, r, dh), (1, 0, 2, 3, 4, 5))
    idx_c = jnp.transpose(sel_idx.reshape(b, n_chunks, SEL_Q_BLOCK, g, N_SEL), (1, 0, 3, 2, 4))
    pos_c = pos.reshape(n_chunks, SEL_Q_BLOCK)
    gather = jax.vmap(jax.vmap(lambda tbl, ix: tbl[ix]))
    tbl_g = jnp.transpose(bias_tbl.reshape(NUM_BUCKETS, g, r), (1, 0, 2))
    lookup = jax.vmap(lambda tb, bk: tb[bk], in_axes=(0, 1), out_axes=1)

    def sel_chunk(args):
        qq, ix, qpos = args
        kg = gather(kb, ix)
        vg = gather(vb, ix)
        kpos = ix[..., None] * SEL_LEN + jnp.arange(SEL_LEN)
        dist = qpos[None, None, :, None, None] - kpos
        ok = dist >= 0
        bias = jnp.transpose(lookup(tbl_g, t5_bucket(dist)), (0, 1, 2, 5, 3, 4)).astype(jnp.float32)
        sc_s = jnp.einsum('btgrd,bgtnsd->bgtrns', qq, kg).astype(jnp.float32) / math.sqrt(dh) + bias
        sc_s = jnp.where(ok[:, :, :, None], sc_s, NEG_INF)
        tq = qq.shape[1]
        p_s = jax.nn.softmax(sc_s.reshape(b, g, tq, r, N_SEL * SEL_LEN), axis=-1)
        return jnp.einsum('bgtrk,bgtkd->btgrd', p_s.astype(vg.dtype), vg.reshape(b, g, tq, N_SEL * SEL_LEN, dh))

    o_sel = lax.map(sel_chunk, (q_c, idx_c, pos_c))
    o_sel = jnp.transpose(o_sel, (1, 0, 2, 3, 4, 5)).reshape(b, s, h, dh)
    o_win, _ = banded_attention(q, k_win, v_win, NSA_WINDOW - 1, bias_tbl, 1)
    gt = jax.nn.sigmoid(gate_logits.reshape(b, s, h, 3))
    o = gt[..., 0:1] * o_cmp + gt[..., 1:2] * o_sel + gt[..., 2:3] * o_win
    return o.reshape(b, s, h * dh)


def dilated_mixer(q, k, v, bias_tbl):
    b, s, _, dh = q.shape
    hh = DIL_HEADS_PER_GROUP
    outs = []
    lses = []
    for gi, (win, dil) in enumerate(DIL_PATTERNS):
        L = s // dil

        def to_residue(a):
            return jnp.transpose(a.reshape(b, L, dil, a.shape[2], dh), (0, 2, 1, 3, 4)).reshape(b * dil, L, a.shape[2], dh)

        o, lse = banded_attention(to_residue(q[:, :, gi * hh:(gi + 1) * hh]), to_residue(k), to_residue(v),
                                  win // dil, bias_tbl[:, gi * hh:(gi + 1) * hh], dil)
        outs.append(jnp.transpose(o.reshape(b, dil, L, hh, dh), (0, 2, 1, 3, 4)).reshape(b, s, hh, dh))
        lses.append(jnp.transpose(lse.reshape(b, dil, L, hh), (0, 2, 1, 3)).reshape(b, s, hh))
    wts = jax.nn.softmax(jnp.stack(lses, 0), axis=0)
    o = jnp.sum(wts[..., None].astype(q.dtype) * jnp.stack(outs, 0), axis=0)
    return o.reshape(b, s, hh * dh)


def stick_breaking(q, k, v):
    b, s, h, dh = q.shape
    blk = min(BLOCK, s)
    nb = s // blk
    q_c = jnp.transpose(q.reshape(b, nb, blk, h, dh), (1, 0, 2, 3, 4))
    pos_c = jnp.arange(s).reshape(nb, blk)
    kpos = jnp.arange(s)

    def block_fn(args):
        qq, qpos = args
        z = jnp.einsum('bqhd,bkhd->bhqk', qq, k).astype(jnp.float32) / math.sqrt(dh)
        strict = (kpos[None, :] < qpos[:, None])[None, None]
        log_fail = jnp.where(strict, jax.nn.log_sigmoid(-z), 0.0)
        after = lax.cumsum(log_fail, axis=3, reverse=True) - log_fail
        a = jnp.where(strict, jnp.exp(jax.nn.log_sigmoid(z) + after), 0.0)
        return jnp.einsum('bhqk,bkhd->bqhd', a.astype(v.dtype), v)

    o = lax.map(block_fn, (q_c, pos_c))
    return jnp.transpose(o, (1, 0, 2, 3, 4)).reshape(b, s, h * dh)


def mixer_sublayer(h, w_in, w_branch, w_out, cmp_pe, cmp_w1, cmp_w2, sinks, rel_bias):
    b, s, _ = h.shape
    cols = split_cols(h @ w_in)

    def heads(name, n):
        return cols[name].reshape(b, s, n, HEAD_DIM)

    o_a = nsa_mixer(heads('nsa_q', NSA_HEADS), heads('nsa_k_cmp', NSA_KV_HEADS), heads('nsa_v_cmp', NSA_KV_HEADS),
                    heads('nsa_k_sel', NSA_KV_HEADS), heads('nsa_v_sel', NSA_KV_HEADS),
                    heads('nsa_k_win', NSA_KV_HEADS), heads('nsa_v_win', NSA_KV_HEADS), cols['nsa_gate'],
                    cmp_pe, cmp_w1, cmp_w2, rel_bias[:, :NSA_HEADS])
    o_b, _ = banded_attention(heads('swa_q', SWA_HEADS), heads('swa_k', SWA_KV_HEADS), heads('swa_v', SWA_KV_HEADS),
                              SWA_WINDOW - 1, rel_bias[:, NSA_HEADS:NSA_HEADS + SWA_HEADS], 1, sinks)
    o_b = o_b.reshape(b, s, BRANCH_WIDTH)
    o_c = dilated_mixer(heads('dil_q', DIL_Q_HEADS), heads('dil_k', DIL_KV_HEADS), heads('dil_v', DIL_KV_HEADS),
                        rel_bias[:, NSA_HEADS + SWA_HEADS:])
    o_d = stick_breaking(heads('sb_q', SB_HEADS), heads('sb_k', SB_HEADS), heads('sb_v', SB_HEADS))
    gates = jax.nn.sigmoid(cols['merge_gate'].reshape(b, s, N_BRANCHES, D_MODEL))
    merged = (gates[:, :, 0] * (o_a @ w_branch[0]) + gates[:, :, 1] * (o_b @ w_branch[1])
              + gates[:, :, 2] * (o_c @ w_branch[2]) + gates[:, :, 3] * (o_d @ w_branch[3]))
    return merged @ w_out


def swiglu(h, w_gate, w_up, w_down):
    return (jax.nn.silu(h @ w_gate) * (h @ w_up)) @ w_down


def moe_ffn(h, w_router, e_gate, e_up, e_down):
    logits = (h @ w_router).astype(jnp.float32)
    top_val, top_idx = lax.top_k(logits, TOP_K)
    top_w = jax.nn.softmax(top_val, axis=-1)
    gates = jnp.sum(jax.nn.one_hot(top_idx, N_EXPERTS, dtype=jnp.float32) * top_w[..., None], axis=-2)
    y = jnp.zeros_like(h)
    for e in range(N_EXPERTS):
        y = y + gates[..., e:e + 1].astype(h.dtype) * swiglu(h, e_gate[e], e_up[e], e_down[e])
    return y


def setup_inputs(seed: int = 0) -> dict:
    key = jax.random.key(seed)
    ks = jax.random.split(key, 21)

    def nrm(k, shape, scale):
        return jax.random.normal(k, shape, jnp.float32) * scale

    return {
        'x': nrm(ks[0], (BATCH, SEQ, D_MODEL), 1.0),
        'c': nrm(ks[1], (BATCH, D_MODEL), 1.0),
        'rel_bias': nrm(ks[2], (NUM_BUCKETS, N_BIAS_HEADS), 0.5),
        'w_ada': nrm(ks[3], (DEPTH, D_MODEL, 6 * D_MODEL), 0.5 * D_MODEL ** -0.5),
        'b_ada': nrm(ks[4], (DEPTH, 6 * D_MODEL), 0.01),
        'w_in': nrm(ks[5], (DEPTH, D_MODEL, D_IN), D_MODEL ** -0.5),
        'w_branch': nrm(ks[6], (DEPTH, N_BRANCHES, BRANCH_WIDTH, D_MODEL), BETA * BRANCH_WIDTH ** -0.5),
        'w_out': nrm(ks[7], (DEPTH, D_MODEL, D_MODEL), BETA * D_MODEL ** -0.5),
        'cmp_pe': nrm(ks[8], (DEPTH, 2, CMP_LEN, HEAD_DIM), 0.1),
        'cmp_w1': nrm(ks[9], (DEPTH, 2, CMP_LEN * HEAD_DIM, HEAD_DIM), (CMP_LEN * HEAD_DIM) ** -0.5),
        'cmp_w2': nrm(ks[10], (DEPTH, 2, HEAD_DIM, HEAD_DIM), HEAD_DIM ** -0.5),
        'swa_sinks': nrm(ks[11], (DEPTH, SWA_HEADS), 1.0),
        'ln_g': 1.0 + nrm(ks[12], (DEPTH, 2, D_MODEL), 0.02),
        'ln_b': nrm(ks[13], (DEPTH, 2, D_MODEL), 0.02),
        'ffn_w_gate': nrm(ks[14], (N_DENSE_LAYERS, D_MODEL, D_FF_DENSE), D_MODEL ** -0.5),
        'ffn_w_up': nrm(ks[15], (N_DENSE_LAYERS, D_MODEL, D_FF_DENSE), D_MODEL ** -0.5),
        'ffn_w_down': nrm(ks[16], (N_DENSE_LAYERS, D_FF_DENSE, D_MODEL), BETA * D_FF_DENSE ** -0.5),
        'moe_router': nrm(ks[17], (N_MOE_LAYERS, D_MODEL, N_EXPERTS), D_MODEL ** -0.5),
        'moe_w_gate': nrm(ks[18], (N_MOE_LAYERS, N_EXPERTS, D_MODEL, D_FF_EXPERT), D_MODEL ** -0.5),
        'moe_w_up': nrm(ks[19], (N_MOE_LAYERS, N_EXPERTS, D_MODEL, D_FF_EXPERT), D_MODEL ** -0.5),
        'moe_w_down': nrm(ks[20], (N_MOE_LAYERS, N_EXPERTS, D_FF_EXPERT, D_MODEL), BETA * D_FF_EXPERT ** -0.5),
    }


def reference(x, c, rel_bias, w_ada, b_ada, w_in, w_branch, w_out, cmp_pe, cmp_w1, cmp_w2, swa_sinks,
              ln_g, ln_b, ffn_w_gate, ffn_w_up, ffn_w_down, moe_router, moe_w_gate, moe_w_up, moe_w_down):
    for layer in range(DEPTH):
        mod = jax.nn.silu(c) @ w_ada[layer] + b_ada[layer]
        sh1, sc1, g1, sh2, sc2, g2 = jnp.split(mod, 6, axis=-1)
        h = layer_norm(x) * (1.0 + sc1[:, None]) + sh1[:, None]
        y = mixer_sublayer(h, w_in[layer], w_branch[layer], w_out[layer], cmp_pe[layer], cmp_w1[layer],
                           cmp_w2[layer], swa_sinks[layer], rel_bias)
        x = layer_norm(ALPHA * x + g1[:, None] * y) * ln_g[layer, 0] + ln_b[layer, 0]
        h = layer_norm(x) * (1.0 + sc2[:, None]) + sh2[:, None]
        if layer % 2 == 0:
            i = layer // 2
            y = swiglu(h, ffn_w_gate[i], ffn_w_up[i], ffn_w_down[i])
        else:
            i = layer // 2
            y = moe_ffn(h, moe_router[i], moe_w_gate[i], moe_w_up[i], moe_w_down[i])
        x = layer_norm(ALPHA * x + g2[:, None] * y) * ln_g[layer, 1] + ln_b[layer, 1]
    return x
```

```python
import math
from contextlib import ExitStack
import numpy as np
import concourse.bass as bass
import concourse.mybir as mybir
from concourse.ap import AP
from concourse.bass_utils import run_bass_kernel_spmd

F32 = mybir.dt.float32
BF16 = mybir.dt.bfloat16
AF = mybir.ActivationFunctionType
ALU = mybir.AluOpType

D = 1024
SEQ = 2048
NTB = 16
DEPTH = 2
NCORES = 8
NDV = 2304
ALPHA = (2 * DEPTH) ** 0.25
LN_EPS = 1e-5
D_IN = 7308
FF_DENSE = 2816
FF_EXP = 3584
EPOCH = 30000
NDMASEM = 8


class Res:
    __slots__ = ("name", "w", "r")

    def __init__(self, name=""):
        self.name = name
        self.w = None
        self.r = []


class Sched:
    def __init__(self, nc):
        self.nc = nc
        self.h = {"pe": nc.tensor, "act": nc.scalar, "dve": nc.vector, "pool": nc.gpsimd, "sp": nc.sync}
        self.names = list(self.h)
        self._ctx = []
        self.sems = {n: self._newsem(n + "_c0") for n in self.names}
        self.cnt = {n: 0 for n in self.names}
        self.epoch = {n: 0 for n in self.names}
        self.waited = {n: {} for n in self.names}
        self.last = {n: None for n in self.names}
        self.dq = ("sp", "act", "pool")
        self.dsems = {n: [self._newsem(f"{n}_d{i}") for i in range(NDMASEM)] for n in self.dq}
        self.dcnt = {n: 0 for n in self.dq}
        self.dlast = {n: [None] * NDMASEM for n in self.dq}
        self.nins = {}
        self.marks = []

    def _newsem(self, name):
        cm = self.nc.semaphore(name)
        s = cm.__enter__()
        self._ctx.append(cm)
        self._keep = getattr(self, "_keep", [])
        self._keep.append(s)
        return s

    def _deps(self, reads, writes):
        deps = []
        for r in reads:
            if r.w is not None:
                deps.append(r.w)
        for w in writes:
            if w.w is not None:
                deps.append(w.w)
            deps.extend(w.r)
        return deps

    def _commit(self, reads, writes, tok):
        for r in reads:
            r.r.append(tok)
            if len(r.r) > 64:
                best = {}
                for (s, v) in r.r:
                    if id(s) not in best or best[id(s)][1] < v:
                        best[id(s)] = (s, v)
                r.r = list(best.values())
        for w in writes:
            w.w = tok
            w.r = []

    def _emit(self, eng, deps, fn, inc):
        wd = self.waited[eng]
        best = {}
        for (s, v) in deps:
            k = id(s)
            if wd.get(k, 0) >= v:
                continue
            if k not in best or best[k][1] < v:
                best[k] = (s, v)
        e = self.h[eng]
        for k, (s, v) in best.items():
            wd[k] = v
            e.wait_ge(s, v)
        if fn is not None:
            ins = fn(e)
            self.nins[eng] = self.nins.get(eng, 0) + 1
            if inc is not None:
                ins.then_inc(inc[0], inc[1])

    def mark(self, name):
        self.marks.append((name, dict(self.nins)))

    def _tick(self, eng):
        if self.cnt[eng] >= EPOCH:
            self.epoch[eng] += 1
            self.sems[eng] = self._newsem(f"{eng}_c{self.epoch[eng]}")
            self.cnt[eng] = 0
        self.cnt[eng] += 1
        return (self.sems[eng], self.cnt[eng])

    def op(self, eng, fn, reads=(), writes=()):
        reads = [r for r in reads if r is not None]
        writes = [w for w in writes if w is not None]
        deps = self._deps(reads, writes)
        tok = self._tick(eng)
        self._emit(eng, deps, fn, (tok[0], 1))
        self._commit(reads, writes, tok)
        self.last[eng] = tok
        return tok

    def pe(self, fns, reads=(), writes=()):
        reads = [r for r in reads if r is not None]
        writes = [w for w in writes if w is not None]
        deps = self._deps(reads, writes)
        tok = self._tick("pe")
        n = len(fns)
        for i, fn in enumerate(fns):
            self._emit("pe", deps if i == 0 else [], fn, (tok[0], 1) if i == n - 1 else None)
        self._commit(reads, writes, tok)
        self.last["pe"] = tok
        return tok

    def dma(self, q, fn, reads=(), writes=()):
        reads = [r for r in reads if r is not None]
        writes = [w for w in writes if w is not None]
        deps = self._deps(reads, writes)
        m = self.dcnt[q]
        self.dcnt[q] += 1
        nsl = 3 if q == "sp" else NDMASEM
        slot = m % nsl
        sem = self.dsems[q][slot]
        val = 16 * (m // nsl + 1)
        if self.dlast[q][slot] is not None:
            deps.append(self.dlast[q][slot])
        tok = (sem, val)
        self.dlast[q][slot] = tok
        self._emit(q, deps, fn, (sem, 16))
        self._commit(reads, writes, tok)
        return tok

    def barrier(self):
        toks = [t for t in self.last.values() if t is not None]
        for q in self.dq:
            toks += [t for t in self.dlast[q] if t is not None]
        for n in self.names:
            self._emit(n, toks, None, None)

    def close(self):
        for cm in reversed(self._ctx):
            cm.__exit__(None, None, None)


def MM(out, lhsT, rhs, start=True, stop=True):
    return lambda e: e.matmul(out, lhsT=lhsT, rhs=rhs, start=start, stop=stop)


def TR(out, in_, ident):
    return lambda e: e.transpose(out, in_, ident)


def ACT(out, in_, func, bias=None, scale=None):
    kw = {}
    if bias is not None:
        kw["bias"] = bias
    if scale is not None:
        kw["scale"] = scale
    return lambda e: e.activation(out=out, in_=in_, func=func, **kw)


def TT(out, a, b, op):
    return lambda e: e.tensor_tensor(out=out, in0=a, in1=b, op=op)


def TS(out, a, s1, op0, s2=None, op1=None):
    if op1 is None:
        return lambda e: e.tensor_scalar(out=out, in0=a, scalar1=s1, scalar2=None, op0=op0)
    return lambda e: e.tensor_scalar(out=out, in0=a, scalar1=s1, scalar2=s2, op0=op0, op1=op1)


def STT(out, in0, scalar, in1, op0, op1):
    return lambda e: e.scalar_tensor_tensor(out=out, in0=in0, scalar=scalar, in1=in1, op0=op0, op1=op1)


def CP(out, in_):
    return lambda e: e.tensor_copy(out=out, in_=in_)


def DMA(out, in_):
    return lambda e: e.dma_start(out=out, in_=in_)


def MSET(ap, v):
    return lambda e: e.memset(ap, v)


def bc_last(ap2, m):
    a = ap2.ap
    return AP(ap2.tensor, ap2.offset, [list(a[0]), list(a[1]), [0, m]])


def bc_mid(ap2, m):
    a = ap2.ap
    return AP(ap2.tensor, ap2.offset, [list(a[0]), [0, m], list(a[1])])


def _t5_bucket(dist):
    dist = np.maximum(dist, 0)
    d_f = np.maximum(dist, 1).astype(np.float32)
    large = 16 + (np.log(d_f / np.float32(16)) / np.float32(math.log(2048 / 16)) * np.float32(16)).astype(np.int32)
    large = np.minimum(large, 31)
    return np.where(dist < 16, dist, large)


def host_consts():
    c = {}
    u = np.arange(NDV)
    d = u - 128
    onehot = np.zeros((32, NDV), np.float32)
    bk = _t5_bucket(d)
    onehot[bk[d >= 0], u[d >= 0]] = 1.0
    c["k_onehot"] = onehot
    vm = np.zeros((6, NDV), np.float32)
    vm[0] = (d >= 0) & (d <= 511)
    vm[1] = (d >= 0) & (d <= 2047)
    vm[2] = (d >= 0) & (d <= 127)
    vm[3] = (d >= 0) & (d <= 128)
    vm[4] = (d >= 0) & (d <= 512) & (d % 4 == 0)
    vm[5] = (d >= 0) & (d <= 2047) & (d % 16 == 0)
    c["k_vmask"] = np.ascontiguousarray(np.broadcast_to(vm[:, None, :], (6, 4, NDV))).astype(np.float32)
    c["k_ident"] = np.eye(128, dtype=np.float32)
    j = np.arange(128)[:, None]
    s = np.arange(128)[None, :]
    c["k_negU"] = -(j > s).astype(np.float32)
    c["k_sbmask"] = (j < s).astype(np.float32)
    ci = np.arange(128)[:, None]
    sj = np.arange(32)[None, :]
    ov = ((ci * 16 + 31 >= sj * 64) & (ci * 16 < (sj + 1) * 64) & (ci < 127)).astype(np.float32)
    c["k_overlap"] = ov
    t = np.arange(SEQ)[None, :]
    c["k_cmpok"] = ((ci * 16 + 31 <= t) & (ci < 127)).astype(np.float32)
    keep = np.zeros((128, 8, 32), np.float32)
    add = np.zeros((128, 8, 32), np.float32)
    for qb in range(8, 16):
        tt = qb * 128 + np.arange(128)
        cur = tt // 64
        for jj in range(32):
            fut = jj > cur
            f0 = np.full(128, jj == 0)
            f1 = jj == cur
            f2 = jj == cur - 1
            k = ~(fut | f0 | f1 | f2)
            a = np.where(f0, 3e4, np.where(f1, 2e4, np.where(f2, 1e4, np.where(fut, -1e30, 0.0))))
            keep[:, qb - 8, jj] = k
            add[:, qb - 8, jj] = a
    c["k_selkeep"] = keep
    c["k_seladd"] = add
    pen = np.zeros((32, 16, 128), np.float32)
    for J in range(16):
        for k in range(128):
            pen[2 * J + k // 64, J, k] = -30000.0
    c["k_pen"] = pen
    return c


def build_nc(NSEQ=2, depth=DEPTH, dbg=False):
    nc = bass.Bass("TRN2", target_bir_lowering=False)
    S = Sched(nc)
    ES = ExitStack()

    def din(name, shape, dt=F32):
        return nc.dram_tensor(name, list(shape), dt, kind="ExternalInput").ap()

    x = din("x", [NSEQ, SEQ, D])
    cT = din("cT", [NSEQ, 128, 8])
    rel_bias = din("rel_bias", [32, 20])
    w_ada = din("w_ada", [2, D, 6 * D])
    b_ada = din("b_ada", [2, 6 * D])
    b_adaT = din("b_adaT", [2, 128, 48])
    w_in = din("w_in", [2, D, D_IN])
    w_branch = din("w_branch", [2, 4, 256, D])
    w_out = din("w_out", [2, D, D])
    cmp_peT = din("cmp_peT", [2, 2, 64, 32])
    cmp_w1 = din("cmp_w1", [2, 2, 2048, 64])
    cmp_w2 = din("cmp_w2", [2, 2, 64, 64])
    swa_sinks = din("swa_sinks", [2, 4])
    ln_g = din("ln_g", [2, 2, D])
    ln_b = din("ln_b", [2, 2, D])
    ffn_w_gate = din("ffn_w_gate", [1, D, FF_DENSE])
    ffn_w_up = din("ffn_w_up", [1, D, FF_DENSE])
    ffn_w_down = din("ffn_w_down", [1, FF_DENSE, D])
    moe_router = din("moe_router", [1, D, 8])
    moe_w_gate = din("moe_w_gate", [1, 8, D, FF_EXP])
    moe_w_up = din("moe_w_up", [1, 8, D, FF_EXP])
    moe_w_down = din("moe_w_down", [1, 8, FF_EXP, D])
    k_onehot = din("k_onehot", [32, NDV])
    k_vmask = din("k_vmask", [6, 4, NDV])
    k_ident = din("k_ident", [128, 128])
    k_negU = din("k_negU", [128, 128])
    k_sbmask = din("k_sbmask", [128, 128])
    k_overlap = din("k_overlap", [128, 32])
    k_cmpok = din("k_cmpok", [128, SEQ])
    k_selkeep = din("k_selkeep", [128, 8, 32])
    k_seladd = din("k_seladd", [128, 8, 32])
    k_pen = din("k_pen", [32, 16, 128])
    out = nc.dram_tensor("out", [NSEQ, SEQ, D], F32, kind="ExternalOutput").ap()
    if dbg:
        dbg_hT = nc.dram_tensor("dbg_hT", [128, 8 * SEQ], BF16, kind="ExternalOutput").ap()
        dbg_oT = nc.dram_tensor("dbg_oT", [128, 4 * 2 * SEQ], BF16, kind="ExternalOutput").ap()
        dbg_mod = nc.dram_tensor("dbg_mod", [128, 96], F32, kind="ExternalOutput").ap()
        dbg_gB = nc.dram_tensor("dbg_gB", [128, 4 * D], BF16, kind="ExternalOutput").ap()
        dbg_xm = nc.dram_tensor("dbg_xm", [SEQ, D], F32, kind="ExternalOutput").ap()
    xs = nc.dram_tensor("xs", [NSEQ, SEQ, D], F32, kind="Internal").ap()
    xm = nc.dram_tensor("xm", [SEQ, D], F32, kind="Internal").ap()
    ebd_t = nc.dram_tensor("ebd", [24, NDV], BF16, kind="Internal")
    ebd = ebd_t.ap()
    WS = NDV + 128
    ebs_t = nc.dram_tensor("ebs", [24, 128, WS], BF16, kind="Internal")

    uniq = {"n": 0}

    def sb(name, shape, dt, stack=ES):
        uniq["n"] += 1
        return stack.enter_context(nc.sbuf_tensor(f"{name}_{uniq['n']}", list(shape), dt))

    def ps(name, shape, dt):
        return ES.enter_context(nc.psum_tensor(name, list(shape), dt))

    ident = sb("ident", [128, 128], BF16)
    negU = sb("negU", [128, 128], BF16)
    negOnes = sb("negOnes", [128, 128], BF16)
    onesb = sb("onesb", [128, 128], BF16)
    sbmask = sb("sbmask", [128, 128], BF16)
    overlap = sb("overlap", [128, 32], BF16)
    cmpok = sb("cmpok", [128, SEQ], BF16)
    selkeep = sb("selkeep", [128, 8, 32], F32)
    seladd = sb("seladd", [128, 8, 32], F32)
    pen = sb("pen", [32, 16, 128], BF16)
    hT = sb("hT", [128, 8, SEQ], BF16)
    modT = sb("modT", [128, 2, 48], F32)
    gB = sb("gB", [128, 2, 2, D], BF16)
    lngb_h = [None]
    xt = [None, None, None, None]
    xn = [None, None]

    def alloc_io(st, want_xt=True, want_xn=True, want_lngb=False, n_xt=4):
        if want_xt:
            for i_ in range(n_xt):
                xt[i_] = sb(f"xt{i_}", [128, D], F32, st)
        if want_xn:
            xn[0] = sb("xn0", [128, D], BF16, st)
            xn[1] = sb("xn1", [128, D], BF16, st)
        if want_lngb:
            lngb_h[0] = sb("lngb", [128, 2, D], F32, st)
    st6 = sb("st6", [128, 2, 6], F32)
    mvk = sb("mvk", [128, NTB, 2], F32)
    rsk = sb("rsk", [128, NTB], F32)
    mv = sb("mv", [128, 2], F32)
    rstd = sb("rstd", [128, 1], F32)
    Et = [sb(f"E{i}", [128, 512], BF16) for i in range(8)]
    Ft = [sb(f"F{i}", [128, 512], F32) for i in range(6)]
    sm = sb("sm", [128, 64], F32)
    regB = sb("regB", [128, 4 * 2 * SEQ], BF16)
    pg = [ps(f"pg{i}", [128, 512], F32) for i in range(8)]

    R = lambda n: Res(n)
    r_const = R("const")
    r_hT = R("hT")
    r_modT = R("modT")
    r_gB = R("gB")
    r_lngb = R("lngb")
    r_xt = [R(f"xt{i}") for i in range(4)]
    r_xn = [R("xn0"), R("xn1")]
    r_st = R("st")
    r_mvk = R("mvk")
    r_E = [R(f"E{i}") for i in range(8)]
    r_F = [R(f"F{i}") for i in range(6)]
    r_sm = R("sm")
    r_pg = [R(f"pg{i}") for i in range(8)]
    r_regB = R("regB")
    r_ebd = R("ebd")
    r_xs = R("xs")
    r_xm = R("xm")
    r_out = R("out")
    ctr = {"pg": 0, "E": 0, "F": 0, "pT": 0, "xt": 0, "acc": 0}

    pools = {"ns": 6}

    def set_pools(nscore):
        pools["ns"] = nscore

    def nb():
        i = ctr["pg"] % pools["ns"]
        ctr["pg"] += 1
        return pg[i], r_pg[i]

    r_accreg = [R(f"accreg{i}") for i in range(7)]
    r_Rreg = [R(f"Rreg{i}") for i in range(4)]

    def nacc():
        na = 8 - pools["ns"]
        i = pools["ns"] + ctr["acc"] % na
        ctr["acc"] += 1
        return pg[i], r_pg[i]

    def interleave(factories, width):
        it = iter(factories)
        active = []
        free = list(range(width))
        while True:
            while free:
                f = next(it, None)
                if f is None:
                    break
                slot = free.pop(0)
                active.append((f(slot), slot))
            if not active:
                break
            for (g, slot) in list(active):
                try:
                    next(g)
                except StopIteration:
                    active.remove((g, slot))
                    free.append(slot)

    def acc_of(slot):
        i = pools["ns"] + slot
        assert i < 8
        return pg[i], r_pg[i]

    def nE():
        i = ctr["E"] % 8
        ctr["E"] += 1
        return Et[i], r_E[i]

    def nF():
        i = ctr["F"] % 6
        ctr["F"] += 1
        return Ft[i], r_F[i]

    def nT():
        bk, rbk = nb()
        return bk[:].bitcast(BF16), rbk

    for dst, src in ((ident, k_ident), (negU, k_negU), (sbmask, k_sbmask), (overlap, k_overlap), (cmpok, k_cmpok), (pen, k_pen)):
        S.dma("pool", DMA(dst[:], src), writes=[r_const])
    S.dma("sp", DMA(selkeep[:], k_selkeep), writes=[r_const])
    S.dma("sp", DMA(seladd[:], k_seladd), writes=[r_const])
    S.op("dve", MSET(negOnes[:], -1.0), writes=[r_const])
    S.op("dve", MSET(onesb[:], 1.0), writes=[r_const])

    with ExitStack() as st:
        oh = sb("oh", [32, NDV], BF16, st)
        rb = sb("rb", [32, 20], F32, st)
        rbh = sb("rbh", [32, 20], BF16, st)
        rbh32 = sb("rbh32", [32, 20], F32, st)
        rbl = sb("rbl", [32, 20], BF16, st)
        vmk = sb("vmk", [4, NDV], F32, st)
        ebf = sb("ebf", [4, NDV], F32, st)
        ebb = sb("ebb", [4, NDV], BF16, st)
        r_p = R("prol")
        S.dma("pool", DMA(oh[:], k_onehot), writes=[r_p])
        S.dma("sp", DMA(rb[:], rel_bias), writes=[r_p])
        S.op("dve", CP(rbh[:], rb[:]), reads=[r_p], writes=[r_p])
        S.op("dve", CP(rbh32[:], rbh[:]), reads=[r_p], writes=[r_p])
        S.op("dve", TT(rbh32[:], rb[:], rbh32[:], ALU.subtract), reads=[r_p], writes=[r_p])
        S.op("dve", CP(rbl[:], rbh32[:]), reads=[r_p], writes=[r_p])
        vheads = [0, 0, 4, 8, 12, 16]
        for v in range(6):
            h0 = vheads[v]
            for cch in range(6):
                bk, rbk = nb()
                cs = slice(cch * 384, (cch + 1) * 384)
                S.pe([MM(bk[0:4, 0:384], rbh[:, h0:h0 + 4], oh[:, cs], True, False),
                      MM(bk[0:4, 0:384], rbl[:, h0:h0 + 4], oh[:, cs], False, True)], reads=[r_p], writes=[rbk])
                S.op("act", ACT(ebf[:, cs], bk[0:4, 0:384], AF.Exp), reads=[rbk], writes=[r_p])
            S.dma("sp", DMA(vmk[:], k_vmask[v]), writes=[r_p])
            S.op("dve", TT(ebb[:], ebf[:], vmk[:], ALU.mult), reads=[r_p], writes=[r_p])
            S.dma("sp", DMA(ebd[v * 4:(v + 1) * 4, :], ebb[:]), reads=[r_p], writes=[r_ebd])
        skw = sb("skw", [128, NDV], BF16, st)
        r_skw = R("skw")
        for r_ in range(24):
            S.dma("sp", DMA(skw[:], ebd[r_:r_ + 1, :].partition_broadcast(128)), reads=[r_ebd], writes=[r_skw])
            S.dma("sp", DMA(AP(ebs_t, r_ * 128 * WS, [[WS + 1, 128], [1, NDV]]), skw[:]), reads=[r_skw], writes=[r_ebd])
        S.barrier()

    def toeplitz(variant, hh, ndl):
        off = (variant * 4 + hh) * 128 * WS + 128
        return AP(ebs_t, off, [[WS, 128], [128, ndl], [1, 128]])

    def ln_stats_group(srcs):
        ks = [k for (_, _, k) in srcs]
        for (src_ap, r_src, k) in srcs:
            S.op("dve", lambda e, src_ap=src_ap: e.bn_stats(out=st6[:, 0, :], in_=src_ap[:, 0:512]), reads=[r_src], writes=[r_st])
            S.op("dve", lambda e, src_ap=src_ap: e.bn_stats(out=st6[:, 1, :], in_=src_ap[:, 512:1024]), reads=[r_src], writes=[r_st])
            S.op("dve", lambda e, k=k: e.bn_aggr(out=mvk[:, k, :], in_=st6[:]), reads=[r_st], writes=[r_st, r_mvk])
        k0, k1 = min(ks), max(ks) + 1
        S.op("dve", TS(rsk[:, k0:k1], mvk[:, k0:k1, 1], LN_EPS, ALU.add), reads=[r_mvk], writes=[r_mvk])
        S.op("act", ACT(rsk[:, k0:k1], rsk[:, k0:k1], AF.Sqrt), reads=[r_mvk], writes=[r_mvk])
        S.op("dve", lambda e: e.reciprocal(out=rsk[:, k0:k1], in_=rsk[:, k0:k1]), reads=[r_mvk], writes=[r_mvk])

    def to_hT(src_ap, r_src, k, tb, l, scb, shb):
        i = tb % 2
        S.op("dve", TS(xn[i][:], src_ap, mvk[:, k, 0:1], ALU.subtract, rsk[:, k:k + 1], ALU.mult), reads=[r_src, r_mvk], writes=[r_xn[i]])
        tp, rtp = nT()
        S.pe([TR(tp[:, kc * 128:(kc + 1) * 128], xn[i][:, kc * 128:(kc + 1) * 128], ident[:]) for kc in range(8)],
             reads=[r_xn[i], r_const], writes=[rtp])
        for kc in range(8):
            S.op("act", ACT(hT[:, kc, tb * 128:(tb + 1) * 128], tp[:, kc * 128:(kc + 1) * 128], AF.Identity,
                            bias=modT[:, l, shb + kc:shb + kc + 1], scale=modT[:, l, scb + kc:scb + kc + 1]),
                 reads=[rtp, r_modT], writes=[r_hT])

    def affine_k(buf_ap, r_buf, k):
        lngb = lngb_h[0]
        S.op("dve", STT(buf_ap, buf_ap, mvk[:, k, 0:1], lngb[:, 0, :], ALU.subtract, ALU.mult), reads=[r_mvk, r_lngb], writes=[r_buf])
        S.op("dve", STT(buf_ap, buf_ap, rsk[:, k:k + 1], lngb[:, 1, :], ALU.mult, ALU.add), reads=[r_mvk, r_lngb], writes=[r_buf])

    def load_lngb(l, sub):
        lngb = lngb_h[0]
        S.dma("sp", DMA(lngb[:, 0, :], ln_g[l, sub:sub + 1, :].partition_broadcast(128)), writes=[r_lngb])
        S.dma("sp", DMA(lngb[:, 1, :], ln_b[l, sub:sub + 1, :].partition_broadcast(128)), writes=[r_lngb])

    evac_rr = {"i": 0}

    def evac(out_ap, in_ap, reads, writes, scale=None):
        evac_rr["i"] += 1
        if evac_rr["i"] % 2:
            S.op("act", ACT(out_ap, in_ap, AF.Identity, scale=scale), reads=reads, writes=writes)
        elif scale is None:
            S.op("dve", CP(out_ap, in_ap), reads=reads, writes=writes)
        else:
            S.op("dve", TS(out_ap, in_ap, float(scale), ALU.mult), reads=reads, writes=writes)

    for b in range(NSEQ):
        with ExitStack() as st:
            c_sb = sb("c_sb", [128, 8], F32, st)
            scf = sb("scf", [128, 8], F32, st)
            scT = sb("scT", [128, 8], BF16, st)
            scB = sb("scB", [128, 8, 128], BF16, st)
            baT = sb("baT", [128, 2, 48], F32, st)
            bB = sb("bB", [128, 2, 2, D], F32, st)
            wa = [sb(f"wa{i}", [128, 8, 512], BF16, st) for i in range(2)]
            r_m = R("m")
            r_wa = [R("wa0"), R("wa1")]
            S.dma("sp", DMA(c_sb[:], cT[b]), writes=[r_m])
            S.dma("sp", DMA(baT[:], b_adaT.rearrange("l p j -> p l j")), writes=[r_m])
            for l in range(2):
                for gi, c0 in enumerate((2048, 5120)):
                    S.dma("sp", DMA(bB[:, l, gi, :], b_ada[l:l + 1, c0:c0 + D].partition_broadcast(128)), writes=[r_m])
            S.op("act", ACT(scf[:], c_sb[:], AF.Silu), reads=[r_m], writes=[r_m])
            S.op("dve", CP(scT[:], scf[:]), reads=[r_m], writes=[r_m])
            S.op("dve", CP(scB[:], bc_last(scT[:], 128)), reads=[r_m], writes=[r_m])
            for l in range(depth):
                wav = w_ada[l].rearrange("(kc p) n -> p kc n", p=128)
                for ch in range(12):
                    i = ch % 2
                    S.dma("pool", DMA(wa[i][:], wav[:, :, ch * 512:(ch + 1) * 512]), writes=[r_wa[i]])
                    bk, rbk = nb()
                    fns = []
                    for jj in range(4):
                        for kc in range(8):
                            fns.append(MM(bk[:, jj:jj + 1], wa[i][:, kc, jj * 128:(jj + 1) * 128], scT[:, kc:kc + 1], kc == 0, kc == 7))
                    S.pe(fns, reads=[r_wa[i], r_m], writes=[rbk])
                    S.op("dve", TT(modT[:, l, ch * 4:(ch + 1) * 4], bk[:, 0:4], baT[:, l, ch * 4:(ch + 1) * 4], ALU.add),
                         reads=[rbk, r_m], writes=[r_modT])
                    if ch in (4, 5, 10, 11):
                        gi = 0 if ch < 6 else 1
                        half = ch % 2
                        bk2, rbk2 = nb()
                        S.pe([MM(bk2[:], scB[:, kc, :], wa[i][:, kc, :], kc == 0, kc == 7) for kc in range(8)],
                             reads=[r_wa[i], r_m], writes=[rbk2])
                        S.op("dve", TT(gB[:, l, gi, half * 512:(half + 1) * 512], bk2[:], bB[:, l, gi, half * 512:(half + 1) * 512], ALU.add),
                             reads=[rbk2, r_m], writes=[r_gB])
                S.op("dve", TS(modT[:, l, 8:16], modT[:, l, 8:16], 1.0, ALU.add), reads=[r_modT], writes=[r_modT])
                S.op("dve", TS(modT[:, l, 32:40], modT[:, l, 32:40], 1.0, ALU.add), reads=[r_modT], writes=[r_modT])
            S.barrier()

        for l in range(depth):
            S.mark(f"b{b}l{l} LN1")
            x_src = x[b] if l == 0 else xs[b]
            r_xsrc = None if l == 0 else r_xs
            w_in_v = w_in[l].rearrange("(kc p) n -> p kc n", p=128)

            with ExitStack() as st:
                alloc_io(st)
                for g4 in range(4):
                    for i in range(4):
                        tb = g4 * 4 + i
                        S.dma("sp", DMA(xt[i][:], x_src[tb * 128:(tb + 1) * 128, :]), reads=[r_xsrc], writes=[r_xt[i]])
                    ln_stats_group([(xt[i][:], r_xt[i], g4 * 4 + i) for i in range(4)])
                    for i in range(4):
                        tb = g4 * 4 + i
                        to_hT(xt[i][:], r_xt[i], tb, tb, l, 8, 0)
                S.barrier()

            oT = regB[:].rearrange("p (b c t) -> p b c t", b=4, c=2)
            r_oT = r_regB

            with ExitStack() as st:
                fmw = sb("fmw", [128, 8, SEQ], BF16, st)
                wmix = sb("wmix", [128, 8, 1024], BF16, st)
                vtm = sb("vtm", [128, NTB, 4, 65], BF16, st)
                ebt = [sb(f"ebt{i}", [128, 23, 128], BF16, st) for i in range(2)]
                o_tok = sb("o_tok", [128, NTB, 256], BF16, st)
                gts = sb("gts", [128, NTB, 12], F32, st)
                sinkE = sb("sinkE", [128, 4], F32, st)
                kcT = sb("kcT", [128, 128], BF16, st)
                vcA = sb("vcA", [128, 65], BF16, st)
                w1t = sb("w1t", [64, 32, 64], BF16, st)
                peT = sb("peT", [64, 32], BF16, st)
                w2d = sb("w2d", [64, 128], BF16, st)
                cvec = sb("cvec", [64, 1], F32, st)
                gu = sb("gu", [64, 4, 128], F32, st)
                gTt = sb("gTt", [64, 128], BF16, st)
                psel = sb("psel", [128, 8, 128], F32, st)
                pselb = sb("pselb", [128, 8, 128], BF16, st)
                scs = sb("scs", [128, 3, 32], F32, st)
                m8 = sb("m8", [128, 2, 8], F32, st)
                unsel = sb("unsel", [128, 32], BF16, st)
                unselT = sb("unselT", [32, SEQ // 2], BF16, st)
                Rsb = sb("Rsb", [128, 128], BF16, st)
                r_fm = [R(f"fm{i}") for i in range(8)]
                r_wmix = R("wmix")
                r_vtm = R("vtm")
                r_ebt = [R("ebt0"), R("ebt1")]
                r_otok = R("otok")
                r_oacc = R("oacc")
                r_gts = R("gts")
                r_cmp = R("cmp")
                r_sel = R("sel")
                r_Rsb = R("Rsb")
                ebc = {"i": 0}

                S.op("pool", MSET(vtm[:], 1.0), writes=[r_vtm])
                S.mark(f"b{b}l{l} NSA-proj")

                def load_w(segs):
                    for (dc_, sc_, n_) in segs:
                        S.dma("pool", DMA(wmix[:, :, dc_:dc_ + n_], w_in_v[:, :, sc_:sc_ + n_]), writes=[r_wmix])

                def proj_fm(ti, c0, m, scale=None):
                    for T in range(4):
                        bk, rbk = nb()
                        S.pe([MM(bk[0:m, :], wmix[:, kc, c0:c0 + m], hT[:, kc, T * 512:(T + 1) * 512], kc == 0, kc == 7) for kc in range(8)],
                             reads=[r_wmix, r_hT], writes=[rbk])
                        evac(fmw[0:m, ti, T * 512:(T + 1) * 512], bk[0:m, :], [rbk], [r_fm[ti]], scale=scale)

                def proj_tm(c0, n, sinks):
                    for tb in range(NTB):
                        bk, rbk = nb()
                        S.pe([MM(bk[:, 0:n], hT[:, kc, tb * 128:(tb + 1) * 128], wmix[:, kc, c0:c0 + n], kc == 0, kc == 7) for kc in range(8)],
                             reads=[r_wmix, r_hT], writes=[rbk])
                        for fn in sinks:
                            fn(tb, bk, rbk)

                def v_sink(col, slot):
                    def f(tb, bk, rbk):
                        evac(vtm[:, tb, slot, 0:64], bk[:, col:col + 64], [rbk], [r_vtm])
                    return f

                def load_ebt(specs):
                    i = ebc["i"] % 2
                    ebc["i"] += 1
                    for (v_, hh_, ndl_, s0_) in specs:
                        S.dma("sp", DMA(ebt[i][:, s0_:s0_ + ndl_, :], toeplitz(v_, hh_, ndl_)), reads=[r_ebd], writes=[r_ebt[i]])
                    return ebt[i], r_ebt[i]

                def chunk(qap, r_q, ktile, ti_k, pb, j0, n, eb_ap, r_eb, v_of, acc, racc, first, last, penq=None):
                    bk, rbk = nb()
                    fns = []
                    for i in range(n):
                        j = j0 - i
                        sl = slice(i * 128, (i + 1) * 128)
                        kap = ktile[pb:pb + 64, j * 128:(j + 1) * 128]
                        if penq is not None:
                            fns.append(MM(bk[:, sl], kap, qap, True, False))
                            fns.append(MM(bk[:, sl], pen[0:32, j, :], penq, False, True))
                        else:
                            fns.append(MM(bk[:, sl], kap, qap, True, True))
                    rd = [r_fm[ti_k], r_q] + ([r_sel, r_const] if penq is not None else [])
                    S.pe(fns, reads=rd, writes=[rbk])
                    yield
                    E, rE = nE()
                    S.op("act", ACT(E[:, 0:n * 128], bk[:, 0:n * 128], AF.Exp), reads=[rbk], writes=[rE])
                    yield
                    S.op("dve", TT(E[:, 0:n * 128], E[:, 0:n * 128], eb_ap, ALU.mult), reads=[r_eb], writes=[rE])
                    yield
                    S.pe([MM(acc, E[:, i * 128:(i + 1) * 128], v_of(j0 - i), first and i == 0, last and i == n - 1) for i in range(n)],
                         reads=[rE, r_vtm, r_cmp], writes=[racc])
                    yield

                r_q_cur = [None]

                def flat(ap3):
                    return ap3.rearrange("p a b -> p (a b)")

                def to_oT(br):
                    for qb in range(NTB):
                        tp, rtp = nT()
                        S.pe([TR(tp[:, c * 128:(c + 1) * 128], o_tok[:, qb, c * 128:(c + 1) * 128], ident[:]) for c in range(2)],
                             reads=[r_otok, r_const], writes=[rtp])
                        evac(oT[:, br, :, qb * 128:(qb + 1) * 128], tp[:, 0:256].rearrange("p (c t) -> p c t", c=2), [rtp], [r_oT])

                WQ = 0
                load_w([(0, 0, 256), (256, 256, 64), (320, 256, 64), (384, 384, 64), (448, 384, 64), (512, 512, 64), (576, 512, 64),
                        (640, 320, 64), (704, 448, 64), (768, 576, 64), (832, 640, 12)])
                proj_fm(0, 0, 128, 0.125)
                proj_fm(1, 128, 128, 0.125)
                proj_fm(2, 256, 128)
                proj_fm(3, 384, 128)
                proj_fm(4, 512, 128)
                proj_fm(5, 640, 64)

                def gate_sink(tb, bk, rbk):
                    S.op("act", ACT(gts[:, tb, :], bk[:, 128:140], AF.Sigmoid), reads=[rbk], writes=[r_gts])
                proj_tm(704, 140, [v_sink(0, 0), v_sink(64, 1), gate_sink])

                S.mark(f"b{b}l{l} NSA-compress")
                S.op("dve", MSET(kcT[:], 0.0), writes=[r_cmp])
                S.op("dve", MSET(vcA[:], 0.0), writes=[r_cmp])
                S.op("dve", MSET(vcA[:, 64:65], 1.0), writes=[r_cmp])
                for which in range(2):
                    src_t = 2 if which == 0 else 5
                    S.dma("pool", DMA(w1t[:], cmp_w1[l, which].rearrange("(pos d) o -> d pos o", d=64)), writes=[r_cmp])
                    S.dma("pool", DMA(peT[:], cmp_peT[l, which]), writes=[r_cmp])
                    S.dma("pool", DMA(w2d[:, 0:64], cmp_w2[l, which]), writes=[r_cmp])
                    S.dma("pool", DMA(w2d[:, 64:128], cmp_w2[l, which]), writes=[r_cmp])
                    bk, rbk = nb()
                    S.pe([MM(bk[0:64, 0:1], w1t[:, pos, :], peT[:, pos:pos + 1], pos == 0, pos == 31) for pos in range(32)],
                         reads=[r_cmp], writes=[rbk])
                    S.op("dve", CP(cvec[:], bk[0:64, 0:1]), reads=[rbk], writes=[r_cmp])
                    bk, rbk = nb()
                    fns = []
                    for pos in range(32):
                        base = fmw[0:64, src_t, pos:pos + 1]
                        src = AP(base.tensor, base.offset, [list(base.ap[0]), [16, 127]])
                        fns.append(MM(bk[0:64, 0:127], w1t[:, pos, :], src, pos == 0, pos == 31))
                    S.pe(fns, reads=[r_cmp, r_fm[src_t]], writes=[rbk])
                    u = gu[:, 0, 0:127]
                    t1 = gu[:, 1, 0:127]
                    t2 = gu[:, 2, 0:127]
                    S.op("act", ACT(u, bk[0:64, 0:127], AF.Identity, bias=cvec[:, 0:1]), reads=[rbk, r_cmp], writes=[r_cmp])
                    S.op("dve", TT(t1, u, u, ALU.mult), reads=[r_cmp], writes=[r_cmp])
                    S.op("dve", TS(t1, t1, 0.044715, ALU.mult, 1.0, ALU.add), reads=[r_cmp], writes=[r_cmp])
                    S.op("dve", TT(t1, t1, u, ALU.mult), reads=[r_cmp], writes=[r_cmp])
                    S.op("act", ACT(t2, t1, AF.Sigmoid, scale=1.5957691216057308), reads=[r_cmp], writes=[r_cmp])
                    S.op("dve", TT(gTt[:, 0:127], u, t2, ALU.mult), reads=[r_cmp], writes=[r_cmp])
                    bk, rbk = nb()
                    if which == 0:
                        S.pe([MM(bk[:, 0:127], w2d[:, :], gTt[:, 0:127], True, True)], reads=[r_cmp], writes=[rbk])
                        S.op("dve", CP(kcT[:, 0:127], bk[:, 0:127]), reads=[rbk], writes=[r_cmp])
                    else:
                        S.pe([MM(bk[0:127, 0:64], gTt[:, 0:127], w2d[:, 0:64], True, True)], reads=[r_cmp], writes=[rbk])
                        S.op("dve", CP(vcA[0:127, 0:64], bk[0:127, 0:64]), reads=[rbk], writes=[r_cmp])

                S.mark(f"b{b}l{l} NSA-cmp")
                for h in range(4):
                    qt = h // 2
                    pb = 64 * (h % 2)
                    for T in range(4):
                        bk, rbk = nb()
                        S.pe([MM(bk[:], kcT[pb:pb + 64, :], fmw[pb:pb + 64, qt, T * 512:(T + 1) * 512], True, True)],
                             reads=[r_cmp, r_fm[qt]], writes=[rbk])
                        E, rE = nE()
                        S.op("act", ACT(E[:], bk[:], AF.Exp), reads=[rbk], writes=[rE])
                        S.op("dve", TT(E[:], E[:], cmpok[:, T * 512:(T + 1) * 512], ALU.mult), reads=[r_const], writes=[rE])
                        acc, racc = nb()
                        S.pe([MM(acc[:, i * 65:(i + 1) * 65], E[:, i * 128:(i + 1) * 128], vcA[:, :], True, True) for i in range(4)],
                             reads=[rE, r_cmp], writes=[racc])
                        a3 = acc[:, 0:260].rearrange("p (i c) -> p i c", c=65)
                        rs4 = sm[:, 0:4]
                        S.op("dve", TS(rs4, a3[:, :, 64], 1e-30, ALU.max), reads=[racc], writes=[r_sm])
                        S.op("dve", lambda e, rs4=rs4: e.reciprocal(out=rs4, in_=rs4), reads=[r_sm], writes=[r_sm])
                        S.op("dve", TT(rs4, rs4, gts[:, T * 4:(T + 1) * 4, h * 3], ALU.mult), reads=[r_gts], writes=[r_sm])
                        S.op("dve", TT(o_tok[:, T * 4:(T + 1) * 4, h * 64:(h + 1) * 64], a3[:, :, 0:64], bc_last(rs4, 64), ALU.mult),
                             reads=[racc, r_sm], writes=[r_otok])
                        if T >= 2:
                            dB, rdB = nb()
                            S.pe([MM(dB[:], onesb[:], E[:], True, True)], reads=[rE, r_const], writes=[rdB])
                            Fd, rFd = nF()
                            S.op("dve", TS(Fd[:], dB[:], 1e-30, ALU.max), reads=[rdB], writes=[rFd])
                            S.op("dve", lambda e, Fd=Fd: e.reciprocal(out=Fd[:], in_=Fd[:]), reads=[rFd], writes=[rFd])
                            pv = flat(psel[:, (T - 2) * 4:(T - 1) * 4, :])
                            if h == 0:
                                S.op("dve", TT(pv, E[:], Fd[:], ALU.mult), reads=[rE, rFd], writes=[r_sel])
                            else:
                                S.op("dve", TT(Fd[:], E[:], Fd[:], ALU.mult), reads=[rE], writes=[rFd])
                                S.op("pool", TT(pv, pv, Fd[:], ALU.add), reads=[rFd], writes=[r_sel])
                S.mark(f"b{b}l{l} NSA-select")
                S.op("dve", CP(flat(pselb[:]), flat(psel[:])), reads=[r_sel], writes=[r_sel])
                for qb in range(8, 16):
                    bk, rbk = nb()
                    S.pe([MM(bk[:, 0:32], pselb[:, qb - 8, :], overlap[:], True, True)], reads=[r_sel, r_const], writes=[rbk])
                    sc0 = scs[:, 0, :]
                    sc1 = scs[:, 1, :]
                    S.op("dve", TT(sc0, bk[:, 0:32], selkeep[:, qb - 8, :], ALU.mult), reads=[rbk, r_const], writes=[r_sel])
                    S.op("dve", TT(sc0, sc0, seladd[:, qb - 8, :], ALU.add), reads=[r_const], writes=[r_sel])
                    S.op("dve", lambda e, sc0=sc0: e.max(out=m8[:, 0, :], in_=sc0), reads=[r_sel], writes=[r_sel])
                    S.op("dve", lambda e, sc0=sc0, sc1=sc1: e.match_replace(out=sc1, in_to_replace=m8[:, 0, :], in_values=sc0, imm_value=-3.0e38),
                         reads=[r_sel], writes=[r_sel])
                    S.op("dve", lambda e, sc1=sc1: e.max(out=m8[:, 1, :], in_=sc1), reads=[r_sel], writes=[r_sel])
                    S.op("dve", TS(unsel[:], sc0, m8[:, 1, 7:8], ALU.is_lt), reads=[r_sel], writes=[r_sel])
                    tp, rtp = nT()
                    S.pe([TR(tp[0:32, 0:128], unsel[:], ident[:])], reads=[r_sel, r_const], writes=[rtp])
                    S.op("dve", CP(unselT[:, (qb - 8) * 128:(qb - 7) * 128], tp[0:32, 0:128]), reads=[rtp], writes=[r_sel])
                S.mark(f"b{b}l{l} NSA-selwin")
                def nsa_unit(h, qb, bi, eb, reb, slot):
                    qt = h // 2
                    pb = 64 * (h % 2)
                    qap = fmw[pb:pb + 64, qt, qb * 128:(qb + 1) * 128]
                    ktile_i, vslot, slot0, nd = ((3, 0, 5, qb + 1), (4, 1, 0, min(5, qb + 1)))[bi]
                    acc_b, racc = acc_of(slot)
                    acc = acc_b[:, 0:65]
                    penq = unselT[:, (qb - 8) * 128:(qb - 7) * 128] if (bi == 0 and qb >= 8) else None
                    d0 = 0
                    while d0 < nd:
                        n = min(4, nd - d0)
                        yield from chunk(qap, r_fm[qt], fmw[:, ktile_i, :], ktile_i, pb, qb - d0, n, flat(eb[:, slot0 + d0:slot0 + d0 + n, :]), reb,
                                         lambda j, vslot=vslot: vtm[:, j, vslot, :], acc, racc, d0 == 0, d0 + n >= nd, penq=penq)
                        d0 += n
                    yield
                    rs = sm[:, 8 + (qb % 4) * 2 + bi:9 + (qb % 4) * 2 + bi]
                    S.op("dve", lambda e, rs=rs, acc_b=acc_b: e.reciprocal(out=rs, in_=acc_b[:, 64:65]), reads=[racc], writes=[r_sm])
                    S.op("dve", TT(rs, rs, gts[:, qb, h * 3 + 1 + bi:h * 3 + 2 + bi], ALU.mult), reads=[r_gts], writes=[r_sm])
                    osl = o_tok[:, qb, h * 64:(h + 1) * 64]
                    S.op("dve", STT(osl, acc_b[:, 0:64], rs, osl, ALU.mult, ALU.add), reads=[racc, r_sm], writes=[r_otok])

                set_pools(4)
                def nsa_all():
                    for h in range(4):
                        eb, reb = load_ebt([(0, h, 5, 0), (1, h, 16, 5)])
                        for qb in range(NTB):
                            for bi in range(2):
                                yield (lambda slot, h=h, qb=qb, bi=bi, eb=eb, reb=reb: nsa_unit(h, qb, bi, eb, reb, slot))
                interleave(nsa_all(), 4)
                to_oT(0)

                S.mark(f"b{b}l{l} SWA")
                load_w([(0, 652, 256), (256, 908, 64), (320, 908, 64), (384, 972, 64), (448, 972, 64), (512, 1036, 128)])
                proj_fm(0, 0, 128, 0.125)
                proj_fm(1, 128, 128, 0.125)
                proj_fm(2, 256, 128)
                proj_fm(3, 384, 128)
                proj_tm(512, 128, [v_sink(0, 0), v_sink(64, 1)])
                S.dma("sp", DMA(sinkE[:], swa_sinks[l:l + 1, :].partition_broadcast(128)), writes=[r_cmp])
                S.op("act", ACT(sinkE[:], sinkE[:], AF.Exp), reads=[r_cmp], writes=[r_cmp])
                def swa_unit(h, qb, eb, reb, slot):
                    qt = h // 2
                    pb = 64 * (h % 2)
                    g = h // 2
                    qap = fmw[pb:pb + 64, qt, qb * 128:(qb + 1) * 128]
                    acc_b, racc = acc_of(slot)
                    nd = min(2, qb + 1)
                    yield from chunk(qap, r_fm[qt], fmw[:, 2 + g, :], 2 + g, pb, qb, nd, flat(eb[:, 0:nd, :]), reb,
                                     lambda j, g=g: vtm[:, j, g, :], acc_b[:, 0:65], racc, True, True)
                    yield
                    rs = sm[:, 8 + (qb % 4):9 + (qb % 4)]
                    S.op("dve", TT(rs, acc_b[:, 64:65], sinkE[:, h:h + 1], ALU.add), reads=[racc, r_cmp], writes=[r_sm])
                    S.op("dve", lambda e, rs=rs: e.reciprocal(out=rs, in_=rs), reads=[r_sm], writes=[r_sm])
                    S.op("dve", TS(o_tok[:, qb, h * 64:(h + 1) * 64], acc_b[:, 0:64], rs, ALU.mult), reads=[racc, r_sm], writes=[r_otok])

                def swa_all():
                    for h in range(4):
                        eb, reb = load_ebt([(2, h, 2, 0)])
                        for qb in range(NTB):
                            yield (lambda slot, h=h, qb=qb, eb=eb, reb=reb: swa_unit(h, qb, eb, reb, slot))
                interleave(swa_all(), 4)
                to_oT(1)

                S.mark(f"b{b}l{l} DIL")
                load_w([(0, 1164, 768), (768, 1932, 256)])
                for ti in range(8):
                    proj_fm(ti, ti * 128, 128, 0.125 if ti < 6 else None)
                load_w([(0, 2188, 256)])
                proj_tm(0, 256, [v_sink(0, 0), v_sink(64, 1), v_sink(128, 2), v_sink(192, 3)])
                def dil_unit(h, qb, eb, reb, slot):
                    pb = 64 * (h % 2)
                    kt = 6 + h // 2
                    acc_b, racc = acc_of(slot)
                    plan = []
                    for gi, (slot0, ndmax) in enumerate(((0, 2), (2, 5), (7, 16))):
                        nd = min(ndmax, qb + 1)
                        d0 = 0
                        while d0 < nd:
                            n = min(4, nd - d0)
                            plan.append((gi, slot0, d0, n))
                            d0 += n
                    for pi, (gi, slot0, d0, n) in enumerate(plan):
                        qt = gi * 2 + h // 2
                        qap = fmw[pb:pb + 64, qt, qb * 128:(qb + 1) * 128]
                        yield from chunk(qap, r_fm[qt], fmw[:, kt, :], kt, pb, qb - d0, n, flat(eb[:, slot0 + d0:slot0 + d0 + n, :]), reb,
                                         lambda j, h=h: vtm[:, j, h, :], acc_b[:, 0:65], racc, pi == 0, pi == len(plan) - 1)
                    yield
                    rs = sm[:, 8 + (qb % 4):9 + (qb % 4)]
                    S.op("dve", lambda e, rs=rs, acc_b=acc_b: e.reciprocal(out=rs, in_=acc_b[:, 64:65]), reads=[racc], writes=[r_sm])
                    S.op("dve", TS(o_tok[:, qb, h * 64:(h + 1) * 64], acc_b[:, 0:64], rs, ALU.mult), reads=[racc, r_sm], writes=[r_otok])

                def dil_all():
                    for h in range(4):
                        eb, reb = load_ebt([(3, h, 2, 0), (4, h, 5, 2), (5, h, 16, 7)])
                        for qb in range(NTB):
                            yield (lambda slot, h=h, qb=qb, eb=eb, reb=reb: dil_unit(h, qb, eb, reb, slot))
                interleave(dil_all(), 4)
                to_oT(2)

                S.mark(f"b{b}l{l} SB")
                load_w([(0, 2444, 768)])
                for ti in range(4):
                    proj_fm(ti, ti * 128, 128, 0.125 if ti < 2 else None)
                proj_tm(512, 256, [v_sink(0, 0), v_sink(64, 1), v_sink(128, 2), v_sink(192, 3)])
                def sb_unit(h, qb, slot):
                    qt = h // 2
                    kt = 2 + h // 2
                    pb = 64 * (h % 2)
                    qap = fmw[pb:pb + 64, qt, qb * 128:(qb + 1) * 128]
                    acc_b, racc = acc_of(slot)
                    Rsb, r_Rsb = Rsbs[slot], r_Rsbs[slot]
                    nd = qb + 1
                    d0 = 0
                    while d0 < nd:
                        n = min(4, nd - d0)
                        W = n * 128
                        zb, rzb = nb()
                        S.pe([MM(zb[:, i * 128:(i + 1) * 128], fmw[pb:pb + 64, kt, (qb - d0 - i) * 128:(qb - d0 - i + 1) * 128], qap, True, True)
                              for i in range(n)], reads=[r_fm[kt], r_fm[qt]], writes=[rzb])
                        yield
                        Fe, rFe = nF()
                        S.op("act", ACT(Fe[:, 0:W], zb[:, 0:W], AF.Exp), reads=[rzb], writes=[rFe])
                        yield
                        Lb, rLb = nE()
                        S.op("act", ACT(Lb[:, 0:W], Fe[:, 0:W], AF.Ln, bias=1.0), reads=[rFe], writes=[rLb])
                        yield
                        if d0 == 0:
                            S.op("dve", TT(Lb[:, 0:128], Lb[:, 0:128], sbmask[:], ALU.mult), reads=[r_const], writes=[rLb])
                        F1, rF1 = nF()
                        S.op("dve", TT(F1[:, 0:W], zb[:, 0:W], Lb[:, 0:W], ALU.subtract), reads=[rzb, rLb], writes=[rF1])
                        yield
                        cb, rcb = nb()
                        fns = []
                        for i in range(n):
                            sl = slice(i * 128, (i + 1) * 128)
                            terms = [(negU[:], Lb[:, sl])] + [(negOnes[:], Lb[:, i2 * 128:(i2 + 1) * 128]) for i2 in range(i)]
                            if d0 > 0:
                                terms.append((ident[:], Rsb[:]))
                            for ti_, (lt, rh) in enumerate(terms):
                                fns.append(MM(cb[:, sl], lt, rh, ti_ == 0, ti_ == len(terms) - 1))
                        S.pe(fns, reads=[rLb, r_const, r_Rsb], writes=[rcb])
                        if d0 + n < nd:
                            rbk_, rrb = nb()
                            terms = [(negOnes[:], Lb[:, i * 128:(i + 1) * 128]) for i in range(n)]
                            if d0 > 0:
                                terms.append((ident[:], Rsb[:]))
                            S.pe([MM(rbk_[:, 0:128], lt, rh, ti_ == 0, ti_ == len(terms) - 1) for ti_, (lt, rh) in enumerate(terms)],
                                 reads=[rLb, r_const, r_Rsb], writes=[rrb])
                            yield
                            S.op("dve", CP(Rsb[:], rbk_[:, 0:128]), reads=[rrb], writes=[r_Rsb])
                        yield
                        S.op("dve", TT(F1[:, 0:W], F1[:, 0:W], cb[:, 0:W], ALU.add), reads=[rcb], writes=[rF1])
                        yield
                        A, rA = nE()
                        S.op("act", ACT(A[:, 0:W], F1[:, 0:W], AF.Exp), reads=[rF1], writes=[rA])
                        yield
                        if d0 == 0:
                            S.op("dve", TT(A[:, 0:128], A[:, 0:128], sbmask[:], ALU.mult), reads=[r_const], writes=[rA])
                        S.pe([MM(acc_b[:, 0:64], A[:, i * 128:(i + 1) * 128], vtm[:, qb - d0 - i, h, 0:64], d0 == 0 and i == 0,
                                 d0 + n >= nd and i == n - 1) for i in range(n)], reads=[rA, r_vtm], writes=[racc])
                        yield
                        d0 += n
                    yield
                    evac(o_tok[:, qb, h * 64:(h + 1) * 64], acc_b[:, 0:64], [racc], [r_otok])

                set_pools(6)
                SBW = 2
                Rsbs = [sb(f"Rsb{i}", [128, 128], BF16, st) for i in range(SBW)]
                r_Rsbs = [R(f"Rsb{i}") for i in range(SBW)]
                units = []
                for h in range(4):
                    for qb in range(NTB):
                        units.append(lambda slot, h=h, qb=qb: sb_unit(h, qb, slot))
                interleave(units, SBW)
                set_pools(6)
                to_oT(3)
                S.barrier()
                if dbg and b == 0 and l == 0:
                    S.dma("sp", DMA(dbg_hT, hT[:].rearrange("p a b -> p (a b)")), reads=[r_hT])
                    S.dma("sp", DMA(dbg_oT, regB[:]), reads=[r_regB])
                    S.dma("sp", DMA(dbg_mod, modT[:].rearrange("p a b -> p (a b)")), reads=[r_modT])
                    S.dma("sp", DMA(dbg_gB, gB[:].rearrange("p a b c -> p (a b c)")), reads=[r_gB])
                    S.barrier()

            S.mark(f"b{b}l{l} merge")
            with ExitStack() as st:
                wout = sb("wout", [128, 8, D], BF16, st)
                wbr = sb("wbr", [128, 4, 2, D], BF16, st)
                mergedT = sb("mergedT", [128, 8, SEQ], BF16, st)
                wgt = [sb(f"wgt{i}", [128, 8, 4, 128], BF16, st) for i in range(2)]
                maccs = [sb(f"macc{i}", [128, 512], F32, st) for i in range(2)]
                r_maccs = [R("macc0"), R("macc1")]
                mi = 0
                alloc_io(st, want_xn=False, want_lngb=True, n_xt=2)
                r_w = R("w")
                r_wgt = [R("wgt0"), R("wgt1")]
                r_mg = R("mg")
                S.dma("pool", DMA(wout[:], w_out[l].rearrange("(kc p) n -> p kc n", p=128)), writes=[r_w])
                S.op("pool", TT(wout[:], wout[:], bc_mid(gB[:, l, 0, :], 8), ALU.mult), reads=[r_gB], writes=[r_w])
                S.dma("pool", DMA(wbr[:], w_branch[l].rearrange("b (c p) n -> p b c n", p=128)), writes=[r_w])
                load_lngb(l, 0)
                for dc in range(8):
                    i = dc % 2
                    for br in range(4):
                        c0 = 3212 + br * D + dc * 128
                        S.dma("pool", DMA(wgt[i][:, :, br, :], w_in_v[:, :, c0:c0 + 128]), writes=[r_wgt[i]])
                    for T in range(4):
                        ts_ = slice(T * 512, (T + 1) * 512)
                        macc = maccs[mi % 2]
                        r_macc = r_maccs[mi % 2]
                        mi += 1
                        for br in range(4):
                            bG, rbG = nb()
                            S.pe([MM(bG[:], wgt[i][:, kc, br, :], hT[:, kc, ts_], kc == 0, kc == 7) for kc in range(8)],
                                 reads=[r_wgt[i], r_hT], writes=[rbG])
                            bP, rbP = nb()
                            S.pe([MM(bP[:], wbr[:, br, c, dc * 128:(dc + 1) * 128], oT[:, br, c, ts_], c == 0, c == 1) for c in range(2)],
                                 reads=[r_w, r_oT], writes=[rbP])
                            Fg, rFg = nF()
                            S.op("act", ACT(Fg[:], bG[:], AF.Sigmoid), reads=[rbG], writes=[rFg])
                            if br == 0:
                                S.op("dve", TT(macc[:], Fg[:], bP[:], ALU.mult), reads=[rFg, rbP], writes=[r_macc])
                            else:
                                S.op("dve", TT(Fg[:], Fg[:], bP[:], ALU.mult), reads=[rbP], writes=[rFg])
                                if br < 3:
                                    S.op("pool", TT(macc[:], macc[:], Fg[:], ALU.add), reads=[rFg], writes=[r_macc])
                                else:
                                    S.op("pool", TT(mergedT[:, dc, ts_], macc[:], Fg[:], ALU.add), reads=[rFg, r_macc], writes=[r_mg])
                S.mark(f"b{b}l{l} wout+LN")
                for g4 in range(8):
                    for i in range(2):
                        tb = g4 * 2 + i
                        S.dma("sp", DMA(xt[i][:], x_src[tb * 128:(tb + 1) * 128, :]), reads=[r_xsrc], writes=[r_xt[i]])
                    for i in range(2):
                        tb = g4 * 2 + i
                        for dh in range(2):
                            bk, rbk = nb()
                            S.pe([MM(bk[:], mergedT[:, kc, tb * 128:(tb + 1) * 128], wout[:, kc, dh * 512:(dh + 1) * 512], kc == 0, kc == 7)
                                  for kc in range(8)], reads=[r_mg, r_w], writes=[rbk])
                            xs_ = xt[i][:, dh * 512:(dh + 1) * 512]
                            S.op("dve", STT(xs_, xs_, float(ALPHA), bk[:], ALU.mult, ALU.add), reads=[rbk], writes=[r_xt[i]])
                    ln_stats_group([(xt[i][:], r_xt[i], g4 * 2 + i) for i in range(2)])
                    for i in range(2):
                        tb = g4 * 2 + i
                        affine_k(xt[i][:], r_xt[i], tb)
                        S.dma("sp", DMA(xm[tb * 128:(tb + 1) * 128, :], xt[i][:]), reads=[r_xt[i]], writes=[r_xm])
                        if dbg and b == 0 and l == 0:
                            S.dma("sp", DMA(dbg_xm[tb * 128:(tb + 1) * 128, :], xt[i][:]), reads=[r_xt[i]])
                S.barrier()

            S.mark(f"b{b}l{l} LN2")
            with ExitStack() as st:
                rr = sb("rr", [128, NTB, D], F32, st)
                alloc_io(st, want_xt=False, want_lngb=True)
                aT = [sb(f"aT{i}", [128, 2, SEQ], BF16, st) for i in range(2)]
                gate_sb = sb("gate_sb", [128, NTB, 8], F32, st)
                wr = sb("wr", [128, 8, 8], BF16, st)
                lg = sb("lg", [128, 8], F32, st)
                mx = sb("mx", [128, 8], F32, st)
                r_rr = [R(f"rr{i}") for i in range(NTB)]
                r_aT = [R("aT0"), R("aT1")]
                r_gate = R("gate")
                r_wr = R("wr")
                wg = [regB[:, i * 2048:(i + 1) * 2048].rearrange("p (k f) -> p k f", k=8) for i in range(2)]
                wu = [regB[:, 4096 + i * 2048:4096 + (i + 1) * 2048].rearrange("p (k f) -> p k f", k=8) for i in range(2)]
                wd = [regB[:, 8192 + i * 2048:8192 + (i + 1) * 2048].rearrange("p (c d) -> p c d", c=2) for i in range(2)]
                r_wg = [R("wg0"), R("wg1")]
                r_wd = [R("wd0"), R("wd1")]
                for tb in range(NTB):
                    S.dma("sp", DMA(rr[:, tb, :], xm[tb * 128:(tb + 1) * 128, :]), reads=[r_xm], writes=[r_rr[tb]])
                for g4 in range(4):
                    ln_stats_group([(rr[:, g4 * 4 + i, :], r_rr[g4 * 4 + i], g4 * 4 + i) for i in range(4)])
                    for i in range(4):
                        tb = g4 * 4 + i
                        to_hT(rr[:, tb, :], r_rr[tb], tb, tb, l, 32, 24)
                        S.op("pool", TS(rr[:, tb, :], rr[:, tb, :], float(ALPHA), ALU.mult), writes=[r_rr[tb]])
                load_lngb(l, 1)
                S.mark(f"b{b}l{l} FFN")
                moe = (l % 2 == 1)
                if moe:
                    S.dma("pool", DMA(wr[:], moe_router[0].rearrange("(kc p) e -> p kc e", p=128)), writes=[r_wr])
                    for tb in range(NTB):
                        bk, rbk = nb()
                        S.pe([MM(bk[:, 0:8], hT[:, kc, tb * 128:(tb + 1) * 128], wr[:, kc, :], kc == 0, kc == 7) for kc in range(8)],
                             reads=[r_hT, r_wr], writes=[rbk])
                        S.op("dve", CP(lg[:], bk[:, 0:8]), reads=[rbk], writes=[r_gate])
                        S.op("dve", lambda e: e.max(out=mx[:], in_=lg[:]), reads=[r_gate], writes=[r_gate])
                        gsl = gate_sb[:, tb, :]
                        S.op("dve", TS(gsl, lg[:], mx[:, 1:2], ALU.is_ge), reads=[r_gate], writes=[r_gate])
                        S.op("dve", TS(mx[:, 2:3], mx[:, 0:1], -1.0, ALU.mult), reads=[r_gate], writes=[r_gate])
                        S.op("act", ACT(lg[:], lg[:], AF.Exp, bias=mx[:, 2:3]), reads=[r_gate], writes=[r_gate])
                        S.op("dve", TT(gsl, gsl, lg[:], ALU.mult), reads=[r_gate], writes=[r_gate])
                        S.op("dve", lambda e, gsl=gsl: e.reduce_sum(out=mx[:, 3:4], in_=gsl, axis=mybir.AxisListType.X), reads=[r_gate], writes=[r_gate])
                        S.op("dve", lambda e: e.reciprocal(out=mx[:, 3:4], in_=mx[:, 3:4]), reads=[r_gate], writes=[r_gate])
                        S.op("dve", TS(gsl, gsl, mx[:, 3:4], ALU.mult), reads=[r_gate], writes=[r_gate])
                    experts = [(moe_w_gate[0, e], moe_w_up[0, e], moe_w_down[0, e], FF_EXP, e) for e in range(8)]
                else:
                    experts = [(ffn_w_gate[0], ffn_w_up[0], ffn_w_down[0], FF_DENSE, None)]
                items = []
                for (Wg, Wu, Wd, FF, eidx) in experts:
                    Wgv = Wg.rearrange("(kc p) f -> p kc f", p=128)
                    Wuv = Wu.rearrange("(kc p) f -> p kc f", p=128)
                    Wdv = Wd.rearrange("(c p) d -> p c d", p=128)
                    for fi in range(FF // 256):
                        items.append((Wgv, Wuv, Wdv, fi, eidx))

                def ffn_loads(k):
                    Wgv, Wuv, Wdv, fi, eidx = items[k]
                    i = k % 2
                    f0 = fi * 256
                    S.dma("pool", DMA(wg[i], Wgv[:, :, f0:f0 + 256]), writes=[r_wg[i]])
                    S.dma("pool", DMA(wu[i], Wuv[:, :, f0:f0 + 256]), writes=[r_wg[i]])
                    S.dma("pool", DMA(wd[i], Wdv[:, fi * 2:fi * 2 + 2, :]), writes=[r_wd[i]])
                    S.op("pool", TT(wd[i], wd[i], bc_mid(gB[:, l, 1, :], 2), ALU.mult), reads=[r_gB], writes=[r_wd[i]])

                def ffn_gu(k):
                    i = k % 2
                    for fc in range(2):
                        for T in range(4):
                            ts_ = slice(T * 512, (T + 1) * 512)
                            bG, rbG = nb()
                            S.pe([MM(bG[:], wg[i][:, kc, fc * 128:(fc + 1) * 128], hT[:, kc, ts_], kc == 0, kc == 7) for kc in range(8)],
                                 reads=[r_wg[i], r_hT], writes=[rbG])
                            bU, rbU = nb()
                            S.pe([MM(bU[:], wu[i][:, kc, fc * 128:(fc + 1) * 128], hT[:, kc, ts_], kc == 0, kc == 7) for kc in range(8)],
                                 reads=[r_wg[i], r_hT], writes=[rbU])
                            Fg, rFg = nF()
                            S.op("act", ACT(Fg[:], bG[:], AF.Silu), reads=[rbG], writes=[rFg])
                            S.op("dve", TT(aT[i][:, fc, ts_], Fg[:], bU[:], ALU.mult), reads=[rFg, rbU], writes=[r_aT[i]])

                def ffn_down(k):
                    i = k % 2
                    eidx = items[k][4]
                    for tb in range(NTB):
                        for dh in range(2):
                            bk, rbk = nb()
                            S.pe([MM(bk[:], aT[i][:, fc, tb * 128:(tb + 1) * 128], wd[i][:, fc, dh * 512:(dh + 1) * 512], fc == 0, fc == 1)
                                  for fc in range(2)], reads=[r_aT[i], r_wd[i]], writes=[rbk])
                            rs_ = rr[:, tb, dh * 512:(dh + 1) * 512]
                            if eidx is None:
                                S.op("dve", TT(rs_, rs_, bk[:], ALU.add), reads=[rbk], writes=[r_rr[tb]])
                            else:
                                S.op("dve", STT(rs_, bk[:], gate_sb[:, tb, eidx:eidx + 1], rs_, ALU.mult, ALU.add),
                                     reads=[rbk, r_gate], writes=[r_rr[tb]])

                nk = len(items)
                ffn_loads(0)
                ffn_gu(0)
                for k in range(nk):
                    if k + 1 < nk:
                        ffn_loads(k + 1)
                        ffn_gu(k + 1)
                    ffn_down(k)
                S.mark(f"b{b}l{l} finalLN")
                dst = out[b] if l == depth - 1 else xs[b]
                r_dst = r_out if l == depth - 1 else r_xs
                for g4 in range(4):
                    ln_stats_group([(rr[:, g4 * 4 + i, :], r_rr[g4 * 4 + i], g4 * 4 + i) for i in range(4)])
                    for i in range(4):
                        tb = g4 * 4 + i
                        affine_k(rr[:, tb, :], r_rr[tb], tb)
                        S.dma("sp", DMA(dst[tb * 128:(tb + 1) * 128, :], rr[:, tb, :]), reads=[r_rr[tb]], writes=[r_dst])
                S.barrier()

    S.barrier()
    S.mark("end")
    ES.close()
    S.close()
    nc._marks = S.marks
    return nc


_CACHE = {}


def _get_nc(nseq, depth):
    key = (nseq, depth)
    if key not in _CACHE:
        _CACHE[key] = build_nc(nseq, depth)
    return _CACHE[key]


def make_in_maps(inputs, ncores, nseq):
    consts = host_consts()
    f = lambda a: np.ascontiguousarray(np.asarray(a, dtype=np.float32))
    shared = {k: f(inputs[k]) for k in ("rel_bias", "w_ada", "b_ada", "w_in", "w_branch", "w_out", "cmp_w1", "cmp_w2", "swa_sinks",
                                        "ln_g", "ln_b", "ffn_w_gate", "ffn_w_up", "ffn_w_down", "moe_router", "moe_w_gate",
                                        "moe_w_up", "moe_w_down")}
    shared["b_adaT"] = f(np.asarray(inputs["b_ada"]).reshape(2, 48, 128).transpose(0, 2, 1))
    shared["cmp_peT"] = f(np.asarray(inputs["cmp_pe"]).transpose(0, 1, 3, 2))
    shared.update(consts)
    x = np.asarray(inputs["x"], dtype=np.float32)
    c = np.asarray(inputs["c"], dtype=np.float32)
    maps = []
    for i in range(ncores):
        m = dict(shared)
        m["x"] = np.ascontiguousarray(x[i * nseq:(i + 1) * nseq])
        cc = c[i * nseq:(i + 1) * nseq]
        m["cT"] = np.ascontiguousarray(cc.reshape(nseq, 8, 128).transpose(0, 2, 1))
        maps.append(m)
    return maps


def kernel(**inputs):
    nseq = 2
    nc = _get_nc(nseq, DEPTH)
    maps = make_in_maps(inputs, NCORES, nseq)
    res = run_bass_kernel_spmd(nc, maps, core_ids=list(range(NCORES)))
    outs = [np.asarray(r["out"], dtype=np.float32) for r in res.results]
    return np.concatenate(outs, axis=0)
```

```python
import math
from contextlib import ExitStack
import numpy as np
import concourse.bass as bass
import concourse.mybir as mybir
from concourse.ap import AP
from concourse.bass_utils import run_bass_kernel_spmd

F32 = mybir.dt.float32
BF16 = mybir.dt.bfloat16
AF = mybir.ActivationFunctionType
ALU = mybir.AluOpType

D = 1024
SEQ = 2048
NTB = 16
DEPTH = 2
NCORES = 8
NDV = 2304
ALPHA = (2 * DEPTH) ** 0.25
LN_EPS = 1e-5
D_IN = 7308
FF_DENSE = 2816
FF_EXP = 3584
EPOCH = 30000
NDMASEM = 8


class Res:
    __slots__ = ("name", "w", "r")

    def __init__(self, name=""):
        self.name = name
        self.w = None
        self.r = []


class Sched:
    def __init__(self, nc):
        self.nc = nc
        self.h = {"pe": nc.tensor, "act": nc.scalar, "dve": nc.vector, "pool": nc.gpsimd, "sp": nc.sync}
        self.names = list(self.h)
        self._ctx = []
        self.sems = {n: self._newsem(n + "_c0") for n in self.names}
        self.cnt = {n: 0 for n in self.names}
        self.epoch = {n: 0 for n in self.names}
        self.waited = {n: {} for n in self.names}
        self.last = {n: None for n in self.names}
        self.dq = ("sp", "act", "pool")
        self.dsems = {n: [self._newsem(f"{n}_d{i}") for i in range(NDMASEM)] for n in self.dq}
        self.dcnt = {n: 0 for n in self.dq}
        self.dlast = {n: [None] * NDMASEM for n in self.dq}
        self.nins = {}
        self.marks = []

    def _newsem(self, name):
        cm = self.nc.semaphore(name)
        s = cm.__enter__()
        self._ctx.append(cm)
        self._keep = getattr(self, "_keep", [])
        self._keep.append(s)
        return s

    def _deps(self, reads, writes):
        deps = []
        for r in reads:
            if r.w is not None:
                deps.append(r.w)
        for w in writes:
            if w.w is not None:
                deps.append(w.w)
            deps.extend(w.r)
        return deps

    def _commit(self, reads, writes, tok):
        for r in reads:
            r.r.append(tok)
            if len(r.r) > 64:
                best = {}
                for (s, v) in r.r:
                    if id(s) not in best or best[id(s)][1] < v:
                        best[id(s)] = (s, v)
                r.r = list(best.values())
        for w in writes:
            w.w = tok
            w.r = []

    def _emit(self, eng, deps, fn, inc):
        wd = self.waited[eng]
        best = {}
        for (s, v) in deps:
            k = id(s)
            if wd.get(k, 0) >= v:
                continue
            if k not in best or best[k][1] < v:
                best[k] = (s, v)
        e = self.h[eng]
        for k, (s, v) in best.items():
            wd[k] = v
            e.wait_ge(s, v)
        if fn is not None:
            ins = fn(e)
            self.nins[eng] = self.nins.get(eng, 0) + 1
            if inc is not None:
                ins.then_inc(inc[0], inc[1])

    def mark(self, name):
        self.marks.append((name, dict(self.nins)))

    def _tick(self, eng):
        if self.cnt[eng] >= EPOCH:
            self.epoch[eng] += 1
            self.sems[eng] = self._newsem(f"{eng}_c{self.epoch[eng]}")
            self.cnt[eng] = 0
        self.cnt[eng] += 1
        return (self.sems[eng], self.cnt[eng])

    def op(self, eng, fn, reads=(), writes=()):
        reads = [r for r in reads if r is not None]
        writes = [w for w in writes if w is not None]
        deps = self._deps(reads, writes)
        tok = self._tick(eng)
        self._emit(eng, deps, fn, (tok[0], 1))
        self._commit(reads, writes, tok)
        self.last[eng] = tok
        return tok

    def pe(self, fns, reads=(), writes=()):
        reads = [r for r in reads if r is not None]
        writes = [w for w in writes if w is not None]
        deps = self._deps(reads, writes)
        tok = self._tick("pe")
        n = len(fns)
        for i, fn in enumerate(fns):
            self._emit("pe", deps if i == 0 else [], fn, (tok[0], 1) if i == n - 1 else None)
        self._commit(reads, writes, tok)
        self.last["pe"] = tok
        return tok

    def dma(self, q, fn, reads=(), writes=()):
        reads = [r for r in reads if r is not None]
        writes = [w for w in writes if w is not None]
        deps = self._deps(reads, writes)
        m = self.dcnt[q]
        self.dcnt[q] += 1
        nsl = 3 if q == "sp" else NDMASEM
        slot = m % nsl
        sem = self.dsems[q][slot]
        val = 16 * (m // nsl + 1)
        if self.dlast[q][slot] is not None:
            deps.append(self.dlast[q][slot])
        tok = (sem, val)
        self.dlast[q][slot] = tok
        self._emit(q, deps, fn, (sem, 16))
        self._commit(reads, writes, tok)
        return tok

    def barrier(self):
        toks = [t for t in self.last.values() if t is not None]
        for q in self.dq:
            toks += [t for t in self.dlast[q] if t is not None]
        for n in self.names:
            self._emit(n, toks, None, None)

    def close(self):
        for cm in reversed(self._ctx):
            cm.__exit__(None, None, None)


def MM(out, lhsT, rhs, start=True, stop=True):
    return lambda e: e.matmul(out, lhsT=lhsT, rhs=rhs, start=start, stop=stop)


def TR(out, in_, ident):
    return lambda e: e.transpose(out, in_, ident)


def ACT(out, in_, func, bias=None, scale=None):
    kw = {}
    if bias is not None:
        kw["bias"] = bias
    if scale is not None:
        kw["scale"] = scale
    return lambda e: e.activation(out=out, in_=in_, func=func, **kw)


def TT(out, a, b, op):
    return lambda e: e.tensor_tensor(out=out, in0=a, in1=b, op=op)


def TS(out, a, s1, op0, s2=None, op1=None):
    if op1 is None:
        return lambda e: e.tensor_scalar(out=out, in0=a, scalar1=s1, scalar2=None, op0=op0)
    return lambda e: e.tensor_scalar(out=out, in0=a, scalar1=s1, scalar2=s2, op0=op0, op1=op1)


def STT(out, in0, scalar, in1, op0, op1):
    return lambda e: e.scalar_tensor_tensor(out=out, in0=in0, scalar=scalar, in1=in1, op0=op0, op1=op1)


def CP(out, in_):
    return lambda e: e.tensor_copy(out=out, in_=in_)


def DMA(out, in_):
    return lambda e: e.dma_start(out=out, in_=in_)


def MSET(ap, v):
    return lambda e: e.memset(ap, v)


def bc_last(ap2, m):
    a = ap2.ap
    return AP(ap2.tensor, ap2.offset, [list(a[0]), list(a[1]), [0, m]])


def bc_mid(ap2, m):
    a = ap2.ap
    return AP(ap2.tensor, ap2.offset, [list(a[0]), [0, m], list(a[1])])


def _t5_bucket(dist):
    dist = np.maximum(dist, 0)
    d_f = np.maximum(dist, 1).astype(np.float32)
    large = 16 + (np.log(d_f / np.float32(16)) / np.float32(math.log(2048 / 16)) * np.float32(16)).astype(np.int32)
    large = np.minimum(large, 31)
    return np.where(dist < 16, dist, large)


def host_consts():
    c = {}
    u = np.arange(NDV)
    d = u - 128
    onehot = np.zeros((32, NDV), np.float32)
    bk = _t5_bucket(d)
    onehot[bk[d >= 0], u[d >= 0]] = 1.0
    c["k_onehot"] = onehot
    vm = np.zeros((6, NDV), np.float32)
    vm[0] = (d >= 0) & (d <= 511)
    vm[1] = (d >= 0) & (d <= 2047)
    vm[2] = (d >= 0) & (d <= 127)
    vm[3] = (d >= 0) & (d <= 128)
    vm[4] = (d >= 0) & (d <= 512) & (d % 4 == 0)
    vm[5] = (d >= 0) & (d <= 2047) & (d % 16 == 0)
    c["k_vmask"] = np.ascontiguousarray(np.broadcast_to(vm[:, None, :], (6, 4, NDV))).astype(np.float32)
    c["k_ident"] = np.eye(128, dtype=np.float32)
    j = np.arange(128)[:, None]
    s = np.arange(128)[None, :]
    c["k_negU"] = -(j > s).astype(np.float32)
    c["k_sbmask"] = (j < s).astype(np.float32)
    ci = np.arange(128)[:, None]
    sj = np.arange(32)[None, :]
    ov = ((ci * 16 + 31 >= sj * 64) & (ci * 16 < (sj + 1) * 64) & (ci < 127)).astype(np.float32)
    c["k_overlap"] = ov
    t = np.arange(SEQ)[None, :]
    c["k_cmpok"] = ((ci * 16 + 31 <= t) & (ci < 127)).astype(np.float32)
    keep = np.zeros((128, 8, 32), np.float32)
    add = np.zeros((128, 8, 32), np.float32)
    for qb in range(8, 16):
        tt = qb * 128 + np.arange(128)
        cur = tt // 64
        for jj in range(32):
            fut = jj > cur
            f0 = np.full(128, jj == 0)
            f1 = jj == cur
            f2 = jj == cur - 1
            k = ~(fut | f0 | f1 | f2)
            a = np.where(f0, 3e4, np.where(f1, 2e4, np.where(f2, 1e4, np.where(fut, -1e30, 0.0))))
            keep[:, qb - 8, jj] = k
            add[:, qb - 8, jj] = a
    c["k_selkeep"] = keep
    c["k_seladd"] = add
    pen = np.zeros((32, 16, 128), np.float32)
    for J in range(16):
        for k in range(128):
            pen[2 * J + k // 64, J, k] = -30000.0
    c["k_pen"] = pen
    return c


def build_nc(NSEQ=2, depth=DEPTH, dbg=False):
    nc = bass.Bass("TRN2", target_bir_lowering=False)
    S = Sched(nc)
    ES = ExitStack()

    def din(name, shape, dt=F32):
        return nc.dram_tensor(name, list(shape), dt, kind="ExternalInput").ap()

    x = din("x", [NSEQ, SEQ, D])
    cT = din("cT", [NSEQ, 128, 8])
    rel_bias = din("rel_bias", [32, 20])
    w_ada = din("w_ada", [2, D, 6 * D])
    b_ada = din("b_ada", [2, 6 * D])
    b_adaT = din("b_adaT", [2, 128, 48])
    w_in = din("w_in", [2, D, D_IN])
    w_branch = din("w_branch", [2, 4, 256, D])
    w_out = din("w_out", [2, D, D])
    cmp_peT = din("cmp_peT", [2, 2, 64, 32])
    cmp_w1 = din("cmp_w1", [2, 2, 2048, 64])
    cmp_w2 = din("cmp_w2", [2, 2, 64, 64])
    swa_sinks = din("swa_sinks", [2, 4])
    ln_g = din("ln_g", [2, 2, D])
    ln_b = din("ln_b", [2, 2, D])
    ffn_w_gate = din("ffn_w_gate", [1, D, FF_DENSE])
    ffn_w_up = din("ffn_w_up", [1, D, FF_DENSE])
    ffn_w_down = din("ffn_w_down", [1, FF_DENSE, D])
    moe_router = din("moe_router", [1, D, 8])
    moe_w_gate = din("moe_w_gate", [1, 8, D, FF_EXP])
    moe_w_up = din("moe_w_up", [1, 8, D, FF_EXP])
    moe_w_down = din("moe_w_down", [1, 8, FF_EXP, D])
    k_onehot = din("k_onehot", [32, NDV])
    k_vmask = din("k_vmask", [6, 4, NDV])
    k_ident = din("k_ident", [128, 128])
    k_negU = din("k_negU", [128, 128])
    k_sbmask = din("k_sbmask", [128, 128])
    k_overlap = din("k_overlap", [128, 32])
    k_cmpok = din("k_cmpok", [128, SEQ])
    k_selkeep = din("k_selkeep", [128, 8, 32])
    k_seladd = din("k_seladd", [128, 8, 32])
    k_pen = din("k_pen", [32, 16, 128])
    out = nc.dram_tensor("out", [NSEQ, SEQ, D], F32, kind="ExternalOutput").ap()
    if dbg:
        dbg_hT = nc.dram_tensor("dbg_hT", [128, 8 * SEQ], BF16, kind="ExternalOutput").ap()
        dbg_oT = nc.dram_tensor("dbg_oT", [128, 4 * 2 * SEQ], BF16, kind="ExternalOutput").ap()
        dbg_mod = nc.dram_tensor("dbg_mod", [128, 96], F32, kind="ExternalOutput").ap()
        dbg_gB = nc.dram_tensor("dbg_gB", [128, 4 * D], BF16, kind="ExternalOutput").ap()
        dbg_xm = nc.dram_tensor("dbg_xm", [SEQ, D], F32, kind="ExternalOutput").ap()
    xs = nc.dram_tensor("xs", [NSEQ, SEQ, D], F32, kind="Internal").ap()
    xm = nc.dram_tensor("xm", [SEQ, D], F32, kind="Internal").ap()
    ebd_t = nc.dram_tensor("ebd", [24, NDV], BF16, kind="Internal")
    ebd = ebd_t.ap()
    WS = NDV + 128
    ebs_t = nc.dram_tensor("ebs", [24, 128, WS], BF16, kind="Internal")

    uniq = {"n": 0}

    def sb(name, shape, dt, stack=ES):
        uniq["n"] += 1
        return stack.enter_context(nc.sbuf_tensor(f"{name}_{uniq['n']}", list(shape), dt))

    def ps(name, shape, dt):
        return ES.enter_context(nc.psum_tensor(name, list(shape), dt))

    ident = sb("ident", [128, 128], BF16)
    negU = sb("negU", [128, 128], BF16)
    negOnes = sb("negOnes", [128, 128], BF16)
    onesb = sb("onesb", [128, 128], BF16)
    sbmask = sb("sbmask", [128, 128], BF16)
    overlap = sb("overlap", [128, 32], BF16)
    cmpok = sb("cmpok", [128, SEQ], BF16)
    selkeep = sb("selkeep", [128, 8, 32], F32)
    seladd = sb("seladd", [128, 8, 32], F32)
    pen = sb("pen", [32, 16, 128], BF16)
    hT = sb("hT", [128, 8, SEQ], BF16)
    modT = sb("modT", [128, 2, 48], F32)
    gB = sb("gB", [128, 2, 2, D], BF16)
    lngb_h = [None]
    xt = [None, None, None, None]
    xn = [None, None]

    def alloc_io(st, want_xt=True, want_xn=True, want_lngb=False, n_xt=4):
        if want_xt:
            for i_ in range(n_xt):
                xt[i_] = sb(f"xt{i_}", [128, D], F32, st)
        if want_xn:
            xn[0] = sb("xn0", [128, D], BF16, st)
            xn[1] = sb("xn1", [128, D], BF16, st)
        if want_lngb:
            lngb_h[0] = sb("lngb", [128, 2, D], F32, st)
    st6 = sb("st6", [128, 2, 6], F32)
    mvk = sb("mvk", [128, NTB, 2], F32)
    rsk = sb("rsk", [128, NTB], F32)
    mv = sb("mv", [128, 2], F32)
    rstd = sb("rstd", [128, 1], F32)
    Et = [sb(f"E{i}", [128, 512], BF16) for i in range(8)]
    Ft = [sb(f"F{i}", [128, 512], F32) for i in range(6)]
    sm = sb("sm", [128, 64], F32)
    regB = sb("regB", [128, 4 * 2 * SEQ], BF16)
    pg = [ps(f"pg{i}", [128, 512], F32) for i in range(8)]

    R = lambda n: Res(n)
    r_const = R("const")
    r_hT = R("hT")
    r_modT = R("modT")
    r_gB = R("gB")
    r_lngb = R("lngb")
    r_xt = [R(f"xt{i}") for i in range(4)]
    r_xn = [R("xn0"), R("xn1")]
    r_st = R("st")
    r_mvk = R("mvk")
    r_E = [R(f"E{i}") for i in range(8)]
    r_F = [R(f"F{i}") for i in range(6)]
    r_sm = R("sm")
    r_pg = [R(f"pg{i}") for i in range(8)]
    r_regB = R("regB")
    r_ebd = R("ebd")
    r_xs = R("xs")
    r_xm = R("xm")
    r_out = R("out")
    ctr = {"pg": 0, "E": 0, "F": 0, "pT": 0, "xt": 0, "acc": 0}

    pools = {"ns": 6}

    def set_pools(nscore):
        pools["ns"] = nscore

    def nb():
        i = ctr["pg"] % pools["ns"]
        ctr["pg"] += 1
        return pg[i], r_pg[i]

    r_accreg = [R(f"accreg{i}") for i in range(7)]
    r_Rreg = [R(f"Rreg{i}") for i in range(4)]

    def nacc():
        na = 8 - pools["ns"]
        i = pools["ns"] + ctr["acc"] % na
        ctr["acc"] += 1
        return pg[i], r_pg[i]

    def interleave(factories, width):
        it = iter(factories)
        active = []
        free = list(range(width))
        while True:
            while free:
                f = next(it, None)
                if f is None:
                    break
                slot = free.pop(0)
                active.append((f(slot), slot))
            if not active:
                break
            for (g, slot) in list(active):
                try:
                    next(g)
                except StopIteration:
                    active.remove((g, slot))
                    free.append(slot)

    def acc_of(slot):
        i = pools["ns"] + slot
        assert i < 8
        return pg[i], r_pg[i]

    def nE():
        i = ctr["E"] % 8
        ctr["E"] += 1
        return Et[i], r_E[i]

    def nF():
        i = ctr["F"] % 6
        ctr["F"] += 1
        return Ft[i], r_F[i]

    def nT():
        bk, rbk = nb()
        return bk[:].bitcast(BF16), rbk

    for dst, src in ((ident, k_ident), (negU, k_negU), (sbmask, k_sbmask), (overlap, k_overlap), (cmpok, k_cmpok), (pen, k_pen)):
        S.dma("pool", DMA(dst[:], src), writes=[r_const])
    S.dma("sp", DMA(selkeep[:], k_selkeep), writes=[r_const])
    S.dma("sp", DMA(seladd[:], k_seladd), writes=[r_const])
    S.op("dve", MSET(negOnes[:], -1.0), writes=[r_const])
    S.op("dve", MSET(onesb[:], 1.0), writes=[r_const])

    with ExitStack() as st:
        oh = sb("oh", [32, NDV], BF16, st)
        rb = sb("rb", [32, 20], F32, st)
        rbh = sb("rbh", [32, 20], BF16, st)
        rbh32 = sb("rbh32", [32, 20], F32, st)
        rbl = sb("rbl", [32, 20], BF16, st)
        vmk = sb("vmk", [4, NDV], F32, st)
        ebf = sb("ebf", [4, NDV], F32, st)
        ebb = sb("ebb", [4, NDV], BF16, st)
        r_p = R("prol")
        S.dma("pool", DMA(oh[:], k_onehot), writes=[r_p])
        S.dma("sp", DMA(rb[:], rel_bias), writes=[r_p])
        S.op("dve", CP(rbh[:], rb[:]), reads=[r_p], writes=[r_p])
        S.op("dve", CP(rbh32[:], rbh[:]), reads=[r_p], writes=[r_p])
        S.op("dve", TT(rbh32[:], rb[:], rbh32[:], ALU.subtract), reads=[r_p], writes=[r_p])
        S.op("dve", CP(rbl[:], rbh32[:]), reads=[r_p], writes=[r_p])
        vheads = [0, 0, 4, 8, 12, 16]
        for v in range(6):
            h0 = vheads[v]
            for cch in range(6):
                bk, rbk = nb()
                cs = slice(cch * 384, (cch + 1) * 384)
                S.pe([MM(bk[0:4, 0:384], rbh[:, h0:h0 + 4], oh[:, cs], True, False),
                      MM(bk[0:4, 0:384], rbl[:, h0:h0 + 4], oh[:, cs], False, True)], reads=[r_p], writes=[rbk])
                S.op("act", ACT(ebf[:, cs], bk[0:4, 0:384], AF.Exp), reads=[rbk], writes=[r_p])
            S.dma("sp", DMA(vmk[:], k_vmask[v]), writes=[r_p])
            S.op("dve", TT(ebb[:], ebf[:], vmk[:], ALU.mult), reads=[r_p], writes=[r_p])
            S.dma("sp", DMA(ebd[v * 4:(v + 1) * 4, :], ebb[:]), reads=[r_p], writes=[r_ebd])
        skw = sb("skw", [128, NDV], BF16, st)
        r_skw = R("skw")
        for r_ in range(24):
            S.dma("sp", DMA(skw[:], ebd[r_:r_ + 1, :].partition_broadcast(128)), reads=[r_ebd], writes=[r_skw])
            S.dma("sp", DMA(AP(ebs_t, r_ * 128 * WS, [[WS + 1, 128], [1, NDV]]), skw[:]), reads=[r_skw], writes=[r_ebd])
        S.barrier()

    def toeplitz(variant, hh, ndl):
        off = (variant * 4 + hh) * 128 * WS + 128
        return AP(ebs_t, off, [[WS, 128], [128, ndl], [1, 128]])

    def ln_stats_group(srcs):
        ks = [k for (_, _, k) in srcs]
        for (src_ap, r_src, k) in srcs:
            S.op("dve", lambda e, src_ap=src_ap: e.bn_stats(out=st6[:, 0, :], in_=src_ap[:, 0:512]), reads=[r_src], writes=[r_st])
            S.op("dve", lambda e, src_ap=src_ap: e.bn_stats(out=st6[:, 1, :], in_=src_ap[:, 512:1024]), reads=[r_src], writes=[r_st])
            S.op("dve", lambda e, k=k: e.bn_aggr(out=mvk[:, k, :], in_=st6[:]), reads=[r_st], writes=[r_st, r_mvk])
        k0, k1 = min(ks), max(ks) + 1
        S.op("dve", TS(rsk[:, k0:k1], mvk[:, k0:k1, 1], LN_EPS, ALU.add), reads=[r_mvk], writes=[r_mvk])
        S.op("act", ACT(rsk[:, k0:k1], rsk[:, k0:k1], AF.Sqrt), reads=[r_mvk], writes=[r_mvk])
        S.op("dve", lambda e: e.reciprocal(out=rsk[:, k0:k1], in_=rsk[:, k0:k1]), reads=[r_mvk], writes=[r_mvk])

    def to_hT(src_ap, r_src, k, tb, l, scb, shb):
        i = tb % 2
        S.op("dve", TS(xn[i][:], src_ap, mvk[:, k, 0:1], ALU.subtract, rsk[:, k:k + 1], ALU.mult), reads=[r_src, r_mvk], writes=[r_xn[i]])
        tp, rtp = nT()
        S.pe([TR(tp[:, kc * 128:(kc + 1) * 128], xn[i][:, kc * 128:(kc + 1) * 128], ident[:]) for kc in range(8)],
             reads=[r_xn[i], r_const], writes=[rtp])
        for kc in range(8):
            S.op("act", ACT(hT[:, kc, tb * 128:(tb + 1) * 128], tp[:, kc * 128:(kc + 1) * 128], AF.Identity,
                            bias=modT[:, l, shb + kc:shb + kc + 1], scale=modT[:, l, scb + kc:scb + kc + 1]),
                 reads=[rtp, r_modT], writes=[r_hT])

    def affine_k(buf_ap, r_buf, k):
        lngb = lngb_h[0]
        S.op("dve", STT(buf_ap, buf_ap, mvk[:, k, 0:1], lngb[:, 0, :], ALU.subtract, ALU.mult), reads=[r_mvk, r_lngb], writes=[r_buf])
        S.op("dve", STT(buf_ap, buf_ap, rsk[:, k:k + 1], lngb[:, 1, :], ALU.mult, ALU.add), reads=[r_mvk, r_lngb], writes=[r_buf])

    def load_lngb(l, sub):
        lngb = lngb_h[0]
        S.dma("sp", DMA(lngb[:, 0, :], ln_g[l, sub:sub + 1, :].partition_broadcast(128)), writes=[r_lngb])
        S.dma("sp", DMA(lngb[:, 1, :], ln_b[l, sub:sub + 1, :].partition_broadcast(128)), writes=[r_lngb])

    evac_rr = {"i": 0}

    def evac(out_ap, in_ap, reads, writes, scale=None):
        evac_rr["i"] += 1
        if evac_rr["i"] % 2:
            S.op("act", ACT(out_ap, in_ap, AF.Identity, scale=scale), reads=reads, writes=writes)
        elif scale is None:
            S.op("dve", CP(out_ap, in_ap), reads=reads, writes=writes)
        else:
            S.op("dve", TS(out_ap, in_ap, float(scale), ALU.mult), reads=reads, writes=writes)

    for b in range(NSEQ):
        with ExitStack() as st:
            c_sb = sb("c_sb", [128, 8], F32, st)
            scf = sb("scf", [128, 8], F32, st)
            scT = sb("scT", [128, 8], BF16, st)
            scB = sb("scB", [128, 8, 128], BF16, st)
            baT = sb("baT", [128, 2, 48], F32, st)
            bB = sb("bB", [128, 2, 2, D], F32, st)
            wa = [sb(f"wa{i}", [128, 8, 512], BF16, st) for i in range(2)]
            r_m = R("m")
            r_wa = [R("wa0"), R("wa1")]
            S.dma("sp", DMA(c_sb[:], cT[b]), writes=[r_m])
            S.dma("sp", DMA(baT[:], b_adaT.rearrange("l p j -> p l j")), writes=[r_m])
            for l in range(2):
                for gi, c0 in enumerate((2048, 5120)):
                    S.dma("sp", DMA(bB[:, l, gi, :], b_ada[l:l + 1, c0:c0 + D].partition_broadcast(128)), writes=[r_m])
            S.op("act", ACT(scf[:], c_sb[:], AF.Silu), reads=[r_m], writes=[r_m])
            S.op("dve", CP(scT[:], scf[:]), reads=[r_m], writes=[r_m])
            S.op("dve", CP(scB[:], bc_last(scT[:], 128)), reads=[r_m], writes=[r_m])
            for l in range(depth):
                wav = w_ada[l].rearrange("(kc p) n -> p kc n", p=128)
                for ch in range(12):
                    i = ch % 2
                    S.dma("pool", DMA(wa[i][:], wav[:, :, ch * 512:(ch + 1) * 512]), writes=[r_wa[i]])
                    bk, rbk = nb()
                    fns = []
                    for jj in range(4):
                        for kc in range(8):
                            fns.append(MM(bk[:, jj:jj + 1], wa[i][:, kc, jj * 128:(jj + 1) * 128], scT[:, kc:kc + 1], kc == 0, kc == 7))
                    S.pe(fns, reads=[r_wa[i], r_m], writes=[rbk])
                    S.op("dve", TT(modT[:, l, ch * 4:(ch + 1) * 4], bk[:, 0:4], baT[:, l, ch * 4:(ch + 1) * 4], ALU.add),
                         reads=[rbk, r_m], writes=[r_modT])
                    if ch in (4, 5, 10, 11):
                        gi = 0 if ch < 6 else 1
                        half = ch % 2
                        bk2, rbk2 = nb()
                        S.pe([MM(bk2[:], scB[:, kc, :], wa[i][:, kc, :], kc == 0, kc == 7) for kc in range(8)],
                             reads=[r_wa[i], r_m], writes=[rbk2])
                        S.op("dve", TT(gB[:, l, gi, half * 512:(half + 1) * 512], bk2[:], bB[:, l, gi, half * 512:(half + 1) * 512], ALU.add),
                             reads=[rbk2, r_m], writes=[r_gB])
                S.op("dve", TS(modT[:, l, 8:16], modT[:, l, 8:16], 1.0, ALU.add), reads=[r_modT], writes=[r_modT])
                S.op("dve", TS(modT[:, l, 32:40], modT[:, l, 32:40], 1.0, ALU.add), reads=[r_modT], writes=[r_modT])
            S.barrier()

        for l in range(depth):
            S.mark(f"b{b}l{l} LN1")
            x_src = x[b] if l == 0 else xs[b]
            r_xsrc = None if l == 0 else r_xs
            w_in_v = w_in[l].rearrange("(kc p) n -> p kc n", p=128)

            with ExitStack() as st:
                alloc_io(st)
                for g4 in range(4):
                    for i in range(4):
                        tb = g4 * 4 + i
                        S.dma("sp", DMA(xt[i][:], x_src[tb * 128:(tb + 1) * 128, :]), reads=[r_xsrc], writes=[r_xt[i]])
                    ln_stats_group([(xt[i][:], r_xt[i], g4 * 4 + i) for i in range(4)])
                    for i in range(4):
                        tb = g4 * 4 + i
                        to_hT(xt[i][:], r_xt[i], tb, tb, l, 8, 0)
                S.barrier()

            oT = regB[:].rearrange("p (b c t) -> p b c t", b=4, c=2)
            r_oT = r_regB

            with ExitStack() as st:
                fmw = sb("fmw", [128, 8, SEQ], BF16, st)
                wmix = sb("wmix", [128, 8, 1024], BF16, st)
                vtm = sb("vtm", [128, NTB, 4, 65], BF16, st)
                ebt = [sb(f"ebt{i}", [128, 23, 128], BF16, st) for i in range(2)]
                o_tok = sb("o_tok", [128, NTB, 256], BF16, st)
                gts = sb("gts", [128, NTB, 12], F32, st)
                sinkE = sb("sinkE", [128, 4], F32, st)
                kcT = sb("kcT", [128, 128], BF16, st)
                vcA = sb("vcA", [128, 65], BF16, st)
                w1t = sb("w1t", [64, 32, 64], BF16, st)
                peT = sb("peT", [64, 32], BF16, st)
                w2d = sb("w2d", [64, 128], BF16, st)
                cvec = sb("cvec", [64, 1], F32, st)
                gu = sb("gu", [64, 4, 128], F32, st)
                gTt = sb("gTt", [64, 128], BF16, st)
                psel = sb("psel", [128, 8, 128], F32, st)
                pselb = sb("pselb", [128, 8, 128], BF16, st)
                scs = sb("scs", [128, 3, 32], F32, st)
                m8 = sb("m8", [128, 2, 8], F32, st)
                unsel = sb("unsel", [128, 32], BF16, st)
                unselT = sb("unselT", [32, SEQ // 2], BF16, st)
                Rsb = sb("Rsb", [128, 128], BF16, st)
                r_fm = [R(f"fm{i}") for i in range(8)]
                r_wmix = R("wmix")
                r_vtm = R("vtm")
                r_ebt = [R("ebt0"), R("ebt1")]
                r_otok = R("otok")
                r_oacc = R("oacc")
                r_gts = R("gts")
                r_cmp = R("cmp")
                r_sel = R("sel")
                r_Rsb = R("Rsb")
                ebc = {"i": 0}

                S.op("pool", MSET(vtm[:], 1.0), writes=[r_vtm])
                S.mark(f"b{b}l{l} NSA-proj")

                def load_w(segs):
                    for (dc_, sc_, n_) in segs:
                        S.dma("pool", DMA(wmix[:, :, dc_:dc_ + n_], w_in_v[:, :, sc_:sc_ + n_]), writes=[r_wmix])

                def proj_fm(ti, c0, m, scale=None):
                    for T in range(4):
                        bk, rbk = nb()
                        S.pe([MM(bk[0:m, :], wmix[:, kc, c0:c0 + m], hT[:, kc, T * 512:(T + 1) * 512], kc == 0, kc == 7) for kc in range(8)],
                             reads=[r_wmix, r_hT], writes=[rbk])
                        evac(fmw[0:m, ti, T * 512:(T + 1) * 512], bk[0:m, :], [rbk], [r_fm[ti]], scale=scale)

                def proj_tm(c0, n, sinks):
                    for tb in range(NTB):
                        bk, rbk = nb()
                        S.pe([MM(bk[:, 0:n], hT[:, kc, tb * 128:(tb + 1) * 128], wmix[:, kc, c0:c0 + n], kc == 0, kc == 7) for kc in range(8)],
                             reads=[r_wmix, r_hT], writes=[rbk])
                        for fn in sinks:
                            fn(tb, bk, rbk)

                def v_sink(col, slot):
                    def f(tb, bk, rbk):
                        evac(vtm[:, tb, slot, 0:64], bk[:, col:col + 64], [rbk], [r_vtm])
                    return f

                def load_ebt(specs):
                    i = ebc["i"] % 2
                    ebc["i"] += 1
                    for (v_, hh_, ndl_, s0_) in specs:
                        S.dma("sp", DMA(ebt[i][:, s0_:s0_ + ndl_, :], toeplitz(v_, hh_, ndl_)), reads=[r_ebd], writes=[r_ebt[i]])
                    return ebt[i], r_ebt[i]

                def chunk(qap, r_q, ktile, ti_k, pb, j0, n, eb_ap, r_eb, v_of, acc, racc, first, last, penq=None):
                    bk, rbk = nb()
                    fns = []
                    for i in range(n):
                        j = j0 - i
                        sl = slice(i * 128, (i + 1) * 128)
                        kap = ktile[pb:pb + 64, j * 128:(j + 1) * 128]
                        if penq is not None:
                            fns.append(MM(bk[:, sl], kap, qap, True, False))
                            fns.append(MM(bk[:, sl], pen[0:32, j, :], penq, False, True))
                        else:
                            fns.append(MM(bk[:, sl], kap, qap, True, True))
                    rd = [r_fm[ti_k], r_q] + ([r_sel, r_const] if penq is not None else [])
                    S.pe(fns, reads=rd, writes=[rbk])
                    yield
                    E, rE = nE()
                    S.op("act", ACT(E[:, 0:n * 128], bk[:, 0:n * 128], AF.Exp), reads=[rbk], writes=[rE])
                    yield
                    S.op("dve", TT(E[:, 0:n * 128], E[:, 0:n * 128], eb_ap, ALU.mult), reads=[r_eb], writes=[rE])
                    yield
                    S.pe([MM(acc, E[:, i * 128:(i + 1) * 128], v_of(j0 - i), first and i == 0, last and i == n - 1) for i in range(n)],
                         reads=[rE, r_vtm, r_cmp], writes=[racc])
                    yield

                r_q_cur = [None]

                def flat(ap3):
                    return ap3.rearrange("p a b -> p (a b)")

                def to_oT(br):
                    for qb in range(NTB):
                        tp, rtp = nT()
                        S.pe([TR(tp[:, c * 128:(c + 1) * 128], o_tok[:, qb, c * 128:(c + 1) * 128], ident[:]) for c in range(2)],
                             reads=[r_otok, r_const], writes=[rtp])
                        evac(oT[:, br, :, qb * 128:(qb + 1) * 128], tp[:, 0:256].rearrange("p (c t) -> p c t", c=2), [rtp], [r_oT])

                WQ = 0
                load_w([(0, 0, 256), (256, 256, 64), (320, 256, 64), (384, 384, 64), (448, 384, 64), (512, 512, 64), (576, 512, 64),
                        (640, 320, 64), (704, 448, 64), (768, 576, 64), (832, 640, 12)])
                proj_fm(0, 0, 128, 0.125)
                proj_fm(1, 128, 128, 0.125)
                proj_fm(2, 256, 128)
                proj_fm(3, 384, 128)
                proj_fm(4, 512, 128)
                proj_fm(5, 640, 64)

                def gate_sink(tb, bk, rbk):
                    S.op("act", ACT(gts[:, tb, :], bk[:, 128:140], AF.Sigmoid), reads=[rbk], writes=[r_gts])
                proj_tm(704, 140, [v_sink(0, 0), v_sink(64, 1), gate_sink])

                S.mark(f"b{b}l{l} NSA-compress")
                S.op("dve", MSET(kcT[:], 0.0), writes=[r_cmp])
                S.op("dve", MSET(vcA[:], 0.0), writes=[r_cmp])
                S.op("dve", MSET(vcA[:, 64:65], 1.0), writes=[r_cmp])
                for which in range(2):
                    src_t = 2 if which == 0 else 5
                    S.dma("pool", DMA(w1t[:], cmp_w1[l, which].rearrange("(pos d) o -> d pos o", d=64)), writes=[r_cmp])
                    S.dma("pool", DMA(peT[:], cmp_peT[l, which]), writes=[r_cmp])
                    S.dma("pool", DMA(w2d[:, 0:64], cmp_w2[l, which]), writes=[r_cmp])
                    S.dma("pool", DMA(w2d[:, 64:128], cmp_w2[l, which]), writes=[r_cmp])
                    bk, rbk = nb()
                    S.pe([MM(bk[0:64, 0:1], w1t[:, pos, :], peT[:, pos:pos + 1], pos == 0, pos == 31) for pos in range(32)],
                         reads=[r_cmp], writes=[rbk])
                    S.op("dve", CP(cvec[:], bk[0:64, 0:1]), reads=[rbk], writes=[r_cmp])
                    bk, rbk = nb()
                    fns = []
                    for pos in range(32):
                        base = fmw[0:64, src_t, pos:pos + 1]
                        src = AP(base.tensor, base.offset, [list(base.ap[0]), [16, 127]])
                        fns.append(MM(bk[0:64, 0:127], w1t[:, pos, :], src, pos == 0, pos == 31))
                    S.pe(fns, reads=[r_cmp, r_fm[src_t]], writes=[rbk])
                    u = gu[:, 0, 0:127]
                    t1 = gu[:, 1, 0:127]
                    t2 = gu[:, 2, 0:127]
                    S.op("act", ACT(u, bk[0:64, 0:127], AF.Identity, bias=cvec[:, 0:1]), reads=[rbk, r_cmp], writes=[r_cmp])
                    S.op("dve", TT(t1, u, u, ALU.mult), reads=[r_cmp], writes=[r_cmp])
                    S.op("dve", TS(t1, t1, 0.044715, ALU.mult, 1.0, ALU.add), reads=[r_cmp], writes=[r_cmp])
                    S.op("dve", TT(t1, t1, u, ALU.mult), reads=[r_cmp], writes=[r_cmp])
                    S.op("act", ACT(t2, t1, AF.Sigmoid, scale=1.5957691216057308), reads=[r_cmp], writes=[r_cmp])
                    S.op("dve", TT(gTt[:, 0:127], u, t2, ALU.mult), reads=[r_cmp], writes=[r_cmp])
                    bk, rbk = nb()
                    if which == 0:
                        S.pe([MM(bk[:, 0:127], w2d[:, :], gTt[:, 0:127], True, True)], reads=[r_cmp], writes=[rbk])
                        S.op("dve", CP(kcT[:, 0:127], bk[:, 0:127]), reads=[rbk], writes=[r_cmp])
                    else:
                        S.pe([MM(bk[0:127, 0:64], gTt[:, 0:127], w2d[:, 0:64], True, True)], reads=[r_cmp], writes=[rbk])
                        S.op("dve", CP(vcA[0:127, 0:64], bk[0:127, 0:64]), reads=[rbk], writes=[r_cmp])

                S.mark(f"b{b}l{l} NSA-cmp")
                for h in range(4):
                    qt = h // 2
                    pb = 64 * (h % 2)
                    for T in range(4):
                        bk, rbk = nb()
                        S.pe([MM(bk[:], kcT[pb:pb + 64, :], fmw[pb:pb + 64, qt, T * 512:(T + 1) * 512], True, True)],
                             reads=[r_cmp, r_fm[qt]], writes=[rbk])
                        E, rE = nE()
                        S.op("act", ACT(E[:], bk[:], AF.Exp), reads=[rbk], writes=[rE])
                        S.op("dve", TT(E[:], E[:], cmpok[:, T * 512:(T + 1) * 512], ALU.mult), reads=[r_const], writes=[rE])
                        acc, racc = nb()
                        S.pe([MM(acc[:, i * 65:(i + 1) * 65], E[:, i * 128:(i + 1) * 128], vcA[:, :], True, True) for i in range(4)],
                             reads=[rE, r_cmp], writes=[racc])
                        a3 = acc[:, 0:260].rearrange("p (i c) -> p i c", c=65)
                        rs4 = sm[:, 0:4]
                        S.op("dve", TS(rs4, a3[:, :, 64], 1e-30, ALU.max), reads=[racc], writes=[r_sm])
                        S.op("dve", lambda e, rs4=rs4: e.reciprocal(out=rs4, in_=rs4), reads=[r_sm], writes=[r_sm])
                        S.op("dve", TT(rs4, rs4, gts[:, T * 4:(T + 1) * 4, h * 3], ALU.mult), reads=[r_gts], writes=[r_sm])
                        S.op("dve", TT(o_tok[:, T * 4:(T + 1) * 4, h * 64:(h + 1) * 64], a3[:, :, 0:64], bc_last(rs4, 64), ALU.mult),
                             reads=[racc, r_sm], writes=[r_otok])
                        if T >= 2:
                            dB, rdB = nb()
                            S.pe([MM(dB[:], onesb[:], E[:], True, True)], reads=[rE, r_const], writes=[rdB])
                            Fd, rFd = nF()
                            S.op("dve", TS(Fd[:], dB[:], 1e-30, ALU.max), reads=[rdB], writes=[rFd])
                            S.op("dve", lambda e, Fd=Fd: e.reciprocal(out=Fd[:], in_=Fd[:]), reads=[rFd], writes=[rFd])
                            pv = flat(psel[:, (T - 2) * 4:(T - 1) * 4, :])
                            if h == 0:
                                S.op("dve", TT(pv, E[:], Fd[:], ALU.mult), reads=[rE, rFd], writes=[r_sel])
                            else:
                                S.op("dve", TT(Fd[:], E[:], Fd[:], ALU.mult), reads=[rE], writes=[rFd])
                                S.op("pool", TT(pv, pv, Fd[:], ALU.add), reads=[rFd], writes=[r_sel])
                S.mark(f"b{b}l{l} NSA-select")
                S.op("dve", CP(flat(pselb[:]), flat(psel[:])), reads=[r_sel], writes=[r_sel])
                for qb in range(8, 16):
                    bk, rbk = nb()
                    S.pe([MM(bk[:, 0:32], pselb[:, qb - 8, :], overlap[:], True, True)], reads=[r_sel, r_const], writes=[rbk])
                    sc0 = scs[:, 0, :]
                    sc1 = scs[:, 1, :]
                    S.op("dve", TT(sc0, bk[:, 0:32], selkeep[:, qb - 8, :], ALU.mult), reads=[rbk, r_const], writes=[r_sel])
                    S.op("dve", TT(sc0, sc0, seladd[:, qb - 8, :], ALU.add), reads=[r_const], writes=[r_sel])
                    S.op("dve", lambda e, sc0=sc0: e.max(out=m8[:, 0, :], in_=sc0), reads=[r_sel], writes=[r_sel])
                    S.op("dve", lambda e, sc0=sc0, sc1=sc1: e.match_replace(out=sc1, in_to_replace=m8[:, 0, :], in_values=sc0, imm_value=-3.0e38),
                         reads=[r_sel], writes=[r_sel])
                    S.op("dve", lambda e, sc1=sc1: e.max(out=m8[:, 1, :], in_=sc1), reads=[r_sel], writes=[r_sel])
                    S.op("dve", TS(unsel[:], sc0, m8[:, 1, 7:8], ALU.is_lt), reads=[r_sel], writes=[r_sel])
                    tp, rtp = nT()
                    S.pe([TR(tp[0:32, 0:128], unsel[:], ident[:])], reads=[r_sel, r_const], writes=[rtp])
                    S.op("dve", CP(unselT[:, (qb - 8) * 128:(qb - 7) * 128], tp[0:32, 0:128]), reads=[rtp], writes=[r_sel])
                S.mark(f"b{b}l{l} NSA-selwin")
                def nsa_unit(h, qb, bi, eb, reb, slot):
                    qt = h // 2
                    pb = 64 * (h % 2)
                    qap = fmw[pb:pb + 64, qt, qb * 128:(qb + 1) * 128]
                    ktile_i, vslot, slot0, nd = ((3, 0, 5, qb + 1), (4, 1, 0, min(5, qb + 1)))[bi]
                    acc_b, racc = acc_of(slot)
                    acc = acc_b[:, 0:65]
                    penq = unselT[:, (qb - 8) * 128:(qb - 7) * 128] if (bi == 0 and qb >= 8) else None
                    d0 = 0
                    while d0 < nd:
                        n = min(4, nd - d0)
                        yield from chunk(qap, r_fm[qt], fmw[:, ktile_i, :], ktile_i, pb, qb - d0, n, flat(eb[:, slot0 + d0:slot0 + d0 + n, :]), reb,
                                         lambda j, vslot=vslot: vtm[:, j, vslot, :], acc, racc, d0 == 0, d0 + n >= nd, penq=penq)
                        d0 += n
                    yield
                    rs = sm[:, 8 + (qb % 4) * 2 + bi:9 + (qb % 4) * 2 + bi]
                    S.op("dve", lambda e, rs=rs, acc_b=acc_b: e.reciprocal(out=rs, in_=acc_b[:, 64:65]), reads=[racc], writes=[r_sm])
                    S.op("dve", TT(rs, rs, gts[:, qb, h * 3 + 1 + bi:h * 3 + 2 + bi], ALU.mult), reads=[r_gts], writes=[r_sm])
                    osl = o_tok[:, qb, h * 64:(h + 1) * 64]
                    S.op("dve", STT(osl, acc_b[:, 0:64], rs, osl, ALU.mult, ALU.add), reads=[racc, r_sm], writes=[r_otok])

                set_pools(4)
                def nsa_all():
                    for h in range(4):
                        eb, reb = load_ebt([(0, h, 5, 0), (1, h, 16, 5)])
                        for qb in range(NTB):
                            for bi in range(2):
                                yield (lambda slot, h=h, qb=qb, bi=bi, eb=eb, reb=reb: nsa_unit(h, qb, bi, eb, reb, slot))
                interleave(nsa_all(), 4)
                to_oT(0)

                S.mark(f"b{b}l{l} SWA")
                load_w([(0, 652, 256), (256, 908, 64), (320, 908, 64), (384, 972, 64), (448, 972, 64), (512, 1036, 128)])
                proj_fm(0, 0, 128, 0.125)
                proj_fm(1, 128, 128, 0.125)
                proj_fm(2, 256, 128)
                proj_fm(3, 384, 128)
                proj_tm(512, 128, [v_sink(0, 0), v_sink(64, 1)])
                S.dma("sp", DMA(sinkE[:], swa_sinks[l:l + 1, :].partition_broadcast(128)), writes=[r_cmp])
                S.op("act", ACT(sinkE[:], sinkE[:], AF.Exp), reads=[r_cmp], writes=[r_cmp])
                def swa_unit(h, qb, eb, reb, slot):
                    qt = h // 2
                    pb = 64 * (h % 2)
                    g = h // 2
                    qap = fmw[pb:pb + 64, qt, qb * 128:(qb + 1) * 128]
                    acc_b, racc = acc_of(slot)
                    nd = min(2, qb + 1)
                    yield from chunk(qap, r_fm[qt], fmw[:, 2 + g, :], 2 + g, pb, qb, nd, flat(eb[:, 0:nd, :]), reb,
                                     lambda j, g=g: vtm[:, j, g, :], acc_b[:, 0:65], racc, True, True)
                    yield
                    rs = sm[:, 8 + (qb % 4):9 + (qb % 4)]
                    S.op("dve", TT(rs, acc_b[:, 64:65], sinkE[:, h:h + 1], ALU.add), reads=[racc, r_cmp], writes=[r_sm])
                    S.op("dve", lambda e, rs=rs: e.reciprocal(out=rs, in_=rs), reads=[r_sm], writes=[r_sm])
                    S.op("dve", TS(o_tok[:, qb, h * 64:(h + 1) * 64], acc_b[:, 0:64], rs, ALU.mult), reads=[racc, r_sm], writes=[r_otok])

                def swa_all():
                    for h in range(4):
                        eb, reb = load_ebt([(2, h, 2, 0)])
                        for qb in range(NTB):
                            yield (lambda slot, h=h, qb=qb, eb=eb, reb=reb: swa_unit(h, qb, eb, reb, slot))
                interleave(swa_all(), 4)
                to_oT(1)

                S.mark(f"b{b}l{l} DIL")
                load_w([(0, 1164, 768), (768, 1932, 256)])
                for ti in range(8):
                    proj_fm(ti, ti * 128, 128, 0.125 if ti < 6 else None)
                load_w([(0, 2188, 256)])
                proj_tm(0, 256, [v_sink(0, 0), v_sink(64, 1), v_sink(128, 2), v_sink(192, 3)])
                def dil_unit(h, qb, eb, reb, slot):
                    pb = 64 * (h % 2)
                    kt = 6 + h // 2
                    acc_b, racc = acc_of(slot)
                    plan = []
                    for gi, (slot0, ndmax) in enumerate(((0, 2), (2, 5), (7, 16))):
                        nd = min(ndmax, qb + 1)
                        d0 = 0
                        while d0 < nd:
                            n = min(4, nd - d0)
                            plan.append((gi, slot0, d0, n))
                            d0 += n
                    for pi, (gi, slot0, d0, n) in enumerate(plan):
                        qt = gi * 2 + h // 2
                        qap = fmw[pb:pb + 64, qt, qb * 128:(qb + 1) * 128]
                        yield from chunk(qap, r_fm[qt], fmw[:, kt, :], kt, pb, qb - d0, n, flat(eb[:, slot0 + d0:slot0 + d0 + n, :]), reb,
                                         lambda j, h=h: vtm[:, j, h, :], acc_b[:, 0:65], racc, pi == 0, pi == len(plan) - 1)
                    yield
                    rs = sm[:, 8 + (qb % 4):9 + (qb % 4)]
                    S.op("dve", lambda e, rs=rs, acc_b=acc_b: e.reciprocal(out=rs, in_=acc_b[:, 64:65]), reads=[racc], writes=[r_sm])
                    S.op("dve", TS(o_tok[:, qb, h * 64:(h + 1) * 64], acc_b[:, 0:64], rs, ALU.mult), reads=[racc, r_sm], writes=[r_otok])

                def dil_all():
                    for h in range(4):
                        eb, reb = load_ebt([(3, h, 2, 0), (4, h, 5, 2), (5, h, 16, 7)])
                        for qb in range(NTB):
                            yield (lambda slot, h=h, qb=qb, eb=eb, reb=reb: dil_unit(h, qb, eb, reb, slot))
                interleave(dil_all(), 4)
                to_oT(2)

                S.mark(f"b{b}l{l} SB")
                load_w([(0, 2444, 768)])
                for ti in range(4):
                    proj_fm(ti, ti * 128, 128, 0.125 if ti < 2 else None)
                proj_tm(512, 256, [v_sink(0, 0), v_sink(64, 1), v_sink(128, 2), v_sink(192, 3)])
                def sb_unit(h, qb, slot):
                    qt = h // 2
                    kt = 2 + h // 2
                    pb = 64 * (h % 2)
                    qap = fmw[pb:pb + 64, qt, qb * 128:(qb + 1) * 128]
                    acc_b, racc = acc_of(slot)
                    Rsb, r_Rsb = Rsbs[slot], r_Rsbs[slot]
                    nd = qb + 1
                    d0 = 0
                    while d0 < nd:
                        n = min(4, nd - d0)
                        W = n * 128
                        zb, rzb = nb()
                        S.pe([MM(zb[:, i * 128:(i + 1) * 128], fmw[pb:pb + 64, kt, (qb - d0 - i) * 128:(qb - d0 - i + 1) * 128], qap, True, True)
                              for i in range(n)], reads=[r_fm[kt], r_fm[qt]], writes=[rzb])
                        yield
                        Fe, rFe = nF()
                        S.op("act", ACT(Fe[:, 0:W], zb[:, 0:W], AF.Exp), reads=[rzb], writes=[rFe])
                        yield
                        Lb, rLb = nE()
                        S.op("act", ACT(Lb[:, 0:W], Fe[:, 0:W], AF.Ln, bias=1.0), reads=[rFe], writes=[rLb])
                        yield
                        if d0 == 0:
                            S.op("dve", TT(Lb[:, 0:128], Lb[:, 0:128], sbmask[:], ALU.mult), reads=[r_const], writes=[rLb])
                        F1, rF1 = nF()
                        S.op("dve", TT(F1[:, 0:W], zb[:, 0:W], Lb[:, 0:W], ALU.subtract), reads=[rzb, rLb], writes=[rF1])
                        yield
                        cb, rcb = nb()
                        fns = []
                        for i in range(n):
                            sl = slice(i * 128, (i + 1) * 128)
                            terms = [(negU[:], Lb[:, sl])] + [(negOnes[:], Lb[:, i2 * 128:(i2 + 1) * 128]) for i2 in range(i)]
                            if d0 > 0:
                                terms.append((ident[:], Rsb[:]))
                            for ti_, (lt, rh) in enumerate(terms):
                                fns.append(MM(cb[:, sl], lt, rh, ti_ == 0, ti_ == len(terms) - 1))
                        S.pe(fns, reads=[rLb, r_const, r_Rsb], writes=[rcb])
                        if d0 + n < nd:
                            rbk_, rrb = nb()
                            terms = [(negOnes[:], Lb[:, i * 128:(i + 1) * 128]) for i in range(n)]
                            if d0 > 0:
                                terms.append((ident[:], Rsb[:]))
                            S.pe([MM(rbk_[:, 0:128], lt, rh, ti_ == 0, ti_ == len(terms) - 1) for ti_, (lt, rh) in enumerate(terms)],
                                 reads=[rLb, r_const, r_Rsb], writes=[rrb])
                            yield
                            S.op("dve", CP(Rsb[:], rbk_[:, 0:128]), reads=[rrb], writes=[r_Rsb])
                        yield
                        S.op("dve", TT(F1[:, 0:W], F1[:, 0:W], cb[:, 0:W], ALU.add), reads=[rcb], writes=[rF1])
                        yield
                        A, rA = nE()
                        S.op("act", ACT(A[:, 0:W], F1[:, 0:W], AF.Exp), reads=[rF1], writes=[rA])
                        yield
                        if d0 == 0:
                            S.op("dve", TT(A[:, 0:128], A[:, 0:128], sbmask[:], ALU.mult), reads=[r_const], writes=[rA])
                        S.pe([MM(acc_b[:, 0:64], A[:, i * 128:(i + 1) * 128], vtm[:, qb - d0 - i, h, 0:64], d0 == 0 and i == 0,
                                 d0 + n >= nd and i == n - 1) for i in range(n)], reads=[rA, r_vtm], writes=[racc])
                        yield
                        d0 += n
                    yield
                    evac(o_tok[:, qb, h * 64:(h + 1) * 64], acc_b[:, 0:64], [racc], [r_otok])

                set_pools(6)
                SBW = 2
                Rsbs = [sb(f"Rsb{i}", [128, 128], BF16, st) for i in range(SBW)]
                r_Rsbs = [R(f"Rsb{i}") for i in range(SBW)]
                units = []
                for h in range(4):
                    for qb in range(NTB):
                        units.append(lambda slot, h=h, qb=qb: sb_unit(h, qb, slot))
                interleave(units, SBW)
                set_pools(6)
                to_oT(3)
                S.barrier()
                if dbg and b == 0 and l == 0:
                    S.dma("sp", DMA(dbg_hT, hT[:].rearrange("p a b -> p (a b)")), reads=[r_hT])
                    S.dma("sp", DMA(dbg_oT, regB[:]), reads=[r_regB])
                    S.dma("sp", DMA(dbg_mod, modT[:].rearrange("p a b -> p (a b)")), reads=[r_modT])
                    S.dma("sp", DMA(dbg_gB, gB[:].rearrange("p a b c -> p (a b c)")), reads=[r_gB])
                    S.barrier()

            S.mark(f"b{b}l{l} merge")
            with ExitStack() as st:
                set_pools(8)
                wout = sb("wout", [128, 8, D], BF16, st)
                wbr = sb("wbr", [128, 4, 2, D], BF16, st)
                mergedT = sb("mergedT", [128, 8, SEQ], BF16, st)
                wgt = [sb(f"wgt{i}", [128, 8, 4, 128], BF16, st) for i in range(2)]
                maccs = [sb(f"macc{i}", [128, 512], F32, st) for i in range(2)]
                r_maccs = [R("macc0"), R("macc1")]
                mi = 0
                alloc_io(st, want_xn=False, want_lngb=True, n_xt=2)
                r_w = R("w")
                r_wgt = [R("wgt0"), R("wgt1")]
                r_mg = R("mg")
                S.dma("pool", DMA(wout[:], w_out[l].rearrange("(kc p) n -> p kc n", p=128)), writes=[r_w])
                S.op("pool", TT(wout[:], wout[:], bc_mid(gB[:, l, 0, :], 8), ALU.mult), reads=[r_gB], writes=[r_w])
                S.dma("pool", DMA(wbr[:], w_branch[l].rearrange("b (c p) n -> p b c n", p=128)), writes=[r_w])
                load_lngb(l, 0)
                for dc in range(8):
                    i = dc % 2
                    for br in range(4):
                        c0 = 3212 + br * D + dc * 128
                        S.dma("pool", DMA(wgt[i][:, :, br, :], w_in_v[:, :, c0:c0 + 128]), writes=[r_wgt[i]])
                    for T in range(4):
                        ts_ = slice(T * 512, (T + 1) * 512)
                        macc = maccs[mi % 2]
                        r_macc = r_maccs[mi % 2]
                        mi += 1
                        for br in range(4):
                            bG, rbG = nb()
                            S.pe([MM(bG[:], wgt[i][:, kc, br, :], hT[:, kc, ts_], kc == 0, kc == 7) for kc in range(8)],
                                 reads=[r_wgt[i], r_hT], writes=[rbG])
                            bP, rbP = nb()
                            S.pe([MM(bP[:], wbr[:, br, c, dc * 128:(dc + 1) * 128], oT[:, br, c, ts_], c == 0, c == 1) for c in range(2)],
                                 reads=[r_w, r_oT], writes=[rbP])
                            Fg, rFg = nF()
                            S.op("act", ACT(Fg[:], bG[:], AF.Sigmoid), reads=[rbG], writes=[rFg])
                            if br == 0:
                                S.op("dve", TT(macc[:], Fg[:], bP[:], ALU.mult), reads=[rFg, rbP], writes=[r_macc])
                            else:
                                S.op("dve", TT(Fg[:], Fg[:], bP[:], ALU.mult), reads=[rbP], writes=[rFg])
                                if br < 3:
                                    S.op("pool", TT(macc[:], macc[:], Fg[:], ALU.add), reads=[rFg], writes=[r_macc])
                                else:
                                    S.op("pool", TT(mergedT[:, dc, ts_], macc[:], Fg[:], ALU.add), reads=[rFg, r_macc], writes=[r_mg])
                S.mark(f"b{b}l{l} wout+LN")
                for g4 in range(8):
                    for i in range(2):
                        tb = g4 * 2 + i
                        S.dma("sp", DMA(xt[i][:], x_src[tb * 128:(tb + 1) * 128, :]), reads=[r_xsrc], writes=[r_xt[i]])
                    for i in range(2):
                        tb = g4 * 2 + i
                        for dh in range(2):
                            bk, rbk = nb()
                            S.pe([MM(bk[:], mergedT[:, kc, tb * 128:(tb + 1) * 128], wout[:, kc, dh * 512:(dh + 1) * 512], kc == 0, kc == 7)
                                  for kc in range(8)], reads=[r_mg, r_w], writes=[rbk])
                            xs_ = xt[i][:, dh * 512:(dh + 1) * 512]
                            S.op("dve", STT(xs_, xs_, float(ALPHA), bk[:], ALU.mult, ALU.add), reads=[rbk], writes=[r_xt[i]])
                    ln_stats_group([(xt[i][:], r_xt[i], g4 * 2 + i) for i in range(2)])
                    for i in range(2):
                        tb = g4 * 2 + i
                        affine_k(xt[i][:], r_xt[i], tb)
                        S.dma("sp", DMA(xm[tb * 128:(tb + 1) * 128, :], xt[i][:]), reads=[r_xt[i]], writes=[r_xm])
                        if dbg and b == 0 and l == 0:
                            S.dma("sp", DMA(dbg_xm[tb * 128:(tb + 1) * 128, :], xt[i][:]), reads=[r_xt[i]])
                S.barrier()

            S.mark(f"b{b}l{l} LN2")
            with ExitStack() as st:
                rr = sb("rr", [128, NTB, D], F32, st)
                alloc_io(st, want_xt=False, want_lngb=True)
                aT = [sb(f"aT{i}", [128, 2, SEQ], BF16, st) for i in range(2)]
                gate_sb = sb("gate_sb", [128, NTB, 8], F32, st)
                wr = sb("wr", [128, 8, 8], BF16, st)
                lg = sb("lg", [128, 8], F32, st)
                mx = sb("mx", [128, 8], F32, st)
                r_rr = [R(f"rr{i}") for i in range(NTB)]
                r_aT = [R("aT0"), R("aT1")]
                r_gate = R("gate")
                r_wr = R("wr")
                wg = [regB[:, i * 2048:(i + 1) * 2048].rearrange("p (k f) -> p k f", k=8) for i in range(2)]
                wu = [regB[:, 4096 + i * 2048:4096 + (i + 1) * 2048].rearrange("p (k f) -> p k f", k=8) for i in range(2)]
                wd = [regB[:, 8192 + i * 2048:8192 + (i + 1) * 2048].rearrange("p (c d) -> p c d", c=2) for i in range(2)]
                r_wg = [R("wg0"), R("wg1")]
                r_wd = [R("wd0"), R("wd1")]
                for tb in range(NTB):
                    S.dma("sp", DMA(rr[:, tb, :], xm[tb * 128:(tb + 1) * 128, :]), reads=[r_xm], writes=[r_rr[tb]])
                for g4 in range(4):
                    ln_stats_group([(rr[:, g4 * 4 + i, :], r_rr[g4 * 4 + i], g4 * 4 + i) for i in range(4)])
                    for i in range(4):
                        tb = g4 * 4 + i
                        to_hT(rr[:, tb, :], r_rr[tb], tb, tb, l, 32, 24)
                        S.op("pool", TS(rr[:, tb, :], rr[:, tb, :], float(ALPHA), ALU.mult), writes=[r_rr[tb]])
                load_lngb(l, 1)
                S.mark(f"b{b}l{l} FFN")
                moe = (l % 2 == 1)
                if moe:
                    S.dma("pool", DMA(wr[:], moe_router[0].rearrange("(kc p) e -> p kc e", p=128)), writes=[r_wr])
                    for tb in range(NTB):
                        bk, rbk = nb()
                        S.pe([MM(bk[:, 0:8], hT[:, kc, tb * 128:(tb + 1) * 128], wr[:, kc, :], kc == 0, kc == 7) for kc in range(8)],
                             reads=[r_hT, r_wr], writes=[rbk])
                        S.op("dve", CP(lg[:], bk[:, 0:8]), reads=[rbk], writes=[r_gate])
                        S.op("dve", lambda e: e.max(out=mx[:], in_=lg[:]), reads=[r_gate], writes=[r_gate])
                        gsl = gate_sb[:, tb, :]
                        S.op("dve", TS(gsl, lg[:], mx[:, 1:2], ALU.is_ge), reads=[r_gate], writes=[r_gate])
                        S.op("dve", TS(mx[:, 2:3], mx[:, 0:1], -1.0, ALU.mult), reads=[r_gate], writes=[r_gate])
                        S.op("act", ACT(lg[:], lg[:], AF.Exp, bias=mx[:, 2:3]), reads=[r_gate], writes=[r_gate])
                        S.op("dve", TT(gsl, gsl, lg[:], ALU.mult), reads=[r_gate], writes=[r_gate])
                        S.op("dve", lambda e, gsl=gsl: e.reduce_sum(out=mx[:, 3:4], in_=gsl, axis=mybir.AxisListType.X), reads=[r_gate], writes=[r_gate])
                        S.op("dve", lambda e: e.reciprocal(out=mx[:, 3:4], in_=mx[:, 3:4]), reads=[r_gate], writes=[r_gate])
                        S.op("dve", TS(gsl, gsl, mx[:, 3:4], ALU.mult), reads=[r_gate], writes=[r_gate])
                    experts = [(moe_w_gate[0, e], moe_w_up[0, e], moe_w_down[0, e], FF_EXP, e) for e in range(8)]
                else:
                    experts = [(ffn_w_gate[0], ffn_w_up[0], ffn_w_down[0], FF_DENSE, None)]
                it = 0
                for (Wg, Wu, Wd, FF, eidx) in experts:
                    Wgv = Wg.rearrange("(kc p) f -> p kc f", p=128)
                    Wuv = Wu.rearrange("(kc p) f -> p kc f", p=128)
                    Wdv = Wd.rearrange("(c p) d -> p c d", p=128)
                    for fi in range(FF // 256):
                        i = it % 2
                        it += 1
                        f0 = fi * 256
                        S.dma("pool", DMA(wg[i], Wgv[:, :, f0:f0 + 256]), writes=[r_wg[i]])
                        S.dma("pool", DMA(wu[i], Wuv[:, :, f0:f0 + 256]), writes=[r_wg[i]])
                        S.dma("pool", DMA(wd[i], Wdv[:, fi * 2:fi * 2 + 2, :]), writes=[r_wd[i]])
                        S.op("pool", TT(wd[i], wd[i], bc_mid(gB[:, l, 1, :], 2), ALU.mult), reads=[r_gB], writes=[r_wd[i]])
                        for fc in range(2):
                            for T in range(4):
                                ts_ = slice(T * 512, (T + 1) * 512)
                                bG, rbG = nb()
                                S.pe([MM(bG[:], wg[i][:, kc, fc * 128:(fc + 1) * 128], hT[:, kc, ts_], kc == 0, kc == 7) for kc in range(8)],
                                     reads=[r_wg[i], r_hT], writes=[rbG])
                                bU, rbU = nb()
                                S.pe([MM(bU[:], wu[i][:, kc, fc * 128:(fc + 1) * 128], hT[:, kc, ts_], kc == 0, kc == 7) for kc in range(8)],
                                     reads=[r_wg[i], r_hT], writes=[rbU])
                                Fg, rFg = nF()
                                S.op("act", ACT(Fg[:], bG[:], AF.Silu), reads=[rbG], writes=[rFg])
                                S.op("dve", TT(aT[i][:, fc, ts_], Fg[:], bU[:], ALU.mult), reads=[rFg, rbU], writes=[r_aT[i]])
                        for tb in range(NTB):
                            for dh in range(2):
                                bk, rbk = nb()
                                S.pe([MM(bk[:], aT[i][:, fc, tb * 128:(tb + 1) * 128], wd[i][:, fc, dh * 512:(dh + 1) * 512], fc == 0, fc == 1)
                                      for fc in range(2)], reads=[r_aT[i], r_wd[i]], writes=[rbk])
                                rs_ = rr[:, tb, dh * 512:(dh + 1) * 512]
                                if eidx is None:
                                    S.op("dve", TT(rs_, rs_, bk[:], ALU.add), reads=[rbk], writes=[r_rr[tb]])
                                else:
                                    S.op("dve", STT(rs_, bk[:], gate_sb[:, tb, eidx:eidx + 1], rs_, ALU.mult, ALU.add),
                                         reads=[rbk, r_gate], writes=[r_rr[tb]])
                S.mark(f"b{b}l{l} finalLN")
                dst = out[b] if l == depth - 1 else xs[b]
                r_dst = r_out if l == depth - 1 else r_xs
                for g4 in range(4):
                    ln_stats_group([(rr[:, g4 * 4 + i, :], r_rr[g4 * 4 + i], g4 * 4 + i) for i in range(4)])
                    for i in range(4):
                        tb = g4 * 4 + i
                        affine_k(rr[:, tb, :], r_rr[tb], tb)
                        S.dma("sp", DMA(dst[tb * 128:(tb + 1) * 128, :], rr[:, tb, :]), reads=[r_rr[tb]], writes=[r_dst])
                S.barrier()

    S.barrier()
    S.mark("end")
    ES.close()
    S.close()
    nc._marks = S.marks
    return nc


_CACHE = {}


def _get_nc(nseq, depth):
    key = (nseq, depth)
    if key not in _CACHE:
        _CACHE[key] = build_nc(nseq, depth)
    return _CACHE[key]


def make_in_maps(inputs, ncores, nseq):
    consts = host_consts()
    f = lambda a: np.ascontiguousarray(np.asarray(a, dtype=np.float32))
    shared = {k: f(inputs[k]) for k in ("rel_bias", "w_ada", "b_ada", "w_in", "w_branch", "w_out", "cmp_w1", "cmp_w2", "swa_sinks",
                                        "ln_g", "ln_b", "ffn_w_gate", "ffn_w_up", "ffn_w_down", "moe_router", "moe_w_gate",
                                        "moe_w_up", "moe_w_down")}
    shared["b_adaT"] = f(np.asarray(inputs["b_ada"]).reshape(2, 48, 128).transpose(0, 2, 1))
    shared["cmp_peT"] = f(np.asarray(inputs["cmp_pe"]).transpose(0, 1, 3, 2))
    shared.update(consts)
    x = np.asarray(inputs["x"], dtype=np.float32)
    c = np.asarray(inputs["c"], dtype=np.float32)
    maps = []
    for i in range(ncores):
        m = dict(shared)
        m["x"] = np.ascontiguousarray(x[i * nseq:(i + 1) * nseq])
        cc = c[i * nseq:(i + 1) * nseq]
        m["cT"] = np.ascontiguousarray(cc.reshape(nseq, 8, 128).transpose(0, 2, 1))
        maps.append(m)
    return maps


def kernel(**inputs):
    nseq = 2
    nc = _get_nc(nseq, DEPTH)
    maps = make_in_maps(inputs, NCORES, nseq)
    res = run_bass_kernel_spmd(nc, maps, core_ids=list(range(NCORES)))
    outs = [np.asarray(r["out"], dtype=np.float32) for r in res.results]
    return np.concatenate(outs, axis=0)
```

```python
import math
from contextlib import ExitStack
import numpy as np
import concourse.bass as bass
import concourse.mybir as mybir
from concourse.ap import AP
from concourse.bass_utils import run_bass_kernel_spmd

F32 = mybir.dt.float32
BF16 = mybir.dt.bfloat16
AF = mybir.ActivationFunctionType
ALU = mybir.AluOpType

D = 1024
SEQ = 2048
NTB = 16
DEPTH = 2
NCORES = 8
NDV = 2304
ALPHA = (2 * DEPTH) ** 0.25
LN_EPS = 1e-5
D_IN = 7308
FF_DENSE = 2816
FF_EXP = 3584
EPOCH = 30000
NDMASEM = 8


class Res:
    __slots__ = ("name", "w", "r")

    def __init__(self, name=""):
        self.name = name
        self.w = None
        self.r = []


class Sched:
    def __init__(self, nc):
        self.nc = nc
        self.h = {"pe": nc.tensor, "act": nc.scalar, "dve": nc.vector, "pool": nc.gpsimd, "sp": nc.sync}
        self.names = list(self.h)
        self._ctx = []
        self.sems = {n: self._newsem(n + "_c0") for n in self.names}
        self.cnt = {n: 0 for n in self.names}
        self.epoch = {n: 0 for n in self.names}
        self.waited = {n: {} for n in self.names}
        self.last = {n: None for n in self.names}
        self.dq = ("sp", "act", "pool")
        self.dsems = {n: [self._newsem(f"{n}_d{i}") for i in range(NDMASEM)] for n in self.dq}
        self.dcnt = {n: 0 for n in self.dq}
        self.dlast = {n: [None] * NDMASEM for n in self.dq}
        self.nins = {}
        self.marks = []

    def _newsem(self, name):
        cm = self.nc.semaphore(name)
        s = cm.__enter__()
        self._ctx.append(cm)
        self._keep = getattr(self, "_keep", [])
        self._keep.append(s)
        return s

    def _deps(self, reads, writes):
        deps = []
        for r in reads:
            if r.w is not None:
                deps.append(r.w)
        for w in writes:
            if w.w is not None:
                deps.append(w.w)
            deps.extend(w.r)
        return deps

    def _commit(self, reads, writes, tok):
        for r in reads:
            r.r.append(tok)
            if len(r.r) > 64:
                best = {}
                for (s, v) in r.r:
                    if id(s) not in best or best[id(s)][1] < v:
                        best[id(s)] = (s, v)
                r.r = list(best.values())
        for w in writes:
            w.w = tok
            w.r = []

    def _emit(self, eng, deps, fn, inc):
        wd = self.waited[eng]
        best = {}
        for (s, v) in deps:
            k = id(s)
            if wd.get(k, 0) >= v:
                continue
            if k not in best or best[k][1] < v:
                best[k] = (s, v)
        e = self.h[eng]
        for k, (s, v) in best.items():
            wd[k] = v
            e.wait_ge(s, v)
        if fn is not None:
            ins = fn(e)
            self.nins[eng] = self.nins.get(eng, 0) + 1
            if inc is not None:
                ins.then_inc(inc[0], inc[1])

    def mark(self, name):
        self.marks.append((name, dict(self.nins)))

    def _tick(self, eng):
        if self.cnt[eng] >= EPOCH:
            self.epoch[eng] += 1
            self.sems[eng] = self._newsem(f"{eng}_c{self.epoch[eng]}")
            self.cnt[eng] = 0
        self.cnt[eng] += 1
        return (self.sems[eng], self.cnt[eng])

    def op(self, eng, fn, reads=(), writes=()):
        reads = [r for r in reads if r is not None]
        writes = [w for w in writes if w is not None]
        deps = self._deps(reads, writes)
        tok = self._tick(eng)
        self._emit(eng, deps, fn, (tok[0], 1))
        self._commit(reads, writes, tok)
        self.last[eng] = tok
        return tok

    def pe(self, fns, reads=(), writes=()):
        reads = [r for r in reads if r is not None]
        writes = [w for w in writes if w is not None]
        deps = self._deps(reads, writes)
        tok = self._tick("pe")
        n = len(fns)
        for i, fn in enumerate(fns):
            self._emit("pe", deps if i == 0 else [], fn, (tok[0], 1) if i == n - 1 else None)
        self._commit(reads, writes, tok)
        self.last["pe"] = tok
        return tok

    def dma(self, q, fn, reads=(), writes=()):
        reads = [r for r in reads if r is not None]
        writes = [w for w in writes if w is not None]
        deps = self._deps(reads, writes)
        m = self.dcnt[q]
        self.dcnt[q] += 1
        nsl = 3 if q == "sp" else NDMASEM
        slot = m % nsl
        sem = self.dsems[q][slot]
        val = 16 * (m // nsl + 1)
        if self.dlast[q][slot] is not None:
            deps.append(self.dlast[q][slot])
        tok = (sem, val)
        self.dlast[q][slot] = tok
        self._emit(q, deps, fn, (sem, 16))
        self._commit(reads, writes, tok)
        return tok

    def barrier(self):
        toks = [t for t in self.last.values() if t is not None]
        for q in self.dq:
            toks += [t for t in self.dlast[q] if t is not None]
        for n in self.names:
            self._emit(n, toks, None, None)

    def close(self):
        for cm in reversed(self._ctx):
            cm.__exit__(None, None, None)


def MM(out, lhsT, rhs, start=True, stop=True):
    return lambda e: e.matmul(out, lhsT=lhsT, rhs=rhs, start=start, stop=stop)


def TR(out, in_, ident):
    return lambda e: e.transpose(out, in_, ident)


def ACT(out, in_, func, bias=None, scale=None):
    kw = {}
    if bias is not None:
        kw["bias"] = bias
    if scale is not None:
        kw["scale"] = scale
    return lambda e: e.activation(out=out, in_=in_, func=func, **kw)


def TT(out, a, b, op):
    return lambda e: e.tensor_tensor(out=out, in0=a, in1=b, op=op)


def TS(out, a, s1, op0, s2=None, op1=None):
    if op1 is None:
        return lambda e: e.tensor_scalar(out=out, in0=a, scalar1=s1, scalar2=None, op0=op0)
    return lambda e: e.tensor_scalar(out=out, in0=a, scalar1=s1, scalar2=s2, op0=op0, op1=op1)


def STT(out, in0, scalar, in1, op0, op1):
    return lambda e: e.scalar_tensor_tensor(out=out, in0=in0, scalar=scalar, in1=in1, op0=op0, op1=op1)


def CP(out, in_):
    return lambda e: e.tensor_copy(out=out, in_=in_)


def DMA(out, in_):
    return lambda e: e.dma_start(out=out, in_=in_)


def MSET(ap, v):
    return lambda e: e.memset(ap, v)


def bc_last(ap2, m):
    a = ap2.ap
    return AP(ap2.tensor, ap2.offset, [list(a[0]), list(a[1]), [0, m]])


def bc_mid(ap2, m):
    a = ap2.ap
    return AP(ap2.tensor, ap2.offset, [list(a[0]), [0, m], list(a[1])])


def _t5_bucket(dist):
    dist = np.maximum(dist, 0)
    d_f = np.maximum(dist, 1).astype(np.float32)
    large = 16 + (np.log(d_f / np.float32(16)) / np.float32(math.log(2048 / 16)) * np.float32(16)).astype(np.int32)
    large = np.minimum(large, 31)
    return np.where(dist < 16, dist, large)


def host_consts():
    c = {}
    u = np.arange(NDV)
    d = u - 128
    onehot = np.zeros((32, NDV), np.float32)
    bk = _t5_bucket(d)
    onehot[bk[d >= 0], u[d >= 0]] = 1.0
    c["k_onehot"] = onehot
    vm = np.zeros((6, NDV), np.float32)
    vm[0] = (d >= 0) & (d <= 511)
    vm[1] = (d >= 0) & (d <= 2047)
    vm[2] = (d >= 0) & (d <= 127)
    vm[3] = (d >= 0) & (d <= 128)
    vm[4] = (d >= 0) & (d <= 512) & (d % 4 == 0)
    vm[5] = (d >= 0) & (d <= 2047) & (d % 16 == 0)
    c["k_vmask"] = np.ascontiguousarray(np.broadcast_to(vm[:, None, :], (6, 4, NDV))).astype(np.float32)
    c["k_ident"] = np.eye(128, dtype=np.float32)
    j = np.arange(128)[:, None]
    s = np.arange(128)[None, :]
    c["k_negU"] = -(j > s).astype(np.float32)
    c["k_sbmask"] = (j < s).astype(np.float32)
    ci = np.arange(128)[:, None]
    sj = np.arange(32)[None, :]
    ov = ((ci * 16 + 31 >= sj * 64) & (ci * 16 < (sj + 1) * 64) & (ci < 127)).astype(np.float32)
    c["k_overlap"] = ov
    t = np.arange(SEQ)[None, :]
    c["k_cmpok"] = ((ci * 16 + 31 <= t) & (ci < 127)).astype(np.float32)
    keep = np.zeros((128, 8, 32), np.float32)
    add = np.zeros((128, 8, 32), np.float32)
    for qb in range(8, 16):
        tt = qb * 128 + np.arange(128)
        cur = tt // 64
        for jj in range(32):
            fut = jj > cur
            f0 = np.full(128, jj == 0)
            f1 = jj == cur
            f2 = jj == cur - 1
            k = ~(fut | f0 | f1 | f2)
            a = np.where(f0, 3e4, np.where(f1, 2e4, np.where(f2, 1e4, np.where(fut, -1e30, 0.0))))
            keep[:, qb - 8, jj] = k
            add[:, qb - 8, jj] = a
    c["k_selkeep"] = keep
    c["k_seladd"] = add
    pen = np.zeros((32, 16, 128), np.float32)
    for J in range(16):
        for k in range(128):
            pen[2 * J + k // 64, J, k] = -30000.0
    c["k_pen"] = pen
    return c


def build_nc(NSEQ=2, depth=DEPTH, dbg=False):
    nc = bass.Bass("TRN2", target_bir_lowering=False)
    S = Sched(nc)
    ES = ExitStack()

    def din(name, shape, dt=F32):
        return nc.dram_tensor(name, list(shape), dt, kind="ExternalInput").ap()

    x = din("x", [NSEQ, SEQ, D])
    cT = din("cT", [NSEQ, 128, 8])
    rel_bias = din("rel_bias", [32, 20])
    w_ada = din("w_ada", [2, D, 6 * D])
    b_ada = din("b_ada", [2, 6 * D])
    b_adaT = din("b_adaT", [2, 128, 48])
    w_in = din("w_in", [2, D, D_IN])
    w_branch = din("w_branch", [2, 4, 256, D])
    w_out = din("w_out", [2, D, D])
    cmp_peT = din("cmp_peT", [2, 2, 64, 32])
    cmp_w1 = din("cmp_w1", [2, 2, 2048, 64])
    cmp_w2 = din("cmp_w2", [2, 2, 64, 64])
    swa_sinks = din("swa_sinks", [2, 4])
    ln_g = din("ln_g", [2, 2, D])
    ln_b = din("ln_b", [2, 2, D])
    ffn_w_gate = din("ffn_w_gate", [1, D, FF_DENSE])
    ffn_w_up = din("ffn_w_up", [1, D, FF_DENSE])
    ffn_w_down = din("ffn_w_down", [1, FF_DENSE, D])
    moe_router = din("moe_router", [1, D, 8])
    moe_w_gate = din("moe_w_gate", [1, 8, D, FF_EXP])
    moe_w_up = din("moe_w_up", [1, 8, D, FF_EXP])
    moe_w_down = din("moe_w_down", [1, 8, FF_EXP, D])
    k_onehot = din("k_onehot", [32, NDV])
    k_vmask = din("k_vmask", [6, 4, NDV])
    k_ident = din("k_ident", [128, 128])
    k_negU = din("k_negU", [128, 128])
    k_sbmask = din("k_sbmask", [128, 128])
    k_overlap = din("k_overlap", [128, 32])
    k_cmpok = din("k_cmpok", [128, SEQ])
    k_selkeep = din("k_selkeep", [128, 8, 32])
    k_seladd = din("k_seladd", [128, 8, 32])
    k_pen = din("k_pen", [32, 16, 128])
    out = nc.dram_tensor("out", [NSEQ, SEQ, D], F32, kind="ExternalOutput").ap()
    if dbg:
        dbg_hT = nc.dram_tensor("dbg_hT", [128, 8 * SEQ], BF16, kind="ExternalOutput").ap()
        dbg_oT = nc.dram_tensor("dbg_oT", [128, 4 * 2 * SEQ], BF16, kind="ExternalOutput").ap()
        dbg_mod = nc.dram_tensor("dbg_mod", [128, 96], F32, kind="ExternalOutput").ap()
        dbg_gB = nc.dram_tensor("dbg_gB", [128, 4 * D], BF16, kind="ExternalOutput").ap()
        dbg_xm = nc.dram_tensor("dbg_xm", [SEQ, D], F32, kind="ExternalOutput").ap()
    xs = nc.dram_tensor("xs", [NSEQ, SEQ, D], F32, kind="Internal").ap()
    xm = nc.dram_tensor("xm", [SEQ, D], F32, kind="Internal").ap()
    ebd_t = nc.dram_tensor("ebd", [24, NDV], BF16, kind="Internal")
    ebd = ebd_t.ap()
    WS = NDV + 128
    ebs_t = nc.dram_tensor("ebs", [24, 128, WS], BF16, kind="Internal")

    uniq = {"n": 0}

    def sb(name, shape, dt, stack=ES):
        uniq["n"] += 1
        return stack.enter_context(nc.sbuf_tensor(f"{name}_{uniq['n']}", list(shape), dt))

    def ps(name, shape, dt):
        return ES.enter_context(nc.psum_tensor(name, list(shape), dt))

    ident = sb("ident", [128, 128], BF16)
    negU = sb("negU", [128, 128], BF16)
    negOnes = sb("negOnes", [128, 128], BF16)
    onesb = sb("onesb", [128, 128], BF16)
    sbmask = sb("sbmask", [128, 128], BF16)
    overlap = sb("overlap", [128, 32], BF16)
    cmpok = sb("cmpok", [128, SEQ], BF16)
    selkeep = sb("selkeep", [128, 8, 32], F32)
    seladd = sb("seladd", [128, 8, 32], F32)
    pen = sb("pen", [32, 16, 128], BF16)
    hT = sb("hT", [128, 8, SEQ], BF16)
    modT = sb("modT", [128, 2, 48], F32)
    gB = sb("gB", [128, 2, 2, D], BF16)
    lngb_h = [None]
    xt = [None, None, None, None]
    xn = [None, None]

    def alloc_io(st, want_xt=True, want_xn=True, want_lngb=False, n_xt=4):
        if want_xt:
            for i_ in range(n_xt):
                xt[i_] = sb(f"xt{i_}", [128, D], F32, st)
        if want_xn:
            xn[0] = sb("xn0", [128, D], BF16, st)
            xn[1] = sb("xn1", [128, D], BF16, st)
        if want_lngb:
            lngb_h[0] = sb("lngb", [128, 2, D], F32, st)
    st6 = sb("st6", [128, 2, 6], F32)
    mvk = sb("mvk", [128, NTB, 2], F32)
    rsk = sb("rsk", [128, NTB], F32)
    mv = sb("mv", [128, 2], F32)
    rstd = sb("rstd", [128, 1], F32)
    Et = [sb(f"E{i}", [128, 512], BF16) for i in range(8)]
    Ft = [sb(f"F{i}", [128, 512], F32) for i in range(6)]
    sm = sb("sm", [128, 64], F32)
    regB = sb("regB", [128, 4 * 2 * SEQ], BF16)
    pg = [ps(f"pg{i}", [128, 512], F32) for i in range(8)]

    R = lambda n: Res(n)
    r_const = R("const")
    r_hT = R("hT")
    r_modT = R("modT")
    r_gB = R("gB")
    r_lngb = R("lngb")
    r_xt = [R(f"xt{i}") for i in range(4)]
    r_xn = [R("xn0"), R("xn1")]
    r_st = R("st")
    r_mvk = R("mvk")
    r_E = [R(f"E{i}") for i in range(8)]
    r_F = [R(f"F{i}") for i in range(6)]
    r_sm = R("sm")
    r_pg = [R(f"pg{i}") for i in range(8)]
    r_regB = R("regB")
    r_ebd = R("ebd")
    r_xs = R("xs")
    r_xm = R("xm")
    r_out = R("out")
    ctr = {"pg": 0, "E": 0, "F": 0, "pT": 0, "xt": 0, "acc": 0}

    pools = {"ns": 6}

    def set_pools(nscore):
        pools["ns"] = nscore

    def nb():
        i = ctr["pg"] % pools["ns"]
        ctr["pg"] += 1
        return pg[i], r_pg[i]

    r_accreg = [R(f"accreg{i}") for i in range(7)]
    r_Rreg = [R(f"Rreg{i}") for i in range(4)]

    def nacc():
        na = 8 - pools["ns"]
        i = pools["ns"] + ctr["acc"] % na
        ctr["acc"] += 1
        return pg[i], r_pg[i]

    def interleave(factories, width):
        it = iter(factories)
        active = []
        free = list(range(width))
        while True:
            while free:
                f = next(it, None)
                if f is None:
                    break
                slot = free.pop(0)
                active.append((f(slot), slot))
            if not active:
                break
            for (g, slot) in list(active):
                try:
                    next(g)
                except StopIteration:
                    active.remove((g, slot))
                    free.append(slot)

    def acc_of(slot):
        i = pools["ns"] + slot
        assert i < 8
        return pg[i], r_pg[i]

    def nE():
        i = ctr["E"] % 8
        ctr["E"] += 1
        return Et[i], r_E[i]

    def nF():
        i = ctr["F"] % 6
        ctr["F"] += 1
        return Ft[i], r_F[i]

    def nT():
        bk, rbk = nb()
        return bk[:].bitcast(BF16), rbk

    for dst, src in ((ident, k_ident), (negU, k_negU), (sbmask, k_sbmask), (overlap, k_overlap), (cmpok, k_cmpok), (pen, k_pen)):
        S.dma("pool", DMA(dst[:], src), writes=[r_const])
    S.dma("sp", DMA(selkeep[:], k_selkeep), writes=[r_const])
    S.dma("sp", DMA(seladd[:], k_seladd), writes=[r_const])
    S.op("dve", MSET(negOnes[:], -1.0), writes=[r_const])
    S.op("dve", MSET(onesb[:], 1.0), writes=[r_const])

    with ExitStack() as st:
        oh = sb("oh", [32, NDV], BF16, st)
        rb = sb("rb", [32, 20], F32, st)
        rbh = sb("rbh", [32, 20], BF16, st)
        rbh32 = sb("rbh32", [32, 20], F32, st)
        rbl = sb("rbl", [32, 20], BF16, st)
        vmk = sb("vmk", [4, NDV], F32, st)
        ebf = sb("ebf", [4, NDV], F32, st)
        ebb = sb("ebb", [4, NDV], BF16, st)
        r_p = R("prol")
        S.dma("pool", DMA(oh[:], k_onehot), writes=[r_p])
        S.dma("sp", DMA(rb[:], rel_bias), writes=[r_p])
        S.op("dve", CP(rbh[:], rb[:]), reads=[r_p], writes=[r_p])
        S.op("dve", CP(rbh32[:], rbh[:]), reads=[r_p], writes=[r_p])
        S.op("dve", TT(rbh32[:], rb[:], rbh32[:], ALU.subtract), reads=[r_p], writes=[r_p])
        S.op("dve", CP(rbl[:], rbh32[:]), reads=[r_p], writes=[r_p])
        vheads = [0, 0, 4, 8, 12, 16]
        for v in range(6):
            h0 = vheads[v]
            for cch in range(6):
                bk, rbk = nb()
                cs = slice(cch * 384, (cch + 1) * 384)
                S.pe([MM(bk[0:4, 0:384], rbh[:, h0:h0 + 4], oh[:, cs], True, False),
                      MM(bk[0:4, 0:384], rbl[:, h0:h0 + 4], oh[:, cs], False, True)], reads=[r_p], writes=[rbk])
                S.op("act", ACT(ebf[:, cs], bk[0:4, 0:384], AF.Exp), reads=[rbk], writes=[r_p])
            S.dma("sp", DMA(vmk[:], k_vmask[v]), writes=[r_p])
            S.op("dve", TT(ebb[:], ebf[:], vmk[:], ALU.mult), reads=[r_p], writes=[r_p])
            S.dma("sp", DMA(ebd[v * 4:(v + 1) * 4, :], ebb[:]), reads=[r_p], writes=[r_ebd])
        skw = sb("skw", [128, NDV], BF16, st)
        r_skw = R("skw")
        for r_ in range(24):
            S.dma("sp", DMA(skw[:], ebd[r_:r_ + 1, :].partition_broadcast(128)), reads=[r_ebd], writes=[r_skw])
            S.dma("sp", DMA(AP(ebs_t, r_ * 128 * WS, [[WS + 1, 128], [1, NDV]]), skw[:]), reads=[r_skw], writes=[r_ebd])
        S.barrier()

    def toeplitz(variant, hh, ndl):
        off = (variant * 4 + hh) * 128 * WS + 128
        return AP(ebs_t, off, [[WS, 128], [128, ndl], [1, 128]])

    def ln_stats_group(srcs):
        ks = [k for (_, _, k) in srcs]
        for (src_ap, r_src, k) in srcs:
            S.op("dve", lambda e, src_ap=src_ap: e.bn_stats(out=st6[:, 0, :], in_=src_ap[:, 0:512]), reads=[r_src], writes=[r_st])
            S.op("dve", lambda e, src_ap=src_ap: e.bn_stats(out=st6[:, 1, :], in_=src_ap[:, 512:1024]), reads=[r_src], writes=[r_st])
            S.op("dve", lambda e, k=k: e.bn_aggr(out=mvk[:, k, :], in_=st6[:]), reads=[r_st], writes=[r_st, r_mvk])
        k0, k1 = min(ks), max(ks) + 1
        S.op("dve", TS(rsk[:, k0:k1], mvk[:, k0:k1, 1], LN_EPS, ALU.add), reads=[r_mvk], writes=[r_mvk])
        S.op("act", ACT(rsk[:, k0:k1], rsk[:, k0:k1], AF.Sqrt), reads=[r_mvk], writes=[r_mvk])
        S.op("dve", lambda e: e.reciprocal(out=rsk[:, k0:k1], in_=rsk[:, k0:k1]), reads=[r_mvk], writes=[r_mvk])

    def to_hT(src_ap, r_src, k, tb, l, scb, shb):
        i = tb % 2
        S.op("dve", TS(xn[i][:], src_ap, mvk[:, k, 0:1], ALU.subtract, rsk[:, k:k + 1], ALU.mult), reads=[r_src, r_mvk], writes=[r_xn[i]])
        tp, rtp = nT()
        S.pe([TR(tp[:, kc * 128:(kc + 1) * 128], xn[i][:, kc * 128:(kc + 1) * 128], ident[:]) for kc in range(8)],
             reads=[r_xn[i], r_const], writes=[rtp])
        for kc in range(8):
            S.op("act", ACT(hT[:, kc, tb * 128:(tb + 1) * 128], tp[:, kc * 128:(kc + 1) * 128], AF.Identity,
                            bias=modT[:, l, shb + kc:shb + kc + 1], scale=modT[:, l, scb + kc:scb + kc + 1]),
                 reads=[rtp, r_modT], writes=[r_hT])

    def affine_k(buf_ap, r_buf, k):
        lngb = lngb_h[0]
        S.op("dve", STT(buf_ap, buf_ap, mvk[:, k, 0:1], lngb[:, 0, :], ALU.subtract, ALU.mult), reads=[r_mvk, r_lngb], writes=[r_buf])
        S.op("dve", STT(buf_ap, buf_ap, rsk[:, k:k + 1], lngb[:, 1, :], ALU.mult, ALU.add), reads=[r_mvk, r_lngb], writes=[r_buf])

    def load_lngb(l, sub):
        lngb = lngb_h[0]
        S.dma("sp", DMA(lngb[:, 0, :], ln_g[l, sub:sub + 1, :].partition_broadcast(128)), writes=[r_lngb])
        S.dma("sp", DMA(lngb[:, 1, :], ln_b[l, sub:sub + 1, :].partition_broadcast(128)), writes=[r_lngb])

    evac_rr = {"i": 0}

    def evac(out_ap, in_ap, reads, writes, scale=None):
        evac_rr["i"] += 1
        if evac_rr["i"] % 2:
            S.op("act", ACT(out_ap, in_ap, AF.Identity, scale=scale), reads=reads, writes=writes)
        elif scale is None:
            S.op("dve", CP(out_ap, in_ap), reads=reads, writes=writes)
        else:
            S.op("dve", TS(out_ap, in_ap, float(scale), ALU.mult), reads=reads, writes=writes)

    for b in range(NSEQ):
        with ExitStack() as st:
            c_sb = sb("c_sb", [128, 8], F32, st)
            scf = sb("scf", [128, 8], F32, st)
            scT = sb("scT", [128, 8], BF16, st)
            scB = sb("scB", [128, 8, 128], BF16, st)
            baT = sb("baT", [128, 2, 48], F32, st)
            bB = sb("bB", [128, 2, 2, D], F32, st)
            wa = [sb(f"wa{i}", [128, 8, 512], BF16, st) for i in range(2)]
            r_m = R("m")
            r_wa = [R("wa0"), R("wa1")]
            S.dma("sp", DMA(c_sb[:], cT[b]), writes=[r_m])
            S.dma("sp", DMA(baT[:], b_adaT.rearrange("l p j -> p l j")), writes=[r_m])
            for l in range(2):
                for gi, c0 in enumerate((2048, 5120)):
                    S.dma("sp", DMA(bB[:, l, gi, :], b_ada[l:l + 1, c0:c0 + D].partition_broadcast(128)), writes=[r_m])
            S.op("act", ACT(scf[:], c_sb[:], AF.Silu), reads=[r_m], writes=[r_m])
            S.op("dve", CP(scT[:], scf[:]), reads=[r_m], writes=[r_m])
            S.op("dve", CP(scB[:], bc_last(scT[:], 128)), reads=[r_m], writes=[r_m])
            for l in range(depth):
                wav = w_ada[l].rearrange("(kc p) n -> p kc n", p=128)
                for ch in range(12):
                    i = ch % 2
                    S.dma("pool", DMA(wa[i][:], wav[:, :, ch * 512:(ch + 1) * 512]), writes=[r_wa[i]])
                    bk, rbk = nb()
                    fns = []
                    for jj in range(4):
                        for kc in range(8):
                            fns.append(MM(bk[:, jj:jj + 1], wa[i][:, kc, jj * 128:(jj + 1) * 128], scT[:, kc:kc + 1], kc == 0, kc == 7))
                    S.pe(fns, reads=[r_wa[i], r_m], writes=[rbk])
                    S.op("dve", TT(modT[:, l, ch * 4:(ch + 1) * 4], bk[:, 0:4], baT[:, l, ch * 4:(ch + 1) * 4], ALU.add),
                         reads=[rbk, r_m], writes=[r_modT])
                    if ch in (4, 5, 10, 11):
                        gi = 0 if ch < 6 else 1
                        half = ch % 2
                        bk2, rbk2 = nb()
                        S.pe([MM(bk2[:], scB[:, kc, :], wa[i][:, kc, :], kc == 0, kc == 7) for kc in range(8)],
                             reads=[r_wa[i], r_m], writes=[rbk2])
                        S.op("dve", TT(gB[:, l, gi, half * 512:(half + 1) * 512], bk2[:], bB[:, l, gi, half * 512:(half + 1) * 512], ALU.add),
                             reads=[rbk2, r_m], writes=[r_gB])
                S.op("dve", TS(modT[:, l, 8:16], modT[:, l, 8:16], 1.0, ALU.add), reads=[r_modT], writes=[r_modT])
                S.op("dve", TS(modT[:, l, 32:40], modT[:, l, 32:40], 1.0, ALU.add), reads=[r_modT], writes=[r_modT])
            S.barrier()

        for l in range(depth):
            S.mark(f"b{b}l{l} LN1")
            x_src = x[b] if l == 0 else xs[b]
            r_xsrc = None if l == 0 else r_xs
            w_in_v = w_in[l].rearrange("(kc p) n -> p kc n", p=128)

            with ExitStack() as st:
                alloc_io(st)
                for g4 in range(4):
                    for i in range(4):
                        tb = g4 * 4 + i
                        S.dma("sp", DMA(xt[i][:], x_src[tb * 128:(tb + 1) * 128, :]), reads=[r_xsrc], writes=[r_xt[i]])
                    ln_stats_group([(xt[i][:], r_xt[i], g4 * 4 + i) for i in range(4)])
                    for i in range(4):
                        tb = g4 * 4 + i
                        to_hT(xt[i][:], r_xt[i], tb, tb, l, 8, 0)
                S.barrier()

            oT = regB[:].rearrange("p (b c t) -> p b c t", b=4, c=2)
            r_oT = r_regB

            with ExitStack() as st:
                fmw = sb("fmw", [128, 8, SEQ], BF16, st)
                wmix = sb("wmix", [128, 8, 1024], BF16, st)
                vtm = sb("vtm", [128, NTB, 4, 65], BF16, st)
                ebt = [sb(f"ebt{i}", [128, 23, 128], BF16, st) for i in range(2)]
                o_tok = sb("o_tok", [128, NTB, 256], BF16, st)
                gts = sb("gts", [128, NTB, 12], F32, st)
                sinkE = sb("sinkE", [128, 4], F32, st)
                kcT = sb("kcT", [128, 128], BF16, st)
                vcA = sb("vcA", [128, 65], BF16, st)
                w1t = sb("w1t", [64, 32, 64], BF16, st)
                peT = sb("peT", [64, 32], BF16, st)
                w2d = sb("w2d", [64, 128], BF16, st)
                cvec = sb("cvec", [64, 1], F32, st)
                gu = sb("gu", [64, 4, 128], F32, st)
                gTt = sb("gTt", [64, 128], BF16, st)
                psel = sb("psel", [128, 8, 128], F32, st)
                pselb = sb("pselb", [128, 8, 128], BF16, st)
                scs = sb("scs", [128, 3, 32], F32, st)
                m8 = sb("m8", [128, 2, 8], F32, st)
                unsel = sb("unsel", [128, 32], BF16, st)
                unselT = sb("unselT", [32, SEQ // 2], BF16, st)
                Rsb = sb("Rsb", [128, 128], BF16, st)
                r_fm = [R(f"fm{i}") for i in range(8)]
                r_wmix = R("wmix")
                r_vtm = R("vtm")
                r_ebt = [R("ebt0"), R("ebt1")]
                r_otok = R("otok")
                r_oacc = R("oacc")
                r_gts = R("gts")
                r_cmp = R("cmp")
                r_sel = R("sel")
                r_Rsb = R("Rsb")
                ebc = {"i": 0}

                S.op("pool", MSET(vtm[:], 1.0), writes=[r_vtm])
                S.mark(f"b{b}l{l} NSA-proj")

                def load_w(segs):
                    for (dc_, sc_, n_) in segs:
                        S.dma("pool", DMA(wmix[:, :, dc_:dc_ + n_], w_in_v[:, :, sc_:sc_ + n_]), writes=[r_wmix])

                def proj_fm(ti, c0, m, scale=None):
                    for T in range(4):
                        bk, rbk = nb()
                        S.pe([MM(bk[0:m, :], wmix[:, kc, c0:c0 + m], hT[:, kc, T * 512:(T + 1) * 512], kc == 0, kc == 7) for kc in range(8)],
                             reads=[r_wmix, r_hT], writes=[rbk])
                        evac(fmw[0:m, ti, T * 512:(T + 1) * 512], bk[0:m, :], [rbk], [r_fm[ti]], scale=scale)

                def proj_tm(c0, n, sinks):
                    for tb in range(NTB):
                        bk, rbk = nb()
                        S.pe([MM(bk[:, 0:n], hT[:, kc, tb * 128:(tb + 1) * 128], wmix[:, kc, c0:c0 + n], kc == 0, kc == 7) for kc in range(8)],
                             reads=[r_wmix, r_hT], writes=[rbk])
                        for fn in sinks:
                            fn(tb, bk, rbk)

                def v_sink(col, slot):
                    def f(tb, bk, rbk):
                        evac(vtm[:, tb, slot, 0:64], bk[:, col:col + 64], [rbk], [r_vtm])
                    return f

                def load_ebt(specs):
                    i = ebc["i"] % 2
                    ebc["i"] += 1
                    for (v_, hh_, ndl_, s0_) in specs:
                        S.dma("sp", DMA(ebt[i][:, s0_:s0_ + ndl_, :], toeplitz(v_, hh_, ndl_)), reads=[r_ebd], writes=[r_ebt[i]])
                    return ebt[i], r_ebt[i]

                def chunk(qap, r_q, ktile, ti_k, pb, j0, n, eb_ap, r_eb, v_of, acc, racc, first, last, penq=None):
                    bk, rbk = nb()
                    fns = []
                    for i in range(n):
                        j = j0 - i
                        sl = slice(i * 128, (i + 1) * 128)
                        kap = ktile[pb:pb + 64, j * 128:(j + 1) * 128]
                        if penq is not None:
                            fns.append(MM(bk[:, sl], kap, qap, True, False))
                            fns.append(MM(bk[:, sl], pen[0:32, j, :], penq, False, True))
                        else:
                            fns.append(MM(bk[:, sl], kap, qap, True, True))
                    rd = [r_fm[ti_k], r_q] + ([r_sel, r_const] if penq is not None else [])
                    S.pe(fns, reads=rd, writes=[rbk])
                    yield
                    E, rE = nE()
                    S.op("act", ACT(E[:, 0:n * 128], bk[:, 0:n * 128], AF.Exp), reads=[rbk], writes=[rE])
                    yield
                    S.op("dve", TT(E[:, 0:n * 128], E[:, 0:n * 128], eb_ap, ALU.mult), reads=[r_eb], writes=[rE])
                    yield
                    S.pe([MM(acc, E[:, i * 128:(i + 1) * 128], v_of(j0 - i), first and i == 0, last and i == n - 1) for i in range(n)],
                         reads=[rE, r_vtm, r_cmp], writes=[racc])
                    yield

                r_q_cur = [None]

                def flat(ap3):
                    return ap3.rearrange("p a b -> p (a b)")

                def to_oT(br):
                    for qb in range(NTB):
                        tp, rtp = nT()
                        S.pe([TR(tp[:, c * 128:(c + 1) * 128], o_tok[:, qb, c * 128:(c + 1) * 128], ident[:]) for c in range(2)],
                             reads=[r_otok, r_const], writes=[rtp])
                        evac(oT[:, br, :, qb * 128:(qb + 1) * 128], tp[:, 0:256].rearrange("p (c t) -> p c t", c=2), [rtp], [r_oT])

                WQ = 0
                load_w([(0, 0, 256), (256, 256, 64), (320, 256, 64), (384, 384, 64), (448, 384, 64), (512, 512, 64), (576, 512, 64),
                        (640, 320, 64), (704, 448, 64), (768, 576, 64), (832, 640, 12)])
                proj_fm(0, 0, 128, 0.125)
                proj_fm(1, 128, 128, 0.125)
                proj_fm(2, 256, 128)
                proj_fm(3, 384, 128)
                proj_fm(4, 512, 128)
                proj_fm(5, 640, 64)

                def gate_sink(tb, bk, rbk):
                    S.op("act", ACT(gts[:, tb, :], bk[:, 128:140], AF.Sigmoid), reads=[rbk], writes=[r_gts])
                proj_tm(704, 140, [v_sink(0, 0), v_sink(64, 1), gate_sink])

                S.mark(f"b{b}l{l} NSA-compress")
                S.op("dve", MSET(kcT[:], 0.0), writes=[r_cmp])
                S.op("dve", MSET(vcA[:], 0.0), writes=[r_cmp])
                S.op("dve", MSET(vcA[:, 64:65], 1.0), writes=[r_cmp])
                for which in range(2):
                    src_t = 2 if which == 0 else 5
                    S.dma("pool", DMA(w1t[:], cmp_w1[l, which].rearrange("(pos d) o -> d pos o", d=64)), writes=[r_cmp])
                    S.dma("pool", DMA(peT[:], cmp_peT[l, which]), writes=[r_cmp])
                    S.dma("pool", DMA(w2d[:, 0:64], cmp_w2[l, which]), writes=[r_cmp])
                    S.dma("pool", DMA(w2d[:, 64:128], cmp_w2[l, which]), writes=[r_cmp])
                    bk, rbk = nb()
                    S.pe([MM(bk[0:64, 0:1], w1t[:, pos, :], peT[:, pos:pos + 1], pos == 0, pos == 31) for pos in range(32)],
                         reads=[r_cmp], writes=[rbk])
                    S.op("dve", CP(cvec[:], bk[0:64, 0:1]), reads=[rbk], writes=[r_cmp])
                    bk, rbk = nb()
                    fns = []
                    for pos in range(32):
                        base = fmw[0:64, src_t, pos:pos + 1]
                        src = AP(base.tensor, base.offset, [list(base.ap[0]), [16, 127]])
                        fns.append(MM(bk[0:64, 0:127], w1t[:, pos, :], src, pos == 0, pos == 31))
                    S.pe(fns, reads=[r_cmp, r_fm[src_t]], writes=[rbk])
                    u = gu[:, 0, 0:127]
                    t1 = gu[:, 1, 0:127]
                    t2 = gu[:, 2, 0:127]
                    S.op("act", ACT(u, bk[0:64, 0:127], AF.Identity, bias=cvec[:, 0:1]), reads=[rbk, r_cmp], writes=[r_cmp])
                    S.op("dve", TT(t1, u, u, ALU.mult), reads=[r_cmp], writes=[r_cmp])
                    S.op("dve", TS(t1, t1, 0.044715, ALU.mult, 1.0, ALU.add), reads=[r_cmp], writes=[r_cmp])
                    S.op("dve", TT(t1, t1, u, ALU.mult), reads=[r_cmp], writes=[r_cmp])
                    S.op("act", ACT(t2, t1, AF.Sigmoid, scale=1.5957691216057308), reads=[r_cmp], writes=[r_cmp])
                    S.op("dve", TT(gTt[:, 0:127], u, t2, ALU.mult), reads=[r_cmp], writes=[r_cmp])
                    bk, rbk = nb()
                    if which == 0:
                        S.pe([MM(bk[:, 0:127], w2d[:, :], gTt[:, 0:127], True, True)], reads=[r_cmp], writes=[rbk])
                        S.op("dve", CP(kcT[:, 0:127], bk[:, 0:127]), reads=[rbk], writes=[r_cmp])
                    else:
                        S.pe([MM(bk[0:127, 0:64], gTt[:, 0:127], w2d[:, 0:64], True, True)], reads=[r_cmp], writes=[rbk])
                        S.op("dve", CP(vcA[0:127, 0:64], bk[0:127, 0:64]), reads=[rbk], writes=[r_cmp])

                S.mark(f"b{b}l{l} NSA-cmp")
                for h in range(4):
                    qt = h // 2
                    pb = 64 * (h % 2)
                    for T in range(4):
                        bk, rbk = nb()
                        S.pe([MM(bk[:], kcT[pb:pb + 64, :], fmw[pb:pb + 64, qt, T * 512:(T + 1) * 512], True, True)],
                             reads=[r_cmp, r_fm[qt]], writes=[rbk])
                        E, rE = nE()
                        S.op("act", ACT(E[:], bk[:], AF.Exp), reads=[rbk], writes=[rE])
                        S.op("dve", TT(E[:], E[:], cmpok[:, T * 512:(T + 1) * 512], ALU.mult), reads=[r_const], writes=[rE])
                        acc, racc = nb()
                        S.pe([MM(acc[:, i * 65:(i + 1) * 65], E[:, i * 128:(i + 1) * 128], vcA[:, :], True, True) for i in range(4)],
                             reads=[rE, r_cmp], writes=[racc])
                        a3 = acc[:, 0:260].rearrange("p (i c) -> p i c", c=65)
                        rs4 = sm[:, 0:4]
                        S.op("dve", TS(rs4, a3[:, :, 64], 1e-30, ALU.max), reads=[racc], writes=[r_sm])
                        S.op("dve", lambda e, rs4=rs4: e.reciprocal(out=rs4, in_=rs4), reads=[r_sm], writes=[r_sm])
                        S.op("dve", TT(rs4, rs4, gts[:, T * 4:(T + 1) * 4, h * 3], ALU.mult), reads=[r_gts], writes=[r_sm])
                        S.op("dve", TT(o_tok[:, T * 4:(T + 1) * 4, h * 64:(h + 1) * 64], a3[:, :, 0:64], bc_last(rs4, 64), ALU.mult),
                             reads=[racc, r_sm], writes=[r_otok])
                        if T >= 2:
                            dB, rdB = nb()
                            S.pe([MM(dB[:], onesb[:], E[:], True, True)], reads=[rE, r_const], writes=[rdB])
                            Fd, rFd = nF()
                            S.op("dve", TS(Fd[:], dB[:], 1e-30, ALU.max), reads=[rdB], writes=[rFd])
                            S.op("dve", lambda e, Fd=Fd: e.reciprocal(out=Fd[:], in_=Fd[:]), reads=[rFd], writes=[rFd])
                            pv = flat(psel[:, (T - 2) * 4:(T - 1) * 4, :])
                            if h == 0:
                                S.op("dve", TT(pv, E[:], Fd[:], ALU.mult), reads=[rE, rFd], writes=[r_sel])
                            else:
                                S.op("dve", TT(Fd[:], E[:], Fd[:], ALU.mult), reads=[rE], writes=[rFd])
                                S.op("pool", TT(pv, pv, Fd[:], ALU.add), reads=[rFd], writes=[r_sel])
                S.mark(f"b{b}l{l} NSA-select")
                S.op("dve", CP(flat(pselb[:]), flat(psel[:])), reads=[r_sel], writes=[r_sel])
                for qb in range(8, 16):
                    bk, rbk = nb()
                    S.pe([MM(bk[:, 0:32], pselb[:, qb - 8, :], overlap[:], True, True)], reads=[r_sel, r_const], writes=[rbk])
                    sc0 = scs[:, 0, :]
                    sc1 = scs[:, 1, :]
                    S.op("dve", TT(sc0, bk[:, 0:32], selkeep[:, qb - 8, :], ALU.mult), reads=[rbk, r_const], writes=[r_sel])
                    S.op("dve", TT(sc0, sc0, seladd[:, qb - 8, :], ALU.add), reads=[r_const], writes=[r_sel])
                    S.op("dve", lambda e, sc0=sc0: e.max(out=m8[:, 0, :], in_=sc0), reads=[r_sel], writes=[r_sel])
                    S.op("dve", lambda e, sc0=sc0, sc1=sc1: e.match_replace(out=sc1, in_to_replace=m8[:, 0, :], in_values=sc0, imm_value=-3.0e38),
                         reads=[r_sel], writes=[r_sel])
                    S.op("dve", lambda e, sc1=sc1: e.max(out=m8[:, 1, :], in_=sc1), reads=[r_sel], writes=[r_sel])
                    S.op("dve", TS(unsel[:], sc0, m8[:, 1, 7:8], ALU.is_lt), reads=[r_sel], writes=[r_sel])
                    tp, rtp = nT()
                    S.pe([TR(tp[0:32, 0:128], unsel[:], ident[:])], reads=[r_sel, r_const], writes=[rtp])
                    S.op("dve", CP(unselT[:, (qb - 8) * 128:(qb - 7) * 128], tp[0:32, 0:128]), reads=[rtp], writes=[r_sel])
                S.mark(f"b{b}l{l} NSA-selwin")
                def nsa_unit(h, qb, bi, eb, reb, slot):
                    qt = h // 2
                    pb = 64 * (h % 2)
                    qap = fmw[pb:pb + 64, qt, qb * 128:(qb + 1) * 128]
                    ktile_i, vslot, slot0, nd = ((3, 0, 5, qb + 1), (4, 1, 0, min(5, qb + 1)))[bi]
                    acc_b, racc = acc_of(slot)
                    acc = acc_b[:, 0:65]
                    penq = unselT[:, (qb - 8) * 128:(qb - 7) * 128] if (bi == 0 and qb >= 8) else None
                    d0 = 0
                    while d0 < nd:
                        n = min(4, nd - d0)
                        yield from chunk(qap, r_fm[qt], fmw[:, ktile_i, :], ktile_i, pb, qb - d0, n, flat(eb[:, slot0 + d0:slot0 + d0 + n, :]), reb,
                                         lambda j, vslot=vslot: vtm[:, j, vslot, :], acc, racc, d0 == 0, d0 + n >= nd, penq=penq)
                        d0 += n
                    yield
                    rs = sm[:, 8 + (qb % 4) * 2 + bi:9 + (qb % 4) * 2 + bi]
                    S.op("dve", lambda e, rs=rs, acc_b=acc_b: e.reciprocal(out=rs, in_=acc_b[:, 64:65]), reads=[racc], writes=[r_sm])
                    S.op("dve", TT(rs, rs, gts[:, qb, h * 3 + 1 + bi:h * 3 + 2 + bi], ALU.mult), reads=[r_gts], writes=[r_sm])
                    osl = o_tok[:, qb, h * 64:(h + 1) * 64]
                    S.op("dve", STT(osl, acc_b[:, 0:64], rs, osl, ALU.mult, ALU.add), reads=[racc, r_sm], writes=[r_otok])

                set_pools(4)
                def nsa_all():
                    for h in range(4):
                        eb, reb = load_ebt([(0, h, 5, 0), (1, h, 16, 5)])
                        for qb in range(NTB):
                            for bi in range(2):
                                yield (lambda slot, h=h, qb=qb, bi=bi, eb=eb, reb=reb: nsa_unit(h, qb, bi, eb, reb, slot))
                interleave(nsa_all(), 4)
                to_oT(0)

                S.mark(f"b{b}l{l} SWA")
                load_w([(0, 652, 256), (256, 908, 64), (320, 908, 64), (384, 972, 64), (448, 972, 64), (512, 1036, 128)])
                proj_fm(0, 0, 128, 0.125)
                proj_fm(1, 128, 128, 0.125)
                proj_fm(2, 256, 128)
                proj_fm(3, 384, 128)
                proj_tm(512, 128, [v_sink(0, 0), v_sink(64, 1)])
                S.dma("sp", DMA(sinkE[:], swa_sinks[l:l + 1, :].partition_broadcast(128)), writes=[r_cmp])
                S.op("act", ACT(sinkE[:], sinkE[:], AF.Exp), reads=[r_cmp], writes=[r_cmp])
                def swa_unit(h, qb, eb, reb, slot):
                    qt = h // 2
                    pb = 64 * (h % 2)
                    g = h // 2
                    qap = fmw[pb:pb + 64, qt, qb * 128:(qb + 1) * 128]
                    acc_b, racc = acc_of(slot)
                    nd = min(2, qb + 1)
                    yield from chunk(qap, r_fm[qt], fmw[:, 2 + g, :], 2 + g, pb, qb, nd, flat(eb[:, 0:nd, :]), reb,
                                     lambda j, g=g: vtm[:, j, g, :], acc_b[:, 0:65], racc, True, True)
                    yield
                    rs = sm[:, 8 + (qb % 4):9 + (qb % 4)]
                    S.op("dve", TT(rs, acc_b[:, 64:65], sinkE[:, h:h + 1], ALU.add), reads=[racc, r_cmp], writes=[r_sm])
                    S.op("dve", lambda e, rs=rs: e.reciprocal(out=rs, in_=rs), reads=[r_sm], writes=[r_sm])
                    S.op("dve", TS(o_tok[:, qb, h * 64:(h + 1) * 64], acc_b[:, 0:64], rs, ALU.mult), reads=[racc, r_sm], writes=[r_otok])

                def swa_all():
                    for h in range(4):
                        eb, reb = load_ebt([(2, h, 2, 0)])
                        for qb in range(NTB):
                            yield (lambda slot, h=h, qb=qb, eb=eb, reb=reb: swa_unit(h, qb, eb, reb, slot))
                interleave(swa_all(), 4)
                to_oT(1)

                S.mark(f"b{b}l{l} DIL")
                load_w([(0, 1164, 768), (768, 1932, 256)])
                for ti in range(8):
                    proj_fm(ti, ti * 128, 128, 0.125 if ti < 6 else None)
                load_w([(0, 2188, 256)])
                proj_tm(0, 256, [v_sink(0, 0), v_sink(64, 1), v_sink(128, 2), v_sink(192, 3)])
                def dil_unit(h, qb, eb, reb, slot):
                    pb = 64 * (h % 2)
                    kt = 6 + h // 2
                    acc_b, racc = acc_of(slot)
                    plan = []
                    for gi, (slot0, ndmax) in enumerate(((0, 2), (2, 5), (7, 16))):
                        nd = min(ndmax, qb + 1)
                        d0 = 0
                        while d0 < nd:
                            n = min(4, nd - d0)
                            plan.append((gi, slot0, d0, n))
                            d0 += n
                    for pi, (gi, slot0, d0, n) in enumerate(plan):
                        qt = gi * 2 + h // 2
                        qap = fmw[pb:pb + 64, qt, qb * 128:(qb + 1) * 128]
                        yield from chunk(qap, r_fm[qt], fmw[:, kt, :], kt, pb, qb - d0, n, flat(eb[:, slot0 + d0:slot0 + d0 + n, :]), reb,
                                         lambda j, h=h: vtm[:, j, h, :], acc_b[:, 0:65], racc, pi == 0, pi == len(plan) - 1)
                    yield
                    rs = sm[:, 8 + (qb % 4):9 + (qb % 4)]
                    S.op("dve", lambda e, rs=rs, acc_b=acc_b: e.reciprocal(out=rs, in_=acc_b[:, 64:65]), reads=[racc], writes=[r_sm])
                    S.op("dve", TS(o_tok[:, qb, h * 64:(h + 1) * 64], acc_b[:, 0:64], rs, ALU.mult), reads=[racc, r_sm], writes=[r_otok])

                def dil_all():
                    for h in range(4):
                        eb, reb = load_ebt([(3, h, 2, 0), (4, h, 5, 2), (5, h, 16, 7)])
                        for qb in range(NTB):
                            yield (lambda slot, h=h, qb=qb, eb=eb, reb=reb: dil_unit(h, qb, eb, reb, slot))
                interleave(dil_all(), 4)
                to_oT(2)

                S.mark(f"b{b}l{l} SB")
                load_w([(0, 2444, 768)])
                for ti in range(4):
                    proj_fm(ti, ti * 128, 128, 0.125 if ti < 2 else None)
                proj_tm(512, 256, [v_sink(0, 0), v_sink(64, 1), v_sink(128, 2), v_sink(192, 3)])
                def sb_unit(h, qb, slot):
                    qt = h // 2
                    kt = 2 + h // 2
                    pb = 64 * (h % 2)
                    qap = fmw[pb:pb + 64, qt, qb * 128:(qb + 1) * 128]
                    acc_b, racc = acc_of(slot)
                    Rsb, r_Rsb = Rsbs[slot], r_Rsbs[slot]
                    nd = qb + 1
                    d0 = 0
                    while d0 < nd:
                        n = min(4, nd - d0)
                        W = n * 128
                        zb, rzb = nb()
                        S.pe([MM(zb[:, i * 128:(i + 1) * 128], fmw[pb:pb + 64, kt, (qb - d0 - i) * 128:(qb - d0 - i + 1) * 128], qap, True, True)
                              for i in range(n)], reads=[r_fm[kt], r_fm[qt]], writes=[rzb])
                        yield
                        Fe, rFe = nF()
                        S.op("act", ACT(Fe[:, 0:W], zb[:, 0:W], AF.Exp), reads=[rzb], writes=[rFe])
                        yield
                        Lb, rLb = nE()
                        S.op("act", ACT(Lb[:, 0:W], Fe[:, 0:W], AF.Ln, bias=1.0), reads=[rFe], writes=[rLb])
                        yield
                        if d0 == 0:
                            S.op("dve", TT(Lb[:, 0:128], Lb[:, 0:128], sbmask[:], ALU.mult), reads=[r_const], writes=[rLb])
                        F1, rF1 = nF()
                        S.op("dve", TT(F1[:, 0:W], zb[:, 0:W], Lb[:, 0:W], ALU.subtract), reads=[rzb, rLb], writes=[rF1])
                        yield
                        cb, rcb = nb()
                        fns = []
                        for i in range(n):
                            sl = slice(i * 128, (i + 1) * 128)
                            terms = [(negU[:], Lb[:, sl])] + [(negOnes[:], Lb[:, i2 * 128:(i2 + 1) * 128]) for i2 in range(i)]
                            if d0 > 0:
                                terms.append((ident[:], Rsb[:]))
                            for ti_, (lt, rh) in enumerate(terms):
                                fns.append(MM(cb[:, sl], lt, rh, ti_ == 0, ti_ == len(terms) - 1))
                        S.pe(fns, reads=[rLb, r_const, r_Rsb], writes=[rcb])
                        if d0 + n < nd:
                            rbk_, rrb = nb()
                            terms = [(negOnes[:], Lb[:, i * 128:(i + 1) * 128]) for i in range(n)]
                            if d0 > 0:
                                terms.append((ident[:], Rsb[:]))
                            S.pe([MM(rbk_[:, 0:128], lt, rh, ti_ == 0, ti_ == len(terms) - 1) for ti_, (lt, rh) in enumerate(terms)],
                                 reads=[rLb, r_const, r_Rsb], writes=[rrb])
                            yield
                            S.op("dve", CP(Rsb[:], rbk_[:, 0:128]), reads=[rrb], writes=[r_Rsb])
                        yield
                        S.op("dve", TT(F1[:, 0:W], F1[:, 0:W], cb[:, 0:W], ALU.add), reads=[rcb], writes=[rF1])
                        yield
                        A, rA = nE()
                        S.op("act", ACT(A[:, 0:W], F1[:, 0:W], AF.Exp), reads=[rF1], writes=[rA])
                        yield
                        if d0 == 0:
                            S.op("dve", TT(A[:, 0:128], A[:, 0:128], sbmask[:], ALU.mult), reads=[r_const], writes=[rA])
                        S.pe([MM(acc_b[:, 0:64], A[:, i * 128:(i + 1) * 128], vtm[:, qb - d0 - i, h, 0:64], d0 == 0 and i == 0,
                                 d0 + n >= nd and i == n - 1) for i in range(n)], reads=[rA, r_vtm], writes=[racc])
                        yield
                        d0 += n
                    yield
                    evac(o_tok[:, qb, h * 64:(h + 1) * 64], acc_b[:, 0:64], [racc], [r_otok])

                set_pools(6)
                SBW = 2
                Rsbs = [sb(f"Rsb{i}", [128, 128], BF16, st) for i in range(SBW)]
                r_Rsbs = [R(f"Rsb{i}") for i in range(SBW)]
                units = []
                for h in range(4):
                    for qb in range(NTB):
                        units.append(lambda slot, h=h, qb=qb: sb_unit(h, qb, slot))
                interleave(units, SBW)
                set_pools(6)
                to_oT(3)
                S.barrier()
                if dbg and b == 0 and l == 0:
                    S.dma("sp", DMA(dbg_hT, hT[:].rearrange("p a b -> p (a b)")), reads=[r_hT])
                    S.dma("sp", DMA(dbg_oT, regB[:]), reads=[r_regB])
                    S.dma("sp", DMA(dbg_mod, modT[:].rearrange("p a b -> p (a b)")), reads=[r_modT])
                    S.dma("sp", DMA(dbg_gB, gB[:].rearrange("p a b c -> p (a b c)")), reads=[r_gB])
                    S.barrier()

            S.mark(f"b{b}l{l} merge")
            with ExitStack() as st:
                set_pools(8)
                wout = sb("wout", [128, 8, D], BF16, st)
                wbr = sb("wbr", [128, 4, 2, D], BF16, st)
                mergedT = sb("mergedT", [128, 8, SEQ], BF16, st)
                wgt = [sb(f"wgt{i}", [128, 8, 4, 128], BF16, st) for i in range(2)]
                maccs = [sb(f"macc{i}", [128, 512], F32, st) for i in range(2)]
                r_maccs = [R("macc0"), R("macc1")]
                mi = 0
                alloc_io(st, want_xn=False, want_lngb=True, n_xt=2)
                r_w = R("w")
                r_wgt = [R("wgt0"), R("wgt1")]
                r_mg = R("mg")
                S.dma("pool", DMA(wout[:], w_out[l].rearrange("(kc p) n -> p kc n", p=128)), writes=[r_w])
                S.op("pool", TT(wout[:], wout[:], bc_mid(gB[:, l, 0, :], 8), ALU.mult), reads=[r_gB], writes=[r_w])
                S.dma("pool", DMA(wbr[:], w_branch[l].rearrange("b (c p) n -> p b c n", p=128)), writes=[r_w])
                load_lngb(l, 0)
                for dc in range(8):
                    i = dc % 2
                    for br in range(4):
                        c0 = 3212 + br * D + dc * 128
                        S.dma("pool", DMA(wgt[i][:, :, br, :], w_in_v[:, :, c0:c0 + 128]), writes=[r_wgt[i]])
                    for T in range(4):
                        ts_ = slice(T * 512, (T + 1) * 512)
                        macc = maccs[mi % 2]
                        r_macc = r_maccs[mi % 2]
                        mi += 1
                        for br in range(4):
                            bG, rbG = nb()
                            S.pe([MM(bG[:], wgt[i][:, kc, br, :], hT[:, kc, ts_], kc == 0, kc == 7) for kc in range(8)],
                                 reads=[r_wgt[i], r_hT], writes=[rbG])
                            bP, rbP = nb()
                            S.pe([MM(bP[:], wbr[:, br, c, dc * 128:(dc + 1) * 128], oT[:, br, c, ts_], c == 0, c == 1) for c in range(2)],
                                 reads=[r_w, r_oT], writes=[rbP])
                            Fg, rFg = nF()
                            S.op("act", ACT(Fg[:], bG[:], AF.Sigmoid), reads=[rbG], writes=[rFg])
                            if br == 0:
                                S.op("dve", TT(macc[:], Fg[:], bP[:], ALU.mult), reads=[rFg, rbP], writes=[r_macc])
                            else:
                                S.op("dve", TT(Fg[:], Fg[:], bP[:], ALU.mult), reads=[rbP], writes=[rFg])
                                if br < 3:
                                    S.op("pool", TT(macc[:], macc[:], Fg[:], ALU.add), reads=[rFg], writes=[r_macc])
                                else:
                                    S.op("pool", TT(mergedT[:, dc, ts_], macc[:], Fg[:], ALU.add), reads=[rFg, r_macc], writes=[r_mg])
                S.mark(f"b{b}l{l} wout+LN")
                for g4 in range(8):
                    for i in range(2):
                        tb = g4 * 2 + i
                        S.dma("sp", DMA(xt[i][:], x_src[tb * 128:(tb + 1) * 128, :]), reads=[r_xsrc], writes=[r_xt[i]])
                    for i in range(2):
                        tb = g4 * 2 + i
                        for dh in range(2):
                            bk, rbk = nb()
                            S.pe([MM(bk[:], mergedT[:, kc, tb * 128:(tb + 1) * 128], wout[:, kc, dh * 512:(dh + 1) * 512], kc == 0, kc == 7)
                                  for kc in range(8)], reads=[r_mg, r_w], writes=[rbk])
                            xs_ = xt[i][:, dh * 512:(dh + 1) * 512]
                            S.op("dve", STT(xs_, xs_, float(ALPHA), bk[:], ALU.mult, ALU.add), reads=[rbk], writes=[r_xt[i]])
                    ln_stats_group([(xt[i][:], r_xt[i], g4 * 2 + i) for i in range(2)])
                    for i in range(2):
                        tb = g4 * 2 + i
                        affine_k(xt[i][:], r_xt[i], tb)
                        S.dma("sp", DMA(xm[tb * 128:(tb + 1) * 128, :], xt[i][:]), reads=[r_xt[i]], writes=[r_xm])
                        if dbg and b == 0 and l == 0:
                            S.dma("sp", DMA(dbg_xm[tb * 128:(tb + 1) * 128, :], xt[i][:]), reads=[r_xt[i]])
                S.barrier()

            S.mark(f"b{b}l{l} LN2")
            with ExitStack() as st:
                rr = sb("rr", [128, NTB, D], F32, st)
                alloc_io(st, want_xt=False, want_lngb=True)
                aT = [sb(f"aT{i}", [128, 2, SEQ], BF16, st) for i in range(2)]
                gate_sb = sb("gate_sb", [128, NTB, 8], F32, st)
                wr = sb("wr", [128, 8, 8], BF16, st)
                lg = sb("lg", [128, 8], F32, st)
                mx = sb("mx", [128, 8], F32, st)
                r_rr = [R(f"rr{i}") for i in range(NTB)]
                r_aT = [R("aT0"), R("aT1")]
                r_gate = R("gate")
                r_wr = R("wr")
                wg = [regB[:, i * 2048:(i + 1) * 2048].rearrange("p (k f) -> p k f", k=8) for i in range(2)]
                wu = [regB[:, 4096 + i * 2048:4096 + (i + 1) * 2048].rearrange("p (k f) -> p k f", k=8) for i in range(2)]
                wd = [regB[:, 8192 + i * 2048:8192 + (i + 1) * 2048].rearrange("p (c d) -> p c d", c=2) for i in range(2)]
                r_wg = [R("wg0"), R("wg1")]
                r_wd = [R("wd0"), R("wd1")]
                for tb in range(NTB):
                    S.dma("sp", DMA(rr[:, tb, :], xm[tb * 128:(tb + 1) * 128, :]), reads=[r_xm], writes=[r_rr[tb]])
                for g4 in range(4):
                    ln_stats_group([(rr[:, g4 * 4 + i, :], r_rr[g4 * 4 + i], g4 * 4 + i) for i in range(4)])
                    for i in range(4):
                        tb = g4 * 4 + i
                        to_hT(rr[:, tb, :], r_rr[tb], tb, tb, l, 32, 24)
                        S.op("pool", TS(rr[:, tb, :], rr[:, tb, :], float(ALPHA), ALU.mult), writes=[r_rr[tb]])
                load_lngb(l, 1)
                S.mark(f"b{b}l{l} FFN")
                moe = (l % 2 == 1)
                if moe:
                    S.dma("pool", DMA(wr[:], moe_router[0].rearrange("(kc p) e -> p kc e", p=128)), writes=[r_wr])
                    for tb in range(NTB):
                        bk, rbk = nb()
                        S.pe([MM(bk[:, 0:8], hT[:, kc, tb * 128:(tb + 1) * 128], wr[:, kc, :], kc == 0, kc == 7) for kc in range(8)],
                             reads=[r_hT, r_wr], writes=[rbk])
                        S.op("dve", CP(lg[:], bk[:, 0:8]), reads=[rbk], writes=[r_gate])
                        S.op("dve", lambda e: e.max(out=mx[:], in_=lg[:]), reads=[r_gate], writes=[r_gate])
                        gsl = gate_sb[:, tb, :]
                        S.op("dve", TS(gsl, lg[:], mx[:, 1:2], ALU.is_ge), reads=[r_gate], writes=[r_gate])
                        S.op("dve", TS(mx[:, 2:3], mx[:, 0:1], -1.0, ALU.mult), reads=[r_gate], writes=[r_gate])
                        S.op("act", ACT(lg[:], lg[:], AF.Exp, bias=mx[:, 2:3]), reads=[r_gate], writes=[r_gate])
                        S.op("dve", TT(gsl, gsl, lg[:], ALU.mult), reads=[r_gate], writes=[r_gate])
                        S.op("dve", lambda e, gsl=gsl: e.reduce_sum(out=mx[:, 3:4], in_=gsl, axis=mybir.AxisListType.X), reads=[r_gate], writes=[r_gate])
                        S.op("dve", lambda e: e.reciprocal(out=mx[:, 3:4], in_=mx[:, 3:4]), reads=[r_gate], writes=[r_gate])
                        S.op("dve", TS(gsl, gsl, mx[:, 3:4], ALU.mult), reads=[r_gate], writes=[r_gate])
                    experts = [(moe_w_gate[0, e], moe_w_up[0, e], moe_w_down[0, e], FF_EXP, e) for e in range(8)]
                else:
                    experts = [(ffn_w_gate[0], ffn_w_up[0], ffn_w_down[0], FF_DENSE, None)]
                items = []
                for (Wg, Wu, Wd, FF, eidx) in experts:
                    Wgv = Wg.rearrange("(kc p) f -> p kc f", p=128)
                    Wuv = Wu.rearrange("(kc p) f -> p kc f", p=128)
                    Wdv = Wd.rearrange("(c p) d -> p c d", p=128)
                    for fi in range(FF // 256):
                        items.append((Wgv, Wuv, Wdv, fi, eidx))

                def ffn_loads(k):
                    Wgv, Wuv, Wdv, fi, eidx = items[k]
                    i = k % 2
                    f0 = fi * 256
                    S.dma("pool", DMA(wg[i], Wgv[:, :, f0:f0 + 256]), writes=[r_wg[i]])
                    S.dma("pool", DMA(wu[i], Wuv[:, :, f0:f0 + 256]), writes=[r_wg[i]])
                    S.dma("pool", DMA(wd[i], Wdv[:, fi * 2:fi * 2 + 2, :]), writes=[r_wd[i]])
                    S.op("pool", TT(wd[i], wd[i], bc_mid(gB[:, l, 1, :], 2), ALU.mult), reads=[r_gB], writes=[r_wd[i]])

                def ffn_gu(k):
                    i = k % 2
                    for fc in range(2):
                        for T in range(4):
                            ts_ = slice(T * 512, (T + 1) * 512)
                            bG, rbG = nb()
                            S.pe([MM(bG[:], wg[i][:, kc, fc * 128:(fc + 1) * 128], hT[:, kc, ts_], kc == 0, kc == 7) for kc in range(8)],
                                 reads=[r_wg[i], r_hT], writes=[rbG])
                            bU, rbU = nb()
                            S.pe([MM(bU[:], wu[i][:, kc, fc * 128:(fc + 1) * 128], hT[:, kc, ts_], kc == 0, kc == 7) for kc in range(8)],
                                 reads=[r_wg[i], r_hT], writes=[rbU])
                            Fg, rFg = nF()
                            S.op("act", ACT(Fg[:], bG[:], AF.Silu), reads=[rbG], writes=[rFg])
                            S.op("dve", TT(aT[i][:, fc, ts_], Fg[:], bU[:], ALU.mult), reads=[rFg, rbU], writes=[r_aT[i]])

                def ffn_down(k):
                    i = k % 2
                    eidx = items[k][4]
                    for tb in range(NTB):
                        for dh in range(2):
                            bk, rbk = nb()
                            S.pe([MM(bk[:], aT[i][:, fc, tb * 128:(tb + 1) * 128], wd[i][:, fc, dh * 512:(dh + 1) * 512], fc == 0, fc == 1)
                                  for fc in range(2)], reads=[r_aT[i], r_wd[i]], writes=[rbk])
                            rs_ = rr[:, tb, dh * 512:(dh + 1) * 512]
                            if eidx is None:
                                S.op("dve", TT(rs_, rs_, bk[:], ALU.add), reads=[rbk], writes=[r_rr[tb]])
                            else:
                                S.op("dve", STT(rs_, bk[:], gate_sb[:, tb, eidx:eidx + 1], rs_, ALU.mult, ALU.add),
                                     reads=[rbk, r_gate], writes=[r_rr[tb]])

                nk = len(items)
                ffn_loads(0)
                ffn_gu(0)
                for k in range(nk):
                    if k + 1 < nk:
                        ffn_loads(k + 1)
                        ffn_gu(k + 1)
                    ffn_down(k)
                S.mark(f"b{b}l{l} finalLN")
                dst = out[b] if l == depth - 1 else xs[b]
                r_dst = r_out if l == depth - 1 else r_xs
                for g4 in range(4):
                    ln_stats_group([(rr[:, g4 * 4 + i, :], r_rr[g4 * 4 + i], g4 * 4 + i) for i in range(4)])
                    for i in range(4):
                        tb = g4 * 4 + i
                        affine_k(rr[:, tb, :], r_rr[tb], tb)
                        S.dma("sp", DMA(dst[tb * 128:(tb + 1) * 128, :], rr[:, tb, :]), reads=[r_rr[tb]], writes=[r_dst])
                S.barrier()

    S.barrier()
    S.mark("end")
    ES.close()
    S.close()
    nc._marks = S.marks
    return nc


_CACHE = {}


def _get_nc(nseq, depth):
    key = (nseq, depth)
    if key not in _CACHE:
        _CACHE[key] = build_nc(nseq, depth)
    return _CACHE[key]


def make_in_maps(inputs, ncores, nseq):
    consts = host_consts()
    f = lambda a: np.ascontiguousarray(np.asarray(a, dtype=np.float32))
    shared = {k: f(inputs[k]) for k in ("rel_bias", "w_ada", "b_ada", "w_in", "w_branch", "w_out", "cmp_w1", "cmp_w2", "swa_sinks",
                                        "ln_g", "ln_b", "ffn_w_gate", "ffn_w_up", "ffn_w_down", "moe_router", "moe_w_gate",
                                        "moe_w_up", "moe_w_down")}
    shared["b_adaT"] = f(np.asarray(inputs["b_ada"]).reshape(2, 48, 128).transpose(0, 2, 1))
    shared["cmp_peT"] = f(np.asarray(inputs["cmp_pe"]).transpose(0, 1, 3, 2))
    shared.update(consts)
    x = np.asarray(inputs["x"], dtype=np.float32)
    c = np.asarray(inputs["c"], dtype=np.float32)
    maps = []
    for i in range(ncores):
        m = dict(shared)
        m["x"] = np.ascontiguousarray(x[i * nseq:(i + 1) * nseq])
        cc = c[i * nseq:(i + 1) * nseq]
        m["cT"] = np.ascontiguousarray(cc.reshape(nseq, 8, 128).transpose(0, 2, 1))
        maps.append(m)
    return maps


def kernel(**inputs):
    nseq = 2
    nc = _get_nc(nseq, DEPTH)
    maps = make_in_maps(inputs, NCORES, nseq)
    res = run_bass_kernel_spmd(nc, maps, core_ids=list(range(NCORES)))
    outs = [np.asarray(r["out"], dtype=np.float32) for r in res.results]
    return np.concatenate(outs, axis=0)
```
